# Optimizing a Trainium2 kernel written in Bass

```python
import math
import jax, jax.numpy as jnp
from jax import lax
import numpy as np


D_MODEL = 1024
BATCH = 8
SEQ = 4096
DEPTH = 4

RW_HEAD_DIM = 64
RW_WIDTH = D_MODEL // 2
RW_HEADS = RW_WIDTH // RW_HEAD_DIM
RW_DECAY_LORA = 64
RW_A_LORA = 64
RW_GATE_LORA = 160
RW_LN_EPS = 64e-5
GLA_HEADS = 4
GLA_VW = D_MODEL // 2
GLA_DV = GLA_VW // GLA_HEADS
GLA_DK = GLA_DV // 2
GLA_KW = GLA_HEADS * GLA_DK
GLA_GATE_LORA = 16
GLA_GATE_NORMALIZER = 16.0
GLA_CHUNK = 64
GLA_NORM_EPS = 1e-5
AB_IN = 3 * RW_WIDTH + 2 * GLA_KW + 2 * GLA_VW
AB_OUT = RW_WIDTH + GLA_VW
DA_QK_DIM = 64
DA_V_DIM = 2 * DA_QK_DIM
DA_HEADS = D_MODEL // DA_V_DIM
DA_QK_WIDTH = DA_HEADS * 2 * DA_QK_DIM
DA_V_WIDTH = DA_HEADS * DA_V_DIM
DA_Q_BLOCK = 128
DA_SUBLN_EPS = 1e-5
NEG_INF = -1e30
REL_BUCKETS = 32
REL_MAX_DIST = 128
FFN_HIDDEN = ((-(-8 * D_MODEL // 3) + 255) // 256) * 256
NORM_EPS = 1e-6
N_EVEN = (DEPTH + 1) // 2
N_ODD = DEPTH // 2

kernel_name = "hybrid_rwkv7_gla_diffattn_swiglu"


def rms_norm(x, g, eps=NORM_EPS):
    xf = x.astype(jnp.float32)
    y = xf * lax.rsqrt(jnp.mean(xf * xf, axis=-1, keepdims=True) + eps)
    return (y * g.astype(jnp.float32)).astype(x.dtype)


def token_shift(t):
    return jnp.pad(t, ((0, 0), (1, 0), (0, 0)))[:, :-1]


def swiglu(h, w_gate, w_up, w_down):
    return (jax.nn.silu(h @ w_gate) * (h @ w_up)) @ w_down


def t5_causal_buckets(dist):
    max_exact = REL_BUCKETS // 2
    ratio = jnp.maximum(dist, max_exact).astype(jnp.float32) / max_exact
    large = max_exact + (jnp.log(ratio) / math.log(REL_MAX_DIST / max_exact)
                         * (REL_BUCKETS - max_exact)).astype(jnp.int32)
    large = jnp.minimum(large, REL_BUCKETS - 1)
    return jnp.where(dist < max_exact, dist, large)


def diff_lambda_init(layer):
    return 0.8 - 0.6 * math.exp(-0.3 * layer)


def rwkv7_recurrence(r, w, k, v, a, b):
    Bsz, T, H, N = r.shape

    def step(S, inp):
        r_t, w_t, k_t, v_t, a_t, b_t = inp
        sa = jnp.einsum('bhvk,bhk->bhv', S, a_t)
        S = S * w_t[:, :, None, :] + sa[..., None] * b_t[:, :, None, :] + v_t[..., None] * k_t[:, :, None, :]
        return S, jnp.einsum('bhvk,bhk->bhv', S, r_t)

    xs = tuple(jnp.swapaxes(t, 0, 1) for t in (r, w, k, v, a, b))
    _, y = lax.scan(step, jnp.zeros((Bsz, H, N, N), jnp.float32), xs)
    return jnp.swapaxes(y, 0, 1)


def gla_chunked(q, k, v, log_a):
    Bsz, H, T, dk = q.shape
    dv = v.shape[-1]
    C = GLA_CHUNK
    n = T // C
    q, k, log_a = (t.reshape(Bsz, H, n, C, dk) for t in (q, k, log_a))
    v = v.reshape(Bsz, H, n, C, dv)
    b = jnp.cumsum(log_a, axis=3)
    b_last = b[:, :, :, -1:, :]
    q_dec = q * jnp.exp(b)
    scores = jnp.einsum('bhncd,bhnsd->bhncs', q_dec, k * jnp.exp(-b))
    causal = jnp.tril(jnp.ones((C, C), dtype=bool))
    o_intra = jnp.einsum('bhncs,bhnse->bhnce', jnp.where(causal, scores, 0.0), v)
    chunk_kv = jnp.einsum('bhnsd,bhnse->bhnde', k * jnp.exp(b_last - b), v)
    chunk_decay = jnp.exp(b_last[:, :, :, 0, :])

    def step(S, inp):
        kv, dec = inp
        return S * dec[..., None] + kv, S

    _, S_prev = lax.scan(step, jnp.zeros((Bsz, H, dk, dv), jnp.float32),
                         (jnp.moveaxis(chunk_kv, 2, 0), jnp.moveaxis(chunk_decay, 2, 0)))
    o_inter = jnp.einsum('bhncd,nbhde->bhnce', q_dec, S_prev)
    return (o_intra + o_inter).reshape(Bsz, H, T, dv)


def rwkv_gla_mixer(h, w_in, w_out, mu_rkv, mu_wag, w0, w1, w2, a0, a1, a2, g1, g2,
                   k_k, k_a, r_k, ln_w, ln_b, wa1, wa2, ba, gla_norm):
    f32 = jnp.float32
    Bsz, T, _ = h.shape
    proj = h @ w_in
    p_rkv = proj[..., :3 * RW_WIDTH]
    gq, gk, gv, gg = jnp.split(proj[..., 3 * RW_WIDTH:],
                               [GLA_KW, 2 * GLA_KW, 2 * GLA_KW + GLA_VW], axis=-1)

    rkv = p_rkv + (token_shift(p_rkv) - p_rkv) * mu_rkv.reshape(-1)
    r, k, v = [t.astype(f32) for t in jnp.split(rkv, 3, axis=-1)]
    dh = token_shift(h) - h
    xw = h + dh * mu_wag[0]
    xa = h + dh * mu_wag[1]
    xg = h + dh * mu_wag[2]
    w_raw = -jax.nn.softplus(-(w0 + jnp.tanh(xw @ w1) @ w2).astype(f32)) - 0.5
    decay = jnp.exp(-jnp.exp(w_raw))
    a = jax.nn.sigmoid((a0 + (xa @ a1) @ a2).astype(f32))
    g = jax.nn.sigmoid(xg @ g1) @ g2

    def heads(t):
        return t.reshape(Bsz, T, RW_HEADS, RW_HEAD_DIM)

    kk = heads(k * k_k.astype(f32))
    kk = kk / jnp.maximum(jnp.sqrt(jnp.sum(kk * kk, axis=-1, keepdims=True)), 1e-12)
    k = k * (1.0 + (a - 1.0) * k_a.astype(f32))
    rh, kh, vh = heads(r), heads(k), heads(v)
    y = rwkv7_recurrence(rh, heads(decay), kh, vh, -kk, kk * heads(a))
    mean = jnp.mean(y, axis=-1, keepdims=True)
    var = jnp.mean(jnp.square(y - mean), axis=-1, keepdims=True)
    y = ((y - mean) * lax.rsqrt(var + RW_LN_EPS)).reshape(Bsz, T, RW_WIDTH)
    y = y * ln_w.astype(f32) + ln_b.astype(f32)
    bonus = jnp.sum(rh * kh * r_k.astype(f32), axis=-1, keepdims=True) * vh
    o_a = (y + bonus.reshape(Bsz, T, RW_WIDTH)).astype(h.dtype) * g

    def gheads(t, d):
        return t.reshape(Bsz, T, GLA_HEADS, d).transpose(0, 2, 1, 3).astype(f32)

    log_a = jax.nn.log_sigmoid(((h @ wa1) @ wa2 + ba).astype(f32)) / GLA_GATE_NORMALIZER
    o = gla_chunked(gheads(gq, GLA_DK) * (GLA_DK ** -0.5), gheads(gk, GLA_DK),
                    gheads(gv, GLA_DV), gheads(log_a, GLA_DK))
    o = o.transpose(0, 2, 1, 3)
    o = o * lax.rsqrt(jnp.mean(o * o, axis=-1, keepdims=True) + GLA_NORM_EPS)
    o = o * gla_norm.astype(f32).reshape(GLA_HEADS, GLA_DV)
    o_b = o.reshape(Bsz, T, GLA_VW).astype(h.dtype) * jax.nn.silu(gg)

    return jnp.concatenate([o_a, o_b], axis=-1) @ w_out


def diff_attention_core(q1, q2, k1, k2, v, lam, bias_by_dist):
    Bsz, H, T, d = q1.shape
    nb = T // DA_Q_BLOCK
    scale = d ** -0.5

    def to_blocks(t):
        return t.reshape(Bsz, H, nb, DA_Q_BLOCK, d).transpose(2, 0, 1, 3, 4)

    starts = jnp.arange(nb, dtype=jnp.int32) * DA_Q_BLOCK
    k_pos = jnp.arange(T, dtype=jnp.int32)

    def block(args):
        q1b, q2b, start = args
        dist = start + jnp.arange(DA_Q_BLOCK, dtype=jnp.int32)[:, None] - k_pos[None, :]
        causal = dist >= 0
        bias = jnp.transpose(bias_by_dist[jnp.clip(dist, 0, T - 1)], (2, 0, 1)).astype(jnp.float32)

        def probs(qb, kb):
            s = jnp.einsum('bhqd,bhkd->bhqk', qb, kb, preferred_element_type=jnp.float32) * scale + bias
            return jax.nn.softmax(jnp.where(causal, s, NEG_INF), axis=-1)

        attn = probs(q1b, k1) - lam * probs(q2b, k2)
        return jnp.einsum('bhqk,bhke->bhqe', attn.astype(v.dtype), v)

    out = lax.map(block, (to_blocks(q1), to_blocks(q2), starts))
    return out.transpose(1, 2, 0, 3, 4).reshape(Bsz, H, T, v.shape[-1])


def diff_attn_mixer(h, w_qkv, w_out, lam_q1, lam_k1, lam_q2, lam_k2, subln, bias_by_dist, lam_init):
    f32 = jnp.float32
    Bsz, T, _ = h.shape
    q, k, v = jnp.split(h @ w_qkv, [DA_QK_WIDTH, 2 * DA_QK_WIDTH], axis=-1)
    q = q.reshape(Bsz, T, DA_HEADS, 2, DA_QK_DIM).transpose(3, 0, 2, 1, 4)
    k = k.reshape(Bsz, T, DA_HEADS, 2, DA_QK_DIM).transpose(3, 0, 2, 1, 4)
    v = v.reshape(Bsz, T, DA_HEADS, DA_V_DIM).transpose(0, 2, 1, 3)
    lam = (jnp.exp(jnp.sum(lam_q1.astype(f32) * lam_k1.astype(f32)))
           - jnp.exp(jnp.sum(lam_q2.astype(f32) * lam_k2.astype(f32))) + lam_init)
    o = diff_attention_core(q[0], q[1], k[0], k[1], v, lam, bias_by_dist).astype(f32)
    o = o * lax.rsqrt(jnp.mean(o * o, axis=-1, keepdims=True) + DA_SUBLN_EPS)
    o = o * subln.astype(f32) * (1.0 - lam_init)
    o = o.astype(h.dtype).transpose(0, 2, 1, 3).reshape(Bsz, T, DA_V_WIDTH)
    return o @ w_out


def setup_inputs(seed: int = 0) -> dict:
    key = jax.random.key(seed)
    ks = jax.random.split(key, 48)
    counter = [0]
    f32 = jnp.float32

    def nxt():
        kk = ks[counter[0]]
        counter[0] += 1
        return kk

    def nrm(shape, scale):
        return jax.random.normal(nxt(), shape, f32) * scale

    def unif(shape, lo, hi):
        return jax.random.uniform(nxt(), shape, f32, lo, hi)

    def gain(shape):
        return 1.0 + nrm(shape, 0.02)

    D, E, O = D_MODEL, N_EVEN, N_ODD
    return {
        "x": nrm((BATCH, SEQ, D), 1.0),
        "rel_bias": nrm((REL_BUCKETS, DA_HEADS), 0.5),
        "norm_mix": gain((DEPTH, D)),
        "norm_ffn": gain((DEPTH, D)),
        "norm_final": gain((D,)),
        "ab_w_in": nrm((E, D, AB_IN), D ** -0.5),
        "ab_w_out": nrm((E, AB_OUT, D), AB_OUT ** -0.5),
        "rw_mu_rkv": unif((E, 3, RW_WIDTH), 0.0, 1.0),
        "rw_mu_wag": unif((E, 3, D), 0.0, 1.0),
        "rw_w0": unif((E, RW_WIDTH), -6.0, -1.0),
        "rw_w1": nrm((E, D, RW_DECAY_LORA), D ** -0.5),
        "rw_w2": nrm((E, RW_DECAY_LORA, RW_WIDTH), 0.5 * RW_DECAY_LORA ** -0.5),
        "rw_a0": nrm((E, RW_WIDTH), 0.1),
        "rw_a1": nrm((E, D, RW_A_LORA), D ** -0.5),
        "rw_a2": nrm((E, RW_A_LORA, RW_WIDTH), 0.5 * RW_A_LORA ** -0.5),
        "rw_g1": nrm((E, D, RW_GATE_LORA), D ** -0.5),
        "rw_g2": nrm((E, RW_GATE_LORA, RW_WIDTH), RW_GATE_LORA ** -0.5),
        "rw_k_k": 0.85 + nrm((E, RW_WIDTH), 0.02),
        "rw_k_a": 1.0 + nrm((E, RW_WIDTH), 0.02),
        "rw_r_k": -0.04 + nrm((E, RW_HEADS, RW_HEAD_DIM), 0.1),
        "rw_ln_w": gain((E, RW_WIDTH)),
        "rw_ln_b": nrm((E, RW_WIDTH), 0.02),
        "gla_wa1": nrm((E, D, GLA_GATE_LORA), D ** -0.5),
        "gla_wa2": nrm((E, GLA_GATE_LORA, GLA_KW), GLA_GATE_LORA ** -0.5),
        "gla_ba": nrm((E, GLA_KW), 0.1),
        "gla_norm": gain((E, GLA_VW)),
        "da_w_qkv": nrm((O, D, 2 * DA_QK_WIDTH + DA_V_WIDTH), D ** -0.5),
        "da_w_out": nrm((O, DA_V_WIDTH, D), DA_V_WIDTH ** -0.5),
        "da_lam_q1": nrm((O, DA_QK_DIM), 0.1),
        "da_lam_k1": nrm((O, DA_QK_DIM), 0.1),
        "da_lam_q2": nrm((O, DA_QK_DIM), 0.1),
        "da_lam_k2": nrm((O, DA_QK_DIM), 0.1),
        "da_subln": gain((O, DA_V_DIM)),
        "ffn_w_gate": nrm((DEPTH, D, FFN_HIDDEN), D ** -0.5),
        "ffn_w_up": nrm((DEPTH, D, FFN_HIDDEN), D ** -0.5),
        "ffn_w_down": nrm((DEPTH, FFN_HIDDEN, D), FFN_HIDDEN ** -0.5),
    }


def reference(x, rel_bias, norm_mix, norm_ffn, norm_final, ab_w_in, ab_w_out,
              rw_mu_rkv, rw_mu_wag, rw_w0, rw_w1, rw_w2, rw_a0, rw_a1, rw_a2, rw_g1, rw_g2,
              rw_k_k, rw_k_a, rw_r_k, rw_ln_w, rw_ln_b, gla_wa1, gla_wa2, gla_ba, gla_norm,
              da_w_qkv, da_w_out, da_lam_q1, da_lam_k1, da_lam_q2, da_lam_k2, da_subln,
              ffn_w_gate, ffn_w_up, ffn_w_down):
    T = x.shape[1]
    bias_by_dist = rel_bias[t5_causal_buckets(jnp.arange(T, dtype=jnp.int32))]
    for layer in range(DEPTH):
        i = layer // 2
        h = rms_norm(x, norm_mix[layer])
        if layer % 2 == 0:
            mix = rwkv_gla_mixer(h, ab_w_in[i], ab_w_out[i], rw_mu_rkv[i], rw_mu_wag[i],
                                 rw_w0[i], rw_w1[i], rw_w2[i], rw_a0[i], rw_a1[i], rw_a2[i],
                                 rw_g1[i], rw_g2[i], rw_k_k[i], rw_k_a[i], rw_r_k[i],
                                 rw_ln_w[i], rw_ln_b[i], gla_wa1[i], gla_wa2[i], gla_ba[i], gla_norm[i])
        else:
            mix = diff_attn_mixer(h, da_w_qkv[i], da_w_out[i], da_lam_q1[i], da_lam_k1[i],
                                  da_lam_q2[i], da_lam_k2[i], da_subln[i], bias_by_dist,
                                  diff_lambda_init(layer))
        x = x + mix
        x = x + swiglu(rms_norm(x, norm_ffn[layer]), ffn_w_gate[layer], ffn_w_up[layer], ffn_w_down[layer])
    return rms_norm(x, norm_final)
```

```python
import math
from contextlib import ExitStack
import numpy as np
import concourse.bass as bass
import concourse.mybir as mybir
from concourse.bass_utils import run_bass_kernel_spmd

F32 = mybir.dt.float32
BF16 = mybir.dt.bfloat16
AF = mybir.ActivationFunctionType
ALU = mybir.AluOpType
AX = mybir.AxisListType

D = 1024
T = 4096
DEPTH = 4
FF = 2816
NFC = FF // 128
NEG = -1e30

ENGS = ("pe", "act", "dve", "pool", "sp")
EPOCH = 30000
N_DMA_SEMS = 24


class Buf:
    __slots__ = ("name", "writers", "readers")

    def __init__(self, name=""):
        self.name = name
        self.writers = []
        self.readers = []


class Op:
    __slots__ = ("eng", "fn", "deps", "signal", "val", "sem", "dma")

    def __init__(self, eng, fn, dma):
        self.eng = eng
        self.fn = fn
        self.dma = dma
        self.deps = []
        self.signal = False
        self.val = None
        self.sem = None


class Prog:
    def __init__(self):
        self.ops = {e: [] for e in ENGS}
        self.dma_slot_last = [None] * N_DMA_SEMS
        self.dma_slot_cnt = [0] * N_DMA_SEMS
        self.dma_n = 0
        self.cnt = {e: 0 for e in ENGS}
        self.seen = {e: {} for e in ENGS}
        self.last_real = {e: None for e in ENGS}
        self.bufs = []

    def buf(self, name=""):
        b = Buf(name)
        self.bufs.append(b)
        return b

    def op(self, eng, fn, reads=(), writes=(), dma=False):
        o = Op(eng, fn, dma)
        deps = {}
        for b in reads:
            for d in b.writers:
                deps[id(d)] = d
        for b in writes:
            for d in b.writers + b.readers:
                if (not dma) and (not d.dma) and d.eng == eng:
                    continue
                deps[id(d)] = d
        if dma:
            slot = self.dma_n % N_DMA_SEMS
            self.dma_n += 1
            prev = self.dma_slot_last[slot]
            if prev is not None:
                deps[id(prev)] = prev
            self.dma_slot_last[slot] = o
            self.dma_slot_cnt[slot] += 1
            o.sem = ("dma", slot)
            o.val = 16 * self.dma_slot_cnt[slot]
        for d in deps.values():
            d.signal = True
            o.deps.append(d)
        for b in reads:
            if not dma:
                b.readers = [r for r in b.readers if r.dma or r.eng != eng]
            b.readers.append(o)
        for b in writes:
            if b.readers:
                b.writers = [o]
                b.readers = []
            else:
                if not dma:
                    b.writers = [w for w in b.writers if w.dma or w.eng != eng]
                b.writers.append(o)
        self.ops[eng].append(o)
        self.last_real[eng] = o
        return o

    def barrier(self):
        lasts = [self.last_real[e] for e in ENGS if self.last_real[e] is not None]
        dl = [d for d in self.dma_slot_last if d is not None]
        for e in ENGS:
            o = Op(e, None, False)
            for d in lasts + dl:
                if d.eng == e and not d.dma:
                    continue
                d.signal = True
                o.deps.append(d)
            self.ops[e].append(o)
        for b in self.bufs:
            b.writers = []
            b.readers = []

    def finalize(self):
        for e in ENGS:
            for o in self.ops[e]:
                if o.dma or o.fn is None or o.val is not None:
                    continue
                if o.signal:
                    c = self.cnt[e]
                    o.sem = (e, c // EPOCH)
                    o.val = c % EPOCH + 1
                    self.cnt[e] = c + 1

    def replay(self, eng_name, e, get_sem, final=False):
        seen = self.seen[eng_name]
        for o in self.ops[eng_name]:
            waits = {}
            for d in o.deps:
                k = d.sem
                if d.val > waits.get(k, 0):
                    waits[k] = d.val
            for k, v in waits.items():
                if seen.get(k, 0) >= v:
                    continue
                e.wait_ge(get_sem(k), v)
                seen[k] = v
            if o.fn is None:
                continue
            ins = o.fn(e)
            if o.dma:
                ins.then_inc(get_sem(o.sem), 16)
            elif o.signal:
                ins.then_inc(get_sem(o.sem), 1)
        if final:
            for slot in range(N_DMA_SEMS):
                c = self.dma_slot_cnt[slot]
                if c and seen.get(("dma", slot), 0) < 16 * c:
                    e.wait_ge(get_sem(("dma", slot)), 16 * c)
        self.ops[eng_name] = []


class Tl:
    __slots__ = ("t", "b")

    def __init__(self, t, b):
        self.t = t
        self.b = b


class Builder:
    def __init__(self, layers, t_len=T):
        self.nc = bass.Bass("TRN2", target_bir_lowering=False)
        self.P = Prog()
        self.ges = ExitStack()
        self.sems = {}
        self.layers = layers
        self.T = t_len
        self.uid = 0

    def get_sem(self, k):
        return self.sems[k]

    def ensure_sems(self):
        need = [("dma", s) for s in range(N_DMA_SEMS)]
        for e in ENGS:
            for ep in range(self.P.cnt[e] // EPOCH + 1):
                need.append((e, ep))
        for k in need:
            if k not in self.sems:
                self.sems[k] = self.ges.enter_context(self.nc.semaphore(f"s_{k[0]}_{k[1]}"))

    def dram(self, name, shape, dt, kind=None):
        if kind:
            return self.nc.dram_tensor(name, list(shape), dt, kind=kind).ap()
        return self.nc.dram_tensor(name, list(shape), dt).ap()

    def sb(self, es, shape, dt, name=None):
        self.uid += 1
        t = es.enter_context(self.nc.sbuf_tensor(f"{name or 't'}_{self.uid}", list(shape), dt))
        return Tl(t, self.P.buf(name))

    def ps(self, es, shape, dt, name=None):
        self.uid += 1
        t = es.enter_context(self.nc.psum_tensor(f"{name or 'p'}_{self.uid}", list(shape), dt))
        return Tl(t, self.P.buf(name))

    @staticmethod
    def _b(xs):
        return [x.b if isinstance(x, Tl) else x for x in xs]

    def mm(self, out, lhsT, rhs, start, stop, R, W):
        self.P.op("pe", lambda e: e.matmul(out, lhsT=lhsT, rhs=rhs, start=start, stop=stop), self._b(R), self._b(W))

    def tr(self, out, in_, ident, R, W):
        self.P.op("pe", lambda e: e.transpose(out, in_, ident), self._b(R), self._b(W))

    def act(self, out, in_, func, R, W, bias=None, scale=None):
        kw = {}
        if bias is not None:
            kw["bias"] = bias
        if scale is not None:
            kw["scale"] = scale
        self.P.op("act", lambda e: e.activation(out=out, in_=in_, func=func, **kw), self._b(R), self._b(W))

    def tt(self, eng, out, in0, in1, op, R, W):
        self.P.op(eng, lambda e: e.tensor_tensor(out=out, in0=in0, in1=in1, op=op), self._b(R), self._b(W))

    def ts(self, eng, out, in0, s1, s2, op0, op1, R, W):
        if s2 is None:
            self.P.op(eng, lambda e: e.tensor_scalar(out=out, in0=in0, scalar1=s1, scalar2=None, op0=op0), self._b(R), self._b(W))
        else:
            self.P.op(eng, lambda e: e.tensor_scalar(out=out, in0=in0, scalar1=s1, scalar2=s2, op0=op0, op1=op1), self._b(R), self._b(W))

    def stt(self, eng, out, in0, scalar, in1, op0, op1, R, W):
        eng = "dve"
        self.P.op(eng, lambda e: e.scalar_tensor_tensor(out=out, in0=in0, scalar=scalar, in1=in1, op0=op0, op1=op1), self._b(R), self._b(W))

    def cp(self, eng, out, in_, R, W):
        if eng == "act":
            self.P.op("act", lambda e: e.activation(out=out, in_=in_, func=AF.Copy), self._b(R), self._b(W))
        else:
            self.P.op(eng, lambda e: e.tensor_copy(out=out, in_=in_), self._b(R), self._b(W))

    def recip(self, out, in_, R, W):
        self.P.op("dve", lambda e: e.reciprocal(out=out, in_=in_), self._b(R), self._b(W))

    def memset(self, eng, ap, val, W):
        self.P.op(eng, lambda e: e.memset(ap, val), [], self._b(W))

    def dma(self, eng, out, in_, R, W):
        self.P.op(eng, lambda e: e.dma_start(out=out, in_=in_), self._b(R), self._b(W), dma=True)

    def emit_phase(self, final=False):
        P = self.P
        P.barrier()
        P.finalize()
        self.ensure_sems()
        nc = self.nc
        with nc.Block() as block:
            @block.tensor
            def _(e):
                P.replay("pe", e, self.get_sem)

            @block.scalar
            def _(e):
                P.replay("act", e, self.get_sem)

            @block.vector
            def _(e):
                P.replay("dve", e, self.get_sem)

            @block.gpsimd
            def _(e):
                P.replay("pool", e, self.get_sem)

            @block.sync
            def _(e):
                P.replay("sp", e, self.get_sem, final=final)


def bcast_free(tl_ap_tensor, offset, pstride, nparts, n):
    return bass.AP(tl_ap_tensor, offset, [[pstride, nparts], [0, n]])


def slabA(w, kchunks):
    K, Fd = w.shape
    return np.ascontiguousarray(w.reshape(kchunks, 128, Fd // 128, 128).transpose(2, 1, 0, 3))


def slabB(w, kchunks, ncol):
    K, Fd = w.shape
    return np.ascontiguousarray(w.reshape(kchunks, 128, Fd // ncol, ncol).transpose(2, 1, 0, 3))


def pvec(v):
    return np.ascontiguousarray(v.reshape(-1, 128).T)


def t5_buckets_np(n):
    dist = np.arange(n, dtype=np.int32)
    max_exact = 16
    ratio = np.maximum(dist, max_exact).astype(np.float32) / np.float32(max_exact)
    large = max_exact + (np.log(ratio).astype(np.float32) / np.float32(math.log(128 / 16)) * np.float32(16)).astype(np.int32)
    large = np.minimum(large, 31)
    return np.where(dist < max_exact, dist, large)


VEC_COLS = {}
_vc = 0


def _vadd(name, n):
    global _vc
    VEC_COLS[name] = (_vc, n)
    _vc += n


for _l in range(DEPTH):
    _vadd(f"nmix{_l}", 8)
    _vadd(f"nffn{_l}", 8)
_vadd("nfinal", 8)
for _i in range(2):
    _vadd(f"subln{_i}", 1)
    _vadd(f"lam{_i}", 4)
    _vadd(f"mu_rkv{_i}", 12)
    _vadd(f"mu_wag{_i}", 24)
    _vadd(f"a0{_i}", 4)
    _vadd(f"k_k{_i}", 4)
    _vadd(f"k_a{_i}", 4)
    _vadd(f"r_k{_i}", 4)
    _vadd(f"ln_w{_i}", 4)
    _vadd(f"ln_b{_i}", 4)
    _vadd(f"gnorm{_i}", 4)
_vadd("bfar", 8)
NVEC = _vc


def pack_vecs(inp):
    v = np.zeros((128, NVEC), np.float32)

    def put(name, arr):
        c, n = VEC_COLS[name]
        v[:, c:c + n] = arr

    for l in range(DEPTH):
        put(f"nmix{l}", pvec(inp["norm_mix"][l]))
        put(f"nffn{l}", pvec(inp["norm_ffn"][l]))
    put("nfinal", pvec(inp["norm_final"]))
    for i in range(2):
        put(f"subln{i}", inp["da_subln"][i].reshape(128, 1))
        lam = np.zeros((128, 4), np.float32)
        for j, nm in enumerate(["da_lam_q1", "da_lam_k1", "da_lam_q2", "da_lam_k2"]):
            lam[:64, j] = inp[nm][i]
        put(f"lam{i}", lam)
        put(f"mu_rkv{i}", pvec(inp["rw_mu_rkv"][i].reshape(-1)))
        put(f"mu_wag{i}", pvec(inp["rw_mu_wag"][i].reshape(-1)))
        put(f"a0{i}", pvec(inp["rw_a0"][i]))
        put(f"k_k{i}", pvec(inp["rw_k_k"][i]))
        put(f"k_a{i}", pvec(inp["rw_k_a"][i]))
        put(f"r_k{i}", pvec(inp["rw_r_k"][i].reshape(-1)))
        put(f"ln_w{i}", pvec(inp["rw_ln_w"][i]))
        put(f"ln_b{i}", pvec(inp["rw_ln_b"][i]))
        put(f"gnorm{i}", pvec(inp["gla_norm"][i]))
    bk = t5_buckets_np(T)
    assert (bk[113:] == bk[113]).all()
    put("bfar", np.broadcast_to(inp["rel_bias"][bk[200]][None, :], (128, 8)))
    return v


def pack_shared(inp):
    sh = {}
    w_in = [inp["ab_w_in"][0], inp["da_w_qkv"][0], inp["ab_w_in"][1], inp["da_w_qkv"][1]]
    w_out = [inp["ab_w_out"][0], inp["da_w_out"][0], inp["ab_w_out"][1], inp["da_w_out"][1]]
    sh["w_inA"] = np.stack([slabA(w, 8) for w in w_in])
    sh["w_inB"] = np.stack([slabB(w, 8, 512) for w in w_in])
    sh["w_out"] = np.stack([slabA(w, 8) for w in w_out])
    gu = []
    for l in range(DEPTH):
        g = slabA(inp["ffn_w_gate"][l], 8)
        u = slabA(inp["ffn_w_up"][l], 8)
        gu.append(np.concatenate([g, u], axis=3))
    sh["w_gu"] = np.stack(gu)
    sh["w_dn"] = np.stack([slabA(inp["ffn_w_down"][l], NFC) for l in range(DEPTH)])
    sh["vecs"] = pack_vecs(inp)
    bk = t5_buckets_np(T)
    bbd = inp["rel_bias"][bk]
    idx = np.arange(1024)[None, :] - np.arange(128)[:, None] - 384
    G = np.full((8, 128, 1024), NEG, np.float32)
    pos = idx >= 0
    for h in range(8):
        G[h][pos] = bbd[idx[pos], h]
    sh["biasG"] = np.ascontiguousarray(G.transpose(1, 0, 2))
    l1 = []
    for i in range(2):
        cat = np.concatenate([inp["rw_w1"][i], inp["rw_a1"][i], inp["rw_g1"][i], inp["gla_wa1"][i]], axis=1)
        l1.append(np.ascontiguousarray(cat.reshape(8, 128, 304).transpose(1, 0, 2)))
    sh["lora1"] = np.stack(l1)
    l2 = np.zeros((2, 128, 5, 512), np.float32)
    for i in range(2):
        l2[i, 0:64, 0, :] = inp["rw_w2"][i]
        l2[i, 64:128, 1, :] = inp["rw_a2"][i]
        l2[i, :, 2, :] = inp["rw_g2"][i][0:128]
        l2[i, 0:32, 3, :] = inp["rw_g2"][i][128:160]
        l2[i, 32:48, 4, 0:256] = inp["gla_wa2"][i]
    sh["lora2"] = l2
    rows = np.zeros((2, 1, 768), np.float32)
    for i in range(2):
        rows[i, 0, 0:512] = inp["rw_w0"][i]
        rows[i, 0, 512:768] = inp["gla_ba"][i]
    sh["rows"] = rows
    return sh


def flat2d(ap, n_elems, maxcols=8192):
    raise NotImplementedError


class Kern(Builder):
    def __init__(self, layers, t_len=T, dbg=False):
        super().__init__(layers, t_len)
        self.dbg = dbg
        nc = self.nc
        Tn = self.T
        self.xT = self.dram("xT", [D, Tn], F32, "ExternalInput")
        shp = {
            "w_inA": [4, 24, 128, 8, 128], "w_inB": [4, 6, 128, 8, 512], "w_out": [4, 8, 128, 8, 128],
            "w_gu": [4, NFC, 128, 8, 256], "w_dn": [4, 8, 128, NFC, 128],
        }
        self.wf = {k: self.dram(k, s, F32, "ExternalInput") for k, s in shp.items()}
        self.wb = {k: self.dram(k + "_bf", s, BF16) for k, s in shp.items()}
        self.wshape = shp
        self.vecs_d = self.dram("vecs", [128, NVEC], F32, "ExternalInput")
        self.biasG_d = self.dram("biasG", [128, 8, 1024], F32, "ExternalInput")
        self.lora1_d = self.dram("lora1", [2, 128, 8, 304], F32, "ExternalInput")
        self.lora2_d = self.dram("lora2", [2, 128, 5, 512], F32, "ExternalInput")
        self.rows_d = self.dram("rows", [2, 1, 768], F32, "ExternalInput")
        self.yT = self.dram("yT", [D, Tn], F32, "ExternalOutput")
        self.x_s = self.dram("x_s", [D, Tn], F32)
        self.mix_s = self.dram("mix_s", [D, Tn], BF16)
        self.q_s = self.dram("q_s", [8, 128, Tn], BF16)
        self.k_s = self.dram("k_s", [8, 128, Tn], BF16)
        self.v_s = self.dram("v_s", [Tn, D], BF16)
        if dbg:
            self.dbg_mix = self.dram("dbg_mix", [D, Tn], BF16, "ExternalOutput")
        self.b_x = self.P.buf("x_dram")
        self.b_mix = self.P.buf("mix_dram")
        self.b_qkv = self.P.buf("qkv_dram")
        self.b_wl = [self.P.buf(f"w_dram{l}") for l in range(DEPTH)]

    def cast_layer(self, l):
        for k, s in self.wshape.items():
            n = int(np.prod(s[1:]))
            cols = n // 128
            src = self.wf[k][l].rearrange("a p k j -> (a p k j)").rearrange("(r c) -> r c", r=128) if False else None
            ft = self.wf[k].tensor
            bt = self.wb[k].tensor
            base = l * n
            piece = 8192
            c0 = 0
            while c0 < cols:
                cc = min(piece, cols - c0)
                src = bass.AP(ft, base + c0, [[cols, 128], [1, cc]])
                dst = bass.AP(bt, base + c0, [[cols, 128], [1, cc]])
                self.dma("pool", dst, src, [], [self.b_wl[l]])
                c0 += cc

    def setup(self):
        nc = self.nc
        es = self.ges
        self.ident = self.sb(es, [128, 128], BF16, "ident")
        self.ones = self.sb(es, [128, 128], BF16, "ones")
        self.vecs = self.sb(es, [128, NVEC], F32, "vecs")
        self.vx = self.sb(es, [128, 64], F32, "vx")
        self.memset("pool", self.ident.t[:], 0.0, [self.ident])
        self.P.op("pool", lambda e: e.affine_select(out=self.ident.t[:], in_=self.ident.t[:], pattern=[[-1, 128]],
                                                    compare_op=ALU.not_equal, fill=1.0, base=0, channel_multiplier=1),
                  [self.ident.b], [self.ident.b])
        self.memset("pool", self.ones.t[:], 1.0, [self.ones])
        self.dma("sp", self.vecs.t[:], self.vecs_d, [], [self.vecs])
        self.cast_layer(self.layers[0])
        self.emit_phase()

    def vcol(self, name, j=0, n=1):
        c, _ = VEC_COLS[name]
        return self.vecs.t[:, c + j:c + j + n]

    def rmsnorm(self, xt, hT, hoff, TT, gsc, sq, pbank, rstd, eps, out_dt_f32=None):
        for kc in range(8):
            self.act(sq.t[:, kc, :], xt.t[:, kc, :], AF.Square, [xt], [sq])
        for kc in range(8):
            self.mm(pbank.t[:, 0:TT], self.ones.t[:], sq.t[:, kc, :], kc == 0, kc == 7, [self.ones, sq], [pbank])
        self.act(rstd.t[:, 0:TT], pbank.t[:, 0:TT], AF.Sqrt, [pbank], [rstd], bias=self.epsD(eps), scale=1.0)
        self.recip(rstd.t[:, 0:TT], rstd.t[:, 0:TT], [rstd], [rstd])
        for kc in range(8):
            eng = "dve" if kc % 2 == 0 else "pool"
            self.stt(eng, hT.t[:, kc, hoff:hoff + TT], xt.t[:, kc, :], gsc[:, kc:kc + 1], rstd.t[:, 0:TT],
                     ALU.mult, ALU.mult, [xt, rstd, self.vx], [hT])

    def epsD(self, eps):
        return float(D * eps)

    def phase_ffn(self, l, x_src, last, next_layer):
        TT = 512
        NT = self.T // TT
        sqD = math.sqrt(D)
        with ExitStack() as es:
            xt = [self.sb(es, [128, 8, TT], F32, "xt") for _ in range(2)]
            mt = [self.sb(es, [128, 8, TT], BF16, "mt") for _ in range(2)]
            hT = self.sb(es, [128, 8, TT], BF16, "hT")
            sq = self.sb(es, [128, 8, TT], BF16, "sq")
            rstd = self.sb(es, [128, TT], F32, "rstd")
            actb = self.sb(es, [128, NFC, TT], BF16, "actb")
            wgu = [self.sb(es, [128, 8, 256], BF16, "wgu") for _ in range(5)]
            wdn = [self.sb(es, [128, NFC, 128], BF16, "wdn") for _ in range(8)]
            sq2 = self.sb(es, [128, 8, TT], BF16, "sq2") if last else None
            rstd2 = self.sb(es, [128, TT], F32, "rstd2") if last else None
            wo = [self.sb(es, [128, 8, 128], BF16, "wo") for _ in range(8)]
            sg = [self.sb(es, [128, TT], BF16, "sg") for _ in range(2)]
            for dc in range(8):
                self.dma("sp", wo[dc].t[:], self.wb["w_out"][l, dc], [self.b_wl[l]], [wo[dc]])
            for dc in range(8):
                self.dma("sp", wdn[dc].t[:], self.wb["w_dn"][l, dc], [self.b_wl[l]], [wdn[dc]])
            pb = [self.ps(es, [128, 512], F32, "pb") for _ in range(8)]
            pbi = [0]

            def bank():
                b = pb[pbi[0] % 8]
                pbi[0] += 1
                return b

            c_ffn, _ = VEC_COLS[f"nffn{l}"]
            self.ts("dve", self.vx.t[:, 0:8], self.vecs.t[:, c_ffn:c_ffn + 8], sqD, None, ALU.mult, None, [self.vecs], [self.vx])
            if last:
                c_fin, _ = VEC_COLS["nfinal"]
                self.ts("dve", self.vx.t[:, 8:16], self.vecs.t[:, c_fin:c_fin + 8], sqD, None, ALU.mult, None, [self.vecs], [self.vx])

            xsrc_v = x_src.rearrange("(kc p) t -> p kc t", p=128)
            xs_v = self.x_s.rearrange("(kc p) t -> p kc t", p=128)
            y_v = self.yT.rearrange("(kc p) t -> p kc t", p=128)
            mix_v = self.mix_s.rearrange("(kc p) t -> p kc t", p=128)
            wi = [0, 0, 0]

            def load_tile(i):
                t0 = i * TT
                self.dma("pool", xt[i % 2].t[:], xsrc_v[:, :, t0:t0 + TT], [self.b_x], [xt[i % 2]])
                self.dma("pool", mt[i % 2].t[:], mix_v[:, :, t0:t0 + TT], [self.b_mix], [mt[i % 2]])

            def outproj(i):
                x = xt[i % 2]
                m = mt[i % 2]
                for dc in range(8):
                    w = wo[dc]
                    p = bank()
                    for kc in range(8):
                        self.mm(p.t[:, 0:TT], w.t[:, kc, :], m.t[:, kc, :], kc == 0, kc == 7, [w, m], [p])
                    self.tt("dve", x.t[:, dc, :], x.t[:, dc, :], p.t[:, 0:TT], ALU.add, [x, p], [x])
                for kc in range(8):
                    self.act(sq.t[:, kc, :], x.t[:, kc, :], AF.Square, [x], [sq])

            def norm_rest(i):
                x = xt[i % 2]
                p = bank()
                for kc in range(8):
                    self.mm(p.t[:, 0:TT], self.ones.t[:], sq.t[:, kc, :], kc == 0, kc == 7, [self.ones, sq], [p])
                self.act(rstd.t[:, 0:TT], p.t[:, 0:TT], AF.Sqrt, [p], [rstd], bias=self.epsD(1e-6), scale=1.0)
                self.recip(rstd.t[:, 0:TT], rstd.t[:, 0:TT], [rstd], [rstd])
                for kc in range(8):
                    self.stt("dve", hT.t[:, kc, 0:TT], x.t[:, kc, :], self.vx.t[:, kc:kc + 1], rstd.t[:, 0:TT],
                             ALU.mult, ALU.mult, [x, rstd, self.vx], [hT])

            load_tile(0)
            if NT > 1:
                load_tile(1)
            outproj(0)
            norm_rest(0)
            for i in range(NT):
                t0 = i * TT
                x = xt[i % 2]
                for fc in range(NFC):
                    w = wgu[wi[1] % len(wgu)]
                    wi[1] += 1
                    self.dma("sp", w.t[:], self.wb["w_gu"][l, fc], [self.b_wl[l]], [w])
                    pg = bank()
                    pu = bank()
                    for kc in range(8):
                        self.mm(pg.t[:, 0:TT], w.t[:, kc, 0:128], hT.t[:, kc, :], kc == 0, kc == 7, [w, hT], [pg])
                    for kc in range(8):
                        self.mm(pu.t[:, 0:TT], w.t[:, kc, 128:256], hT.t[:, kc, :], kc == 0, kc == 7, [w, hT], [pu])
                    s_ = sg[fc % 2]
                    self.act(s_.t[:], pg.t[:, 0:TT], AF.Silu, [pg], [s_])
                    self.tt("dve", actb.t[:, fc, :], s_.t[:], pu.t[:, 0:TT], ALU.mult, [s_, pu], [actb])
                if i + 1 < NT:
                    outproj(i + 1)
                for dc in range(8):
                    if dc == 4 and i + 1 < NT:
                        norm_rest(i + 1)
                    w = wdn[dc]
                    p = bank()
                    for fc in range(NFC):
                        self.mm(p.t[:, 0:TT], w.t[:, fc, :], actb.t[:, fc, :], fc == 0, fc == NFC - 1, [w, actb], [p])
                    self.tt("dve", x.t[:, dc, :], x.t[:, dc, :], p.t[:, 0:TT], ALU.add, [x, p], [x])
                if not last:
                    self.dma("pool", xs_v[:, :, t0:t0 + TT], x.t[:], [x], [self.b_x])
                else:
                    p = bank()
                    self.rmsnorm(x, x, 0, TT, self.vx.t[:, 8:16], sq2, p, rstd2, 1e-6)
                    self.dma("pool", y_v[:, :, t0:t0 + TT], x.t[:], [x], [])
                if i + 2 < NT:
                    load_tile(i + 2)
            self.emit_phase(final=last)

    def phase_qkv(self, l, x_src):
        TT = 512
        NT = self.T // TT
        sqD = math.sqrt(D)
        with ExitStack() as es:
            xt = [self.sb(es, [128, 8, TT], F32, "xt") for _ in range(2)]
            hT = self.sb(es, [128, 8, TT], BF16, "hT")
            sq = self.sb(es, [128, 8, TT], BF16, "sq")
            rstd = self.sb(es, [128, TT], F32, "rstd")
            wa = [self.sb(es, [128, 8, 128], BF16, "wa") for _ in range(3)]
            wbs = [self.sb(es, [128, 8, 512], BF16, "wbs") for _ in range(2)]
            qst = [self.sb(es, [128, 8, TT], BF16, "qst") for _ in range(2)]
            kst = [self.sb(es, [128, 8, TT], BF16, "kst") for _ in range(2)]
            vst = [self.sb(es, [128, 4, D], BF16, "vst") for _ in range(2)]
            pb = [self.ps(es, [128, 512], F32, "pb") for _ in range(8)]
            pbi = [0]

            def bank():
                b = pb[pbi[0] % 8]
                pbi[0] += 1
                return b

            c_n, _ = VEC_COLS[f"nmix{l}"]
            self.ts("dve", self.vx.t[:, 16:24], self.vecs.t[:, c_n:c_n + 8], sqD, None, ALU.mult, None, [self.vecs], [self.vx])
            xsrc_v = x_src.rearrange("(kc p) t -> p kc t", p=128)
            q_v = self.q_s.rearrange("c p t -> p c t")
            k_v = self.k_s.rearrange("c p t -> p c t")
            v_v = self.v_s.rearrange("(tb p) f -> p tb f", p=128)
            wi = [0, 0]
            self.dma("sp", xt[0].t[:], xsrc_v[:, :, 0:TT], [self.b_x], [xt[0]])
            for i in range(NT):
                t0 = i * TT
                x = xt[i % 2]
                if i + 1 < NT:
                    self.dma("sp", xt[(i + 1) % 2].t[:], xsrc_v[:, :, t0 + TT:t0 + 2 * TT], [self.b_x], [xt[(i + 1) % 2]])
                p = bank()
                self.rmsnorm(x, hT, 0, TT, self.vx.t[:, 16:24], sq, p, rstd, 1e-6)
                qs = qst[i % 2]
                ks = kst[i % 2]
                vs = vst[i % 2]
                for c in range(16):
                    w = wa[wi[0] % 3]
                    wi[0] += 1
                    self.dma("sp", w.t[:], self.wb["w_inA"][l, c], [self.b_wl[l]], [w])
                    p = bank()
                    for kc in range(8):
                        self.mm(p.t[:, 0:TT], w.t[:, kc, :], hT.t[:, kc, :], kc == 0, kc == 7, [w, hT], [p])
                    if c < 8:
                        self.act(qs.t[:, c, :], p.t[:, 0:TT], AF.Copy, [p], [qs], scale=0.125)
                    else:
                        self.cp("dve", ks.t[:, c - 8, :], p.t[:, 0:TT], [p], [ks])
                for cg in range(2):
                    w = wbs[wi[1] % 2]
                    wi[1] += 1
                    self.dma("sp", w.t[:], self.wb["w_inB"][l, 4 + cg], [self.b_wl[l]], [w])
                    for tb in range(4):
                        p = bank()
                        for kc in range(8):
                            self.mm(p.t[:, :], hT.t[:, kc, tb * 128:(tb + 1) * 128], w.t[:, kc, :], kc == 0, kc == 7, [w, hT], [p])
                        if tb % 2 == 0:
                            self.cp("act", vs.t[:, tb, cg * 512:(cg + 1) * 512], p.t[:, :], [p], [vs])
                        else:
                            self.cp("dve", vs.t[:, tb, cg * 512:(cg + 1) * 512], p.t[:, :], [p], [vs])
                self.dma("pool", q_v[:, :, t0:t0 + TT], qs.t[:], [qs], [self.b_qkv])
                self.dma("pool", k_v[:, :, t0:t0 + TT], ks.t[:], [ks], [self.b_qkv])
                self.dma("pool", v_v[:, 4 * i:4 * i + 4, :], vs.t[:], [vs], [self.b_qkv])
            self.emit_phase()

    def phase_attn(self, l, next_layer=None):
        i_odd = l // 2
        lam_init = 0.8 - 0.6 * math.exp(-0.3 * l)
        Tn = self.T
        QB = 512
        NQ = Tn // QB
        NKB = Tn // 128
        with ExitStack() as es:
            qT = [self.sb(es, [128, Tn], BF16, "qT") for _ in range(2)]
            kT = [self.sb(es, [128, Tn], BF16, "kT") for _ in range(2)]
            vv = [self.sb(es, [128, NKB, 128], BF16, "vv") for _ in range(2)]
            Gf = self.sb(es, [128, 1024], F32, "Gf")
            Gb = self.sb(es, [128, 8, 1024], BF16, "Gb")
            pt = [[self.sb(es, [128, QB], BF16, "pt") for _ in range(3)] for _ in range(2)]
            pacc = [self.sb(es, [128, QB], F32, "pacc") for _ in range(2)]
            tmp = [self.sb(es, [128, QB], F32, "tmp") for _ in range(5)]
            sqb = self.sb(es, [128, QB], BF16, "sqb")
            ost = [self.sb(es, [128, QB], BF16, "ost") for _ in range(2)]
            lamt = self.sb(es, [128, 8], F32, "lamt")
            stp = [self.ps(es, [128, 512], F32, "st") for _ in range(5)]
            lt0 = self.ps(es, [128, 512], F32, "lt0")
            ot = [self.ps(es, [128, 512], F32, "ot") for _ in range(2)]
            sti = [0]
            st = {}

            if next_layer is not None:
                self.cast_layer(next_layer)
            for h in range(8):
                self.dma("sp", Gf.t[:], self.biasG_d[:, h, :], [], [Gf])
                self.cp("dve", Gb.t[:, h, :], Gf.t[:], [Gf], [Gb])
            c_l, _ = VEC_COLS[f"lam{i_odd}"]
            lv = self.vecs.t
            self.tt("dve", lamt.t[:, 0:1], lv[:, c_l:c_l + 1], lv[:, c_l + 1:c_l + 2], ALU.mult, [self.vecs], [lamt])
            self.tt("dve", lamt.t[:, 1:2], lv[:, c_l + 2:c_l + 3], lv[:, c_l + 3:c_l + 4], ALU.mult, [self.vecs], [lamt])
            self.memset("pool", tmp[0].t[:, 0:128], 1.0, [tmp[0]])
            self.mm(stp[0].t[:, 0:2], tmp[0].t[:, 0:128], lamt.t[:, 0:2], True, True, [tmp[0], lamt], [stp[0]])
            self.act(lamt.t[:, 2:4], stp[0].t[:, 0:2], AF.Exp, [stp[0]], [lamt])
            onesf = self.sb(es, [128, 128], F32, "onesf")
            self.memset("pool", onesf.t[:], 1.0, [onesf])
            self.tt("dve", lamt.t[:, 4:5], lamt.t[:, 3:4], lamt.t[:, 2:3], ALU.subtract, [lamt], [lamt])
            self.ts("dve", lamt.t[:, 4:5], lamt.t[:, 4:5], -lam_init, None, ALU.add, None, [lamt], [lamt])
            c_s, _ = VEC_COLS[f"subln{i_odd}"]
            self.ts("dve", lamt.t[:, 5:6], lv[:, c_s:c_s + 1], (1.0 - lam_init) * math.sqrt(128.0), None, ALU.mult, None, [self.vecs], [lamt])
            c_bf, _ = VEC_COLS["bfar"]

            v_v = self.v_s.rearrange("(kb p) f -> p kb f", p=128)

            def load_head(h):
                self.dma("sp", qT[h % 2].t[:], self.q_s[h], [self.b_qkv], [qT[h % 2]])
                self.dma("sp", kT[h % 2].t[:], self.k_s[h], [self.b_qkv], [kT[h % 2]])
                self.dma("sp", vv[h % 2].t[:], v_v[:, :, h * 128:(h + 1) * 128], [self.b_qkv], [vv[h % 2]])

            load_head(0)
            ep = 0
            for h in range(8):
                if h + 1 < 8:
                    load_head(h + 1)
                q = qT[h % 2]
                k = kT[h % 2]
                v = vv[h % 2]
                bfar = lv[:, c_bf + h:c_bf + h + 1]
                for qb in range(NQ):
                    q0 = qb * QB
                    nkb = 4 * qb + 4
                    qsl = slice(q0, q0 + QB)

                    def issue_st(kb, m):
                        k0 = kb * 128
                        delta = k0 - q0
                        special = delta >= -128
                        s_ = stp[sti[0] % 5]
                        sti[0] += 1
                        st[(kb, m)] = s_
                        self.mm(s_.t[:, :], k.t[64 * m:64 * m + 64, k0:k0 + 128], q.t[64 * m:64 * m + 64, qsl],
                                True, not special, [k, q], [s_])
                        if special:
                            j0 = 384 - delta
                            self.mm(s_.t[:, :], self.ident.t[:], Gb.t[:, h, j0:j0 + 512], False, True, [self.ident, Gb], [s_])
                        return special

                    spec = {}
                    for kb in range(min(2, nkb)):
                        for m in range(2):
                            spec[kb] = issue_st(kb, m)
                    for kb in range(nkb):
                        for m in range(2):
                            s_ = st.pop((kb, m))
                            p_ = pt[m][kb % 3]
                            if spec[kb]:
                                self.act(p_.t[:], s_.t[:, :], AF.Exp, [s_], [p_])
                            else:
                                self.act(p_.t[:], s_.t[:, :], AF.Exp, [s_, self.vecs], [p_], bias=bfar)
                            if kb + 2 < nkb:
                                spec[kb + 2] = issue_st(kb + 2, m)
                            if m == 1:
                                if kb == 0:
                                    self.cp("dve", pacc[m].t[:], p_.t[:], [p_], [pacc[m]])
                                else:
                                    self.tt("dve", pacc[m].t[:], pacc[m].t[:], p_.t[:], ALU.add, [p_, pacc[m]], [pacc[m]])
                            self.mm(ot[m].t[:, :], v.t[:, kb, :], p_.t[:], kb == 0, kb == nkb - 1, [v, p_], [ot[m]])
                            if m == 0:
                                self.mm(lt0.t[:, :], self.ones.t[:], p_.t[:], kb == 0, kb == nkb - 1, [self.ones, p_], [lt0])
                    l_ = stp[sti[0] % 5]
                    sti[0] += 1
                    self.mm(l_.t[:, :], onesf.t[:], pacc[1].t[:], True, True, [onesf, pacc[1]], [l_])
                    lt = [lt0, l_]
                    r0, r1, o0, o1, oo = tmp
                    self.recip(r0.t[:], lt[0].t[:, :], [lt[0]], [r0])
                    self.recip(r1.t[:], lt[1].t[:, :], [lt[1]], [r1])
                    self.tt("dve", o0.t[:], ot[0].t[:, :], r0.t[:], ALU.mult, [ot[0], r0], [o0])
                    self.tt("dve", o1.t[:], ot[1].t[:, :], r1.t[:], ALU.mult, [ot[1], r1], [o1])
                    self.stt("pool", oo.t[:], o1.t[:], lamt.t[:, 4:5], o0.t[:], ALU.mult, ALU.add, [o1, o0, lamt], [oo])
                    self.act(sqb.t[:], oo.t[:], AF.Square, [oo], [sqb])
                    pss = stp[sti[0] % 5]
                    sti[0] += 1
                    self.mm(pss.t[:, :], self.ones.t[:], sqb.t[:], True, True, [self.ones, sqb], [pss])
                    self.act(r0.t[:], pss.t[:, :], AF.Sqrt, [pss], [r0], bias=float(128 * 1e-5), scale=1.0)
                    self.recip(r0.t[:], r0.t[:], [r0], [r0])
                    os_ = ost[ep % 2]
                    ep += 1
                    self.stt("pool", os_.t[:], oo.t[:], lamt.t[:, 5:6], r0.t[:], ALU.mult, ALU.mult, [oo, lamt, r0], [os_])
                    self.dma("pool", self.mix_s[h * 128:(h + 1) * 128, q0:q0 + QB], os_.t[:], [os_], [self.b_mix])
            self.emit_phase()

    def build(self):
        self.setup()
        x_src = self.xT
        n = len(self.layers)
        for j, l in enumerate(self.layers):
            last = j == n - 1
            nxt = None if last else self.layers[j + 1]
            if l % 2 == 0:
                self.phase_even(l, x_src, nxt)
            else:
                self.phase_qkv(l, x_src)
                self.phase_attn(l, nxt)
            if self.dbg and j == 0:
                self.dma("sp", self.dbg_mix, self.mix_s, [self.b_mix], [])
            self.phase_ffn(l, x_src, last, nxt)
            x_src = self.x_s
        return self.nc


_CACHE = {}


def run(inputs, layers=(0, 1, 2, 3), n_cores=8, dbg=False, trace=False):
    Tn = inputs["x"].shape[1]
    key = (tuple(layers), Tn, dbg)
    if key not in _CACHE:
        kb = Kern(list(layers), Tn, dbg)
        _CACHE[key] = kb.build()
    nc = _CACHE[key]
    sh = pack_shared(inputs)
    in_maps = []
    for b in range(n_cores):
        m = dict(sh)
        m["xT"] = np.ascontiguousarray(inputs["x"][b].T)
        in_maps.append(m)
    res = run_bass_kernel_spmd(nc, in_maps, core_ids=list(range(n_cores)), trace=trace)
    return res


def kernel(**inputs):
    inputs = {k: np.asarray(v) for k, v in inputs.items()}
    res = run(inputs)
    out = np.stack([np.ascontiguousarray(r["yT"].T) for r in res.results], axis=0)
    return out.astype(np.float32)


CDEC = math.exp(-0.5)


def _even_consts(self, es):
    c = {}

    def mask(name, kind, val, dt=F32):
        t = self.sb(es, [128, 128], dt, name)
        self.memset("pool", t.t[:], val, [t])
        if kind == "u_incl":
            pat, cm, op = [[1, 128]], -1, ALU.is_ge
            z = (slice(0, 64), slice(64, 128))
        elif kind == "u_strict":
            pat, cm, op = [[1, 128]], -1, ALU.is_gt
            z = (slice(0, 64), slice(64, 128))
        else:
            pat, cm, op = [[-1, 128]], 1, ALU.is_gt
            z = (slice(64, 128), slice(0, 64))
        self.P.op("pool", lambda e: e.affine_select(out=t.t[:], in_=t.t[:], pattern=pat, compare_op=op, fill=0.0,
                                                    base=0, channel_multiplier=cm), [t.b], [t.b])
        self.memset("pool", t.t[z[0], z[1]], 0.0, [t])
        c[name] = t
        return t

    mask("MleF", "u_incl", -CDEC)
    mask("MltF", "u_strict", -CDEC)
    mask("MgtF", "l_strict", -CDEC)
    mask("MleG", "u_incl", 1.0 / 16.0)
    mask("MgtG", "l_strict", 1.0 / 16.0)
    ui = mask("UI", "u_incl", 1.0, BF16)
    us = mask("US", "u_strict", 1.0, BF16)
    ls = mask("LS", "l_strict", 1.0, BF16)
    for nm, src in (("UI4", ui), ("US4", us), ("LS4", ls)):
        t = self.sb(es, [128, 4, 128], BF16, nm)
        for r in range(4):
            self.cp("pool", t.t[:, r, :], src.t[:], [src], [t])
        c[nm] = t
    t = self.sb(es, [128, 8, 128], BF16, "identrep")
    for r in range(8):
        self.cp("pool", t.t[:, r, :], self.ident.t[:], [self.ident], [t])
    c["identrep"] = t
    ob = self.sb(es, [128, 128], BF16, "onesblk")
    self.memset("pool", ob.t[:], 1.0, [ob])
    self.memset("pool", ob.t[0:64, 64:128], 0.0, [ob])
    self.memset("pool", ob.t[64:128, 0:64], 0.0, [ob])
    c["onesblk"] = ob
    return c


Kern._even_consts = _even_consts


def phase_even(self, l, x_src, next_layer=None):
    i_ev = l // 2
    TT = 256
    NT = self.T // TT
    sqD = math.sqrt(D)
    V = lambda name, j=0, n=1: self.vcol(f"{name}{i_ev}", j, n)
    with ExitStack() as es0:
        C = self._even_consts(es0)
        L1c = self.sb(es0, [128, 8, 304], BF16, "L1c")
        L1p = self.sb(es0, [128, 8, 304], BF16, "L1p")
        with ExitStack() as es:
            L1f = self.sb(es, [128, 8, 304], F32, "L1f")
            L1pf = self.sb(es, [128, 8, 304], F32, "L1pf")
            self.dma("sp", L1f.t[:], self.lora1_d[i_ev], [], [L1f])
            self.memset("pool", L1pf.t[:, :, 288:304], 0.0, [L1pf])
            for kc in range(8):
                for (c0, c1, mj) in ((0, 64, 0), (64, 128, 8), (128, 288, 16)):
                    eng = "dve" if kc % 2 == 0 else "pool"
                    self.ts(eng, L1pf.t[:, kc, c0:c1], L1f.t[:, kc, c0:c1], V("mu_wag", mj + kc), None, ALU.mult, None,
                            [L1f, self.vecs], [L1pf])
            self.cp("act", L1p.t[:], L1pf.t[:], [L1pf], [L1p])
            self.tt("dve", L1c.t[:], L1f.t[:], L1pf.t[:], ALU.subtract, [L1f, L1pf], [L1c])
            c_n, _ = VEC_COLS[f"nmix{l}"]
            self.ts("dve", self.vx.t[:, 16:24], self.vecs.t[:, c_n:c_n + 8], sqD, None, ALU.mult, None, [self.vecs], [self.vx])
            self.ts("dve", self.vx.t[:, 24:36], V("mu_rkv", 0, 12), -1.0, 1.0, ALU.mult, ALU.add, [self.vecs], [self.vx])
            self.ts("dve", self.vx.t[:, 36:40], V("k_a", 0, 4), -1.0, 1.0, ALU.mult, ALU.add, [self.vecs], [self.vx])
            self.emit_phase()
        with ExitStack() as es:
            sb = lambda shape, dt, nm: self.sb(es, shape, dt, nm)
            xt = sb([128, 8, TT], F32, "xt")
            hT = sb([128, 8, TT + 2], BF16, "hT")
            sq = sb([128, 8, TT], BF16, "sq")
            rstd = sb([128, TT], F32, "rstd")
            wA = [sb([128, 8, 128], BF16, "wA") for _ in range(3)]
            wB3 = sb([128, 8, 256], BF16, "wB3")
            wB4 = sb([128, 8, 512], BF16, "wB4")
            L2f = sb([128, 5, 512], F32, "L2f")
            L2 = sb([128, 5, 512], BF16, "L2")
            rows = sb([128, 768], F32, "rows")
            pm = sb([128, 12, TT + 2], BF16, "pm")
            midA = sb([128, TT], BF16, "midA")
            midB = sb([128, TT], BF16, "midB")
            midC = sb([128, TT], BF16, "midC")
            sgw = sb([128, 2, 512], F32, "sgw")
            la = sb([128, 2, 256], F32, "la")
            ztmp = sb([128, 512], F32, "ztmp")
            g_ag = sb([128, 4, TT], F32, "g_ag")
            g_kk = sb([128, 4, TT], F32, "g_kk")
            g_rn = sb([128, 4, TT], F32, "g_rn")
            g_r = sb([128, 4, TT], BF16, "g_r")
            g_k = sb([128, 4, TT], BF16, "g_k")
            g_sq = sb([128, 4, TT], BF16, "g_sq")
            g_en = sb([128, 4, TT], BF16, "g_en")
            g_ex = sb([128, 4, TT], BF16, "g_ex")
            ecum = sb([128, 4, TT], F32, "ecum")
            bb = sb([128, 4, TT], BF16, "bb")
            kp = sb([128, 4, TT], BF16, "kp")
            vv = sb([128, 4, TT], BF16, "vv")
            rt = sb([128, 4, TT], BF16, "rt")
            at_ = sb([128, 4, TT], BF16, "at")
            bt = sb([128, 4, TT], BF16, "bt")
            kt = sb([128, 4, TT], BF16, "kt")
            Bh = sb([128, 2, 512], BF16, "Bh")
            Kh = sb([128, 2, 512], BF16, "Kh")
            Vt = sb([128, 2, 512], BF16, "Vt")
            etoend = sb([128, 2, 512], BF16, "etoend")
            bonus = sb([128, 4, TT], F32, "bonus")
            AabT = sb([128, 8, 128], BF16, "AabT")
            Aab = sb([128, 8, 128], BF16, "Aab")
            AakT = sb([128, 8, 128], BF16, "AakT")
            ArbT = sb([128, 8, 128], BF16, "ArbT")
            ArkT = sb([128, 8, 128], BF16, "ArkT")
            Pn = [sb([128, 8, 128], BF16, "Pn") for _ in range(2)]
            PTn = [sb([128, 8, 128], BF16, "PTn") for _ in range(2)]
            TTn = [sb([128, 8, 128], BF16, "TTn") for _ in range(2)]
            gq = sb([128, 2, TT], BF16, "gq")
            gk = sb([128, 2, TT], BF16, "gk")
            gKh = sb([128, 2, 256], BF16, "gKh")
            gV = sb([128, 2, 512], BF16, "gV")
            gsil = sb([128, 4, TT], BF16, "gsil")
            gecum = sb([128, 2, TT], F32, "gecum")
            gencum = sb([128, 2, TT], F32, "gencum")
            getoend = sb([128, 2, 256], F32, "getoend")
            gST = sb([128, 4, 128], BF16, "gST")
            yT = sb([128, 4, TT], F32, "yT")
            og = sb([128, 4, TT], F32, "og")
            mixo = sb([128, 8, TT], BF16, "mixo")
            Hf = sb([128, 4, 128], F32, "Hf")
            Hb = sb([128, 4, 128], BF16, "Hb")
            Xs = sb([128, 512], BF16, "Xs")
            Us = sb([128, 512], BF16, "Us")
            Sf = sb([128, 2, 256], F32, "Sf")
            Sb = sb([128, 2, 256], BF16, "Sb")
            pb = [self.ps(es, [128, 512], F32, "pb") for _ in range(6)]
            pbf = [self.ps(es, [128, 512], BF16, "pbf") for _ in range(2)]
            pbi = [0, 0]
            tfi = [0, 0]
            if getattr(self, "dbg_mem", False):
                try:
                    print("EVEN sbuf remaining:", self.nc.sbuf_bytes_remaining)
                except Exception as ex:
                    print("sbuf query failed", ex)

            def bank():
                b = pb[pbi[0] % 6]
                pbi[0] += 1
                return b

            def bankbf():
                b = pbf[pbi[1] % 2]
                pbi[1] += 1
                return b

            def TF():
                t = tf[tfi[0] % 8]
                tfi[0] += 1
                return t

            def TB():
                t = tb[tfi[1] % 8]
                tfi[1] += 1
                return t

            if next_layer is not None:
                self.cast_layer(next_layer)
            self.dma("sp", L2f.t[:], self.lora2_d[i_ev], [], [L2f])
            self.cp("act", L2.t[:], L2f.t[:], [L2f], [L2])
            self.dma("sp", rows.t[:], self.rows_d[i_ev].partition_broadcast(128), [], [rows])
            self.dma("sp", wB3.t[:], self.wb["w_inB"][l, 3, :, :, 256:512], [self.b_wl[l]], [wB3])
            self.dma("sp", wB4.t[:], self.wb["w_inB"][l, 4], [self.b_wl[l]], [wB4])
            self.memset("pool", hT.t[:, :, 0:2], 0.0, [hT])
            self.memset("pool", pm.t[:, :, 0:2], 0.0, [pm])
            for z in (Hf, Hb, Xs, Us, Sf, Sb):
                self.memset("pool", z.t[:], 0.0, [z])
            xsrc_v = x_src.rearrange("(kc p) t -> p kc t", p=128)
            mix_v = self.mix_s.rearrange("(kc p) t -> p kc t", p=128)
            wi = [0]
            self.dma("sp", xt.t[:], xsrc_v[:, :, 0:TT], [self.b_x], [xt])

            def slabA(c):
                w = wA[wi[0] % 3]
                wi[0] += 1
                self.dma("sp", w.t[:], self.wb["w_inA"][l, c], [self.b_wl[l]], [w])
                return w

            def proj_fm(c):
                w = slabA(c)
                p = bank()
                for kc in range(8):
                    self.mm(p.t[:, 0:TT], w.t[:, kc, :], hT.t[:, kc, 2:2 + TT], kc == 0, kc == 7, [w, hT], [p])
                return p

            stop = getattr(self, "even_stop", None)

            class _Stop(Exception):
                pass

            def chk(st):
                if stop == st:
                    raise _Stop()

            for i in range(NT):
              try:
                t0 = i * TT
                if i > 0:
                    self.cp("dve", hT.t[:, :, 1:2], hT.t[:, :, TT + 1:TT + 2], [hT], [hT])
                    self.cp("dve", pm.t[:, :, 1:2], pm.t[:, :, TT + 1:TT + 2], [pm], [pm])
                p = bank()
                self.rmsnorm(xt, hT, 2, TT, self.vx.t[:, 16:24], sq, p, rstd, 1e-6)
                if i + 1 < NT:
                    self.dma("sp", xt.t[:], xsrc_v[:, :, t0 + TT:t0 + 2 * TT], [self.b_x], [xt])
                chk("B")
                for (mid, c0, c1) in ((midA, 0, 128), (midB, 128, 256), (midC, 256, 304)):
                    m = c1 - c0
                    p = bank()
                    for kc in range(8):
                        self.mm(p.t[0:m, 0:TT], L1c.t[:, kc, c0:c1], hT.t[:, kc, 2:2 + TT], kc == 0, False, [L1c, hT], [p])
                    for kc in range(8):
                        self.mm(p.t[0:m, 0:TT], L1p.t[:, kc, c0:c1], hT.t[:, kc, 1:1 + TT], False, kc == 7, [L1p, hT], [p])
                    if mid is midA:
                        self.act(mid.t[0:64, :], p.t[0:64, 0:TT], AF.Tanh, [p], [mid])
                        self.cp("dve", mid.t[64:128, :], p.t[64:128, 0:TT], [p], [mid])
                    elif mid is midB:
                        self.act(mid.t[:, :], p.t[:, 0:TT], AF.Sigmoid, [p], [mid])
                    else:
                        self.act(mid.t[0:32, :], p.t[0:32, 0:TT], AF.Sigmoid, [p], [mid])
                        self.cp("dve", mid.t[32:48, :], p.t[32:48, 0:TT], [p], [mid])
                chk("C")
                for b in range(2):
                    bs = slice(b * 128, (b + 1) * 128)
                    p = bank()
                    self.mm(p.t[:, :], midA.t[0:64, bs], L2.t[0:64, 0, :], True, True, [midA, L2], [p])
                    self.tt("dve", ztmp.t[:], p.t[:, :], rows.t[:, 0:512], ALU.add, [p, rows], [ztmp])
                    self.act(sgw.t[:, b, :], ztmp.t[:], AF.Sigmoid, [ztmp], [sgw])
                    p = bank()
                    self.mm(p.t[:, 0:256], midC.t[32:48, bs], L2.t[32:48, 4, 0:256], True, True, [midC, L2], [p])
                    self.tt("dve", ztmp.t[:, 0:256], p.t[:, 0:256], rows.t[:, 512:768], ALU.add, [p, rows], [ztmp])
                    self.act(ztmp.t[:, 256:512], ztmp.t[:, 0:256], AF.Sigmoid, [ztmp], [ztmp])
                    self.act(la.t[:, b, :], ztmp.t[:, 256:512], AF.Ln, [ztmp], [la])
                chk("D")
                for b in range(2):
                    p = bank()
                    self.mm(p.t[:, :], C["MgtF"].t[:], sgw.t[:, b, :], True, True, [C["MgtF"], sgw], [p])
                    self.act(etoend.t[:, b, :], p.t[:, :], AF.Exp, [p], [etoend])
                    p = bank()
                    self.mm(p.t[:, 0:256], C["MgtG"].t[:], la.t[:, b, :], True, True, [C["MgtG"], la], [p])
                    self.act(getoend.t[:, b, :], p.t[:, 0:256], AF.Exp, [p], [getoend])
                for fg in range(2):
                    p = bank()
                    for b in range(2):
                        self.mm(p.t[:, b * 128:(b + 1) * 128], la.t[:, b, fg * 128:(fg + 1) * 128], C["MleG"].t[:], True, True,
                                [la, C["MleG"]], [p])
                    self.act(gecum.t[:, fg, :], p.t[:, 0:TT], AF.Exp, [p], [gecum])
                    self.act(gencum.t[:, fg, :], p.t[:, 0:TT], AF.Exp, [p], [gencum], scale=-1.0)
                chk("E")
                def stage_F1():
                    for fg in range(2):
                        p = proj_fm(12 + fg)
                        self.stt("dve", gq.t[:, fg, :], p.t[:, 0:TT], 0.125, gecum.t[:, fg, :], ALU.mult, ALU.mult, [p, gecum], [gq])
                        p = proj_fm(14 + fg)
                        self.tt("dve", gk.t[:, fg, :], p.t[:, 0:TT], gencum.t[:, fg, :], ALU.mult, [p, gencum], [gk])

                def stage_F2():
                    for fc in range(4):
                        p = proj_fm(20 + fc)
                        self.act(gsil.t[:, fc, :], p.t[:, 0:TT], AF.Silu, [p], [gsil])
                    for b in range(2):
                        p = bank()
                        for kc in range(8):
                            self.mm(p.t[:, 0:256], hT.t[:, kc, 2 + b * 128:2 + (b + 1) * 128], wB3.t[:, kc, :], kc == 0, kc == 7, [hT, wB3], [p])
                        self.tt("dve", gKh.t[:, b, :], p.t[:, 0:256], getoend.t[:, b, :], ALU.mult, [p, getoend], [gKh])
                        p = bank()
                        for kc in range(8):
                            self.mm(p.t[:, :], hT.t[:, kc, 2 + b * 128:2 + (b + 1) * 128], wB4.t[:, kc, :], kc == 0, kc == 7, [hT, wB4], [p])
                        self.cp("act", gV.t[:, b, :], p.t[:, :], [p], [gV])
                chk("F")
                FCS = range(4)
                for q3 in range(3):
                    for fc in FCS:
                        c = 4 * q3 + fc
                        p = proj_fm(c)
                        self.ts("dve", pm.t[:, c, 2:2 + TT], p.t[:, 0:TT], V("mu_rkv", c), None, ALU.mult, None, [p, self.vecs], [pm])
                        dst = (g_r, g_k, vv)[q3]
                        self.stt("dve", dst.t[:, fc, :], p.t[:, 0:TT], self.vx.t[:, 24 + c:25 + c], pm.t[:, c, 1:1 + TT], ALU.mult, ALU.add,
                                 [p, self.vx, pm], [dst])
                chk("G1")
                for fc in FCS:
                    p = bank()
                    self.mm(p.t[:, 0:TT], L2.t[64:128, 1, fc * 128:(fc + 1) * 128], midA.t[64:128, :], True, True, [L2, midA], [p])
                    self.act(g_ag.t[:, fc, :], p.t[:, 0:TT], AF.Sigmoid, [p, self.vecs], [g_ag], bias=V("a0", fc))
                for fc in FCS:
                    pc = bank()
                    for b in range(2):
                        self.mm(pc.t[:, b * 128:(b + 1) * 128], sgw.t[:, b, fc * 128:(fc + 1) * 128], C["MleF"].t[:], True, True,
                                [sgw, C["MleF"]], [pc])
                        self.mm(pc.t[:, 256 + b * 128:256 + (b + 1) * 128], sgw.t[:, b, fc * 128:(fc + 1) * 128], C["MltF"].t[:], True, True,
                                [sgw, C["MltF"]], [pc])
                    self.act(ecum.t[:, fc, :], pc.t[:, 0:TT], AF.Exp, [pc], [ecum])
                    self.act(g_en.t[:, fc, :], pc.t[:, 0:TT], AF.Exp, [pc], [g_en], scale=-1.0)
                    self.act(g_ex.t[:, fc, :], pc.t[:, 256:256 + TT], AF.Exp, [pc], [g_ex])
                for fc in FCS:
                    self.ts("pool", g_kk.t[:, fc, :], g_k.t[:, fc, :], V("k_k", fc), None, ALU.mult, None, [g_k, self.vecs], [g_kk])
                for fc in FCS:
                    self.act(g_sq.t[:, fc, :], g_kk.t[:, fc, :], AF.Square, [g_kk], [g_sq])
                pn = []
                for fc in FCS:
                    p = bank()
                    self.mm(p.t[:, 0:TT], C["onesblk"].t[:], g_sq.t[:, fc, :], True, True, [C["onesblk"], g_sq], [p])
                    pn.append(p)
                for fc in FCS:
                    self.act(g_rn.t[:, fc, :], pn[fc].t[:, 0:TT], AF.Sqrt, [pn[fc]], [g_rn], bias=1e-24, scale=1.0)
                stage_F1()
                self.recip(g_rn.t[:], g_rn.t[:], [g_rn], [g_rn])
                self.tt("dve", g_kk.t[:], g_kk.t[:], g_rn.t[:], ALU.mult, [g_kk, g_rn], [g_kk])
                for fc in FCS:
                    self.ts("pool", g_rn.t[:, fc, :], g_ag.t[:, fc, :], V("k_a", fc), self.vx.t[:, 36 + fc:37 + fc], ALU.mult, ALU.add,
                            [g_ag, self.vecs, self.vx], [g_rn])
                self.tt("pool", kp.t[:], g_k.t[:], g_rn.t[:], ALU.mult, [g_k, g_rn], [kp])
                self.tt("pool", bb.t[:], g_kk.t[:], g_ag.t[:], ALU.mult, [g_kk, g_ag], [bb])
                self.tt("dve", rt.t[:], g_r.t[:], ecum.t[:], ALU.mult, [g_r, ecum], [rt])
                self.stt("dve", at_.t[:], g_kk.t[:], -1.0, g_ex.t[:], ALU.mult, ALU.mult, [g_kk, g_ex], [at_])
                self.tt("pool", bt.t[:], bb.t[:], g_en.t[:], ALU.mult, [bb, g_en], [bt])
                self.tt("pool", kt.t[:], kp.t[:], g_en.t[:], ALU.mult, [kp, g_en], [kt])
                for fc in FCS:
                    self.stt("dve", g_sq.t[:, fc, :], g_r.t[:, fc, :], V("r_k", fc), kp.t[:, fc, :], ALU.mult, ALU.mult, [g_r, kp, self.vecs], [g_sq])
                stage_F2()
                pn = []
                for fc in FCS:
                    p = bank()
                    self.mm(p.t[:, 0:TT], C["onesblk"].t[:], g_sq.t[:, fc, :], True, True, [C["onesblk"], g_sq], [p])
                    pn.append(p)
                for fc in FCS:
                    self.tt("dve", bonus.t[:, fc, :], pn[fc].t[:, 0:TT], vv.t[:, fc, :], ALU.mult, [pn[fc], vv], [bonus])
                chk("G")
                for b in range(2):
                    bs = slice(b * 128, (b + 1) * 128)
                    for (src, dst, useE) in ((bb, Bh, True), (kp, Kh, True), (vv, Vt, False)):
                        p = bankbf()
                        for fc in range(4):
                            self.tr(p.t[:, fc * 128:(fc + 1) * 128], src.t[:, fc, bs], self.ident.t[:], [src, self.ident], [p])
                        if useE:
                            self.tt("dve", dst.t[:, b, :], p.t[:, :], etoend.t[:, b, :], ALU.mult, [p, etoend], [dst])
                        else:
                            self.cp("act", dst.t[:, b, :], p.t[:, :], [p], [dst])
                    chk("H")
                    for (dstS, lh, rh, msk) in ((AabT, bt, at_, "US4"), (Aab, at_, bt, "LS4"), (AakT, kt, at_, "US4"),
                                                (ArbT, bt, rt, "UI4"), (ArkT, kt, rt, "UI4")):
                        pg2 = [bank(), bank()]
                        for fc in range(4):
                            for g in range(2):
                                ro = 64 * g
                                self.mm(pg2[g].t[:, fc * 128:(fc + 1) * 128], lh.t[ro:ro + 64, fc, bs], rh.t[ro:ro + 64, fc, bs], True, True,
                                        [lh, rh], [pg2[g]])
                        for g in range(2):
                            self.tt("dve", dstS.t[:, g:8:2, :], pg2[g].t[:, :].rearrange("p (a b) -> p a b", a=4), C[msk].t[:], ALU.mult,
                                    [pg2[g], C[msk]], [dstS])
                    pg2 = [bank(), bank()]
                    for fg in range(2):
                        for g in range(2):
                            ro = 64 * g
                            self.mm(pg2[g].t[:, fg * 128:(fg + 1) * 128], gk.t[ro:ro + 64, fg, bs], gq.t[ro:ro + 64, fg, bs], True, True,
                                    [gk, gq], [pg2[g]])
                    for g in range(2):
                        self.tt("dve", gST.t[:, g:4:2, :], pg2[g].t[:, 0:256].rearrange("p (a b) -> p a b", a=2), C["UI4"].t[:, 0:2, :], ALU.mult,
                                [pg2[g], C["UI4"]], [gST])
                    chk("S")
                    Pc, PTc = Aab, AabT
                    TTc = TTn[0]
                    self.tt("pool", TTc.t[:], AabT.t[:], C["identrep"].t[:], ALU.add, [AabT, C["identrep"]], [TTc])
                    for it in range(5):
                        Pnew = Pn[it % 2]
                        PTnew = PTn[it % 2]
                        TTnew = TTn[(it + 1) % 2]
                        for g in range(2):
                            p = bank()
                            for hh in range(4):
                                h = 4 * g + hh
                                self.mm(p.t[:, hh * 128:(hh + 1) * 128], PTc.t[:, h, :], Pc.t[:, h, :], True, True, [PTc, Pc], [p])
                            self.cp("act", Pnew.t[:, 4 * g:4 * g + 4, :], p.t[:, :].rearrange("p (a b) -> p a b", a=4), [p], [Pnew])
                        if it < 4:
                            for g in range(2):
                                p = bank()
                                for hh in range(4):
                                    h = 4 * g + hh
                                    self.mm(p.t[:, hh * 128:(hh + 1) * 128], Pc.t[:, h, :], PTc.t[:, h, :], True, True, [PTc, Pc], [p])
                                self.cp("act", PTnew.t[:, 4 * g:4 * g + 4, :], p.t[:, :].rearrange("p (a b) -> p a b", a=4), [p], [PTnew])
                        for g in range(2):
                            p = bank()
                            for hh in range(4):
                                h = 4 * g + hh
                                self.mm(p.t[:, hh * 128:(hh + 1) * 128], Pnew.t[:, h, :], TTc.t[:, h, :], True, True, [Pnew, TTc], [p])
                            self.tt("dve", TTnew.t[:, 4 * g:4 * g + 4, :], p.t[:, :].rearrange("p (a b) -> p a b", a=4),
                                    TTc.t[:, 4 * g:4 * g + 4, :], ALU.add, [p, TTc], [TTnew])
                        Pc, PTc, TTc = Pnew, PTnew, TTnew
                    chk("I")
                    for cc in range(2):
                        tr0 = 64 * cc
                        trs = slice(tr0, tr0 + 64)
                        c0 = b * 128 + tr0
                        ccs = slice(c0, c0 + 64)
                        cend = c0 + 63
                        px = bank()
                        for h in range(8):
                            fc, j = h // 2, h % 2
                            self.mm(px.t[:, h * 64:(h + 1) * 64], at_.t[:, fc, bs], Hb.t[:, fc, 64 * j:64 * j + 64], True, False, [at_, Hb], [px])
                            self.mm(px.t[:, h * 64:(h + 1) * 64], AakT.t[:, h, :], Vt.t[:, b, h * 64:(h + 1) * 64], False, True, [AakT, Vt], [px])
                        self.cp("act", Xs.t[trs, :], px.t[trs, :], [px], [Xs])
                        pu = bank()
                        for h in range(8):
                            self.mm(pu.t[:, h * 64:(h + 1) * 64], TTc.t[:, h, :], Xs.t[:, h * 64:(h + 1) * 64], True, True, [TTc, Xs], [pu])
                        self.cp("act", Us.t[trs, :], pu.t[trs, :], [pu], [Us])
                        py = bank()
                        for fc in range(4):
                            ysl = py.t[:, fc * 64:(fc + 1) * 64]
                            self.mm(ysl, Hb.t[:, fc, :], rt.t[:, fc, ccs], True, False, [Hb, rt], [py])
                            for j in range(2):
                                h = 2 * fc + j
                                ysub = py.t[64 * j:64 * j + 64, fc * 64:(fc + 1) * 64]
                                self.mm(ysub, Us.t[:, h * 64:(h + 1) * 64], ArbT.t[:, h, trs], False, False, [Us, ArbT], [py])
                                self.mm(ysub, Vt.t[:, b, h * 64:(h + 1) * 64], ArkT.t[:, h, trs], False, j == 1, [Vt, ArkT], [py])
                        self.cp("act", yT.t[:, :, ccs], py.t[:, 0:256].rearrange("p (a b) -> p a b", a=4), [py], [yT])
                        pg_ = bank()
                        for h in range(4):
                            fg, j = h // 2, h % 2
                            osl = pg_.t[:, h * 64:(h + 1) * 64]
                            self.mm(osl, Sb.t[:, fg, j * 128:(j + 1) * 128], gq.t[:, fg, ccs], True, False, [Sb, gq], [pg_])
                            self.mm(osl, gV.t[:, b, h * 128:(h + 1) * 128], gST.t[:, h, trs], False, True, [gV, gST], [pg_])
                        self.cp("act", og.t[:, :, ccs], pg_.t[:, 0:256].rearrange("p (a b) -> p a b", a=4), [pg_], [og])
                        ph = bank()
                        for h in range(8):
                            fc, j = h // 2, h % 2
                            hsl = ph.t[:, fc * 128 + 64 * j:fc * 128 + 64 * j + 64]
                            self.mm(hsl, Bh.t[trs, b, fc * 128:(fc + 1) * 128], Us.t[trs, h * 64:(h + 1) * 64], True, False, [Bh, Us], [ph])
                            self.mm(hsl, Kh.t[trs, b, fc * 128:(fc + 1) * 128], Vt.t[trs, b, h * 64:(h + 1) * 64], False, True, [Kh, Vt], [ph])
                        for j in range(2):
                            rs = slice(64 * j, 64 * j + 64)
                            cs_ = slice(64 * j, 64 * j + 64)
                            self.P.op("dve", (lambda e, rs=rs, cs_=cs_, cend=cend:
                                              e.tensor_tensor(out=Hf.t[rs, :, cs_], in0=Hf.t[rs, :, cs_],
                                                              in1=bcast_last(ecum.t[rs, :, cend:cend + 1], 64), op=ALU.mult)),
                                      [ecum.b, Hf.b], [Hf.b])
                            self.tt("dve", Hf.t[rs, :, cs_], Hf.t[rs, :, cs_], ph.t[rs, :].rearrange("p (a b) -> p a b", a=4)[:, :, cs_], ALU.add,
                                    [ph, Hf], [Hf])
                            self.cp("pool", Hb.t[rs, :, cs_], Hf.t[rs, :, cs_], [Hf], [Hb])
                        psg = bank()
                        for h in range(4):
                            fg, j = h // 2, h % 2
                            self.mm(psg.t[:, h * 128:(h + 1) * 128], gKh.t[trs, b, fg * 128:(fg + 1) * 128], gV.t[trs, b, h * 128:(h + 1) * 128],
                                    True, True, [gKh, gV], [psg])
                        for j in range(2):
                            rs = slice(64 * j, 64 * j + 64)
                            cs_ = slice(128 * j, 128 * j + 128)
                            self.P.op("dve", (lambda e, rs=rs, cs_=cs_, cend=cend:
                                              e.tensor_tensor(out=Sf.t[rs, :, cs_], in0=Sf.t[rs, :, cs_],
                                                              in1=bcast_last(gecum.t[rs, :, cend:cend + 1], 128), op=ALU.mult)),
                                      [gecum.b, Sf.b], [Sf.b])
                            self.tt("dve", Sf.t[rs, :, cs_], Sf.t[rs, :, cs_], psg.t[rs, :].rearrange("p (a b) -> p a b", a=2)[:, :, cs_], ALU.add,
                                    [psg, Sf], [Sf])
                            self.cp("pool", Sb.t[rs, :, cs_], Sf.t[rs, :, cs_], [Sf], [Sb])
                chk("R")
                FCS = range(4)
                for fc in FCS:
                    self.cp("act", g_r.t[:, fc, :], yT.t[:, fc, :], [yT], [g_r])
                pn = []
                for fc in FCS:
                    p = bank()
                    self.mm(p.t[:, 0:TT], C["onesblk"].t[:], g_r.t[:, fc, :], True, True, [C["onesblk"], g_r], [p])
                    pn.append(p)
                for fc in FCS:
                    self.stt("dve", g_ag.t[:, fc, :], pn[fc].t[:, 0:TT], -1.0 / 64.0, yT.t[:, fc, :], ALU.mult, ALU.add, [pn[fc], yT], [g_ag])
                for fc in FCS:
                    self.act(g_k.t[:, fc, :], g_ag.t[:, fc, :], AF.Square, [g_ag], [g_k])
                pn = []
                for fc in FCS:
                    p = bank()
                    self.mm(p.t[:, 0:TT], C["onesblk"].t[:], g_k.t[:, fc, :], True, True, [C["onesblk"], g_k], [p])
                    pn.append(p)
                for fc in FCS:
                    self.act(g_kk.t[:, fc, :], pn[fc].t[:, 0:TT], AF.Sqrt, [pn[fc]], [g_kk], bias=64e-5, scale=1.0 / 64.0)
                self.recip(g_kk.t[:], g_kk.t[:], [g_kk], [g_kk])
                self.tt("dve", g_ag.t[:], g_ag.t[:], g_kk.t[:], ALU.mult, [g_ag, g_kk], [g_ag])
                for fc in FCS:
                    self.ts("pool", g_ag.t[:, fc, :], g_ag.t[:, fc, :], V("ln_w", fc), V("ln_b", fc), ALU.mult, ALU.add, [g_ag, self.vecs], [g_ag])
                self.tt("pool", g_ag.t[:], g_ag.t[:], bonus.t[:], ALU.add, [g_ag, bonus], [g_ag])
                pn = []
                for fc in FCS:
                    p = bank()
                    self.mm(p.t[:, 0:TT], L2.t[:, 2, fc * 128:(fc + 1) * 128], midB.t[:, :], True, False, [L2, midB], [p])
                    self.mm(p.t[:, 0:TT], L2.t[0:32, 3, fc * 128:(fc + 1) * 128], midC.t[0:32, :], False, True, [L2, midC], [p])
                    pn.append(p)
                for fc in FCS:
                    self.tt("dve", mixo.t[:, fc, :], g_ag.t[:, fc, :], pn[fc].t[:, 0:TT], ALU.mult, [g_ag, pn[fc]], [mixo])
                for h in FCS:
                    self.act(g_sq.t[:, h, :], og.t[:, h, :], AF.Square, [og], [g_sq])
                pn = []
                for h in FCS:
                    p = bank()
                    self.mm(p.t[:, 0:TT], self.ones.t[:], g_sq.t[:, h, :], True, True, [self.ones, g_sq], [p])
                    pn.append(p)
                for h in FCS:
                    self.act(g_rn.t[:, h, :], pn[h].t[:, 0:TT], AF.Sqrt, [pn[h]], [g_rn], bias=1e-5, scale=1.0 / 128.0)
                self.recip(g_rn.t[:], g_rn.t[:], [g_rn], [g_rn])
                for h in FCS:
                    self.stt("dve", g_kk.t[:, h, :], og.t[:, h, :], V("gnorm", h), g_rn.t[:, h, :], ALU.mult, ALU.mult, [og, g_rn, self.vecs], [g_kk])
                self.tt("pool", mixo.t[:, 4:8, :], g_kk.t[:], gsil.t[:], ALU.mult, [g_kk, gsil], [mixo])
                self.dma("pool", mix_v[:, :, t0:t0 + TT], mixo.t[:], [mixo], [self.b_mix])
              except _Stop:
                pass
            self.emit_phase()


def bcast_last(ap, nb):
    dims = [list(d) for d in ap.ap]
    return bass.AP(ap.tensor, ap.offset, [dims[0], dims[1], [0, nb]])


def bcast3(tensor, p0, nmid, midstride, col, nb):
    return bass.AP(tensor, p0 * nmid * midstride + col, [[nmid * midstride, 64], [midstride, nmid], [0, nb]])


Kern.phase_even = phase_even
```

```python
import math
from contextlib import ExitStack
import numpy as np
import concourse.bass as bass
import concourse.mybir as mybir
from concourse.bass_utils import run_bass_kernel_spmd

F32 = mybir.dt.float32
BF16 = mybir.dt.bfloat16
AF = mybir.ActivationFunctionType
ALU = mybir.AluOpType
AX = mybir.AxisListType

D = 1024
T = 4096
DEPTH = 4
FF = 2816
NFC = FF // 128
NEG = -1e30

ENGS = ("pe", "act", "dve", "pool", "sp")
EPOCH = 30000
N_DMA_SEMS = 24


class Buf:
    __slots__ = ("name", "writers", "readers")

    def __init__(self, name=""):
        self.name = name
        self.writers = []
        self.readers = []


class Op:
    __slots__ = ("eng", "fn", "deps", "signal", "val", "sem", "dma")

    def __init__(self, eng, fn, dma):
        self.eng = eng
        self.fn = fn
        self.dma = dma
        self.deps = []
        self.signal = False
        self.val = None
        self.sem = None


class Prog:
    def __init__(self):
        self.ops = {e: [] for e in ENGS}
        self.dma_slot_last = [None] * N_DMA_SEMS
        self.dma_slot_cnt = [0] * N_DMA_SEMS
        self.dma_n = 0
        self.cnt = {e: 0 for e in ENGS}
        self.seen = {e: {} for e in ENGS}
        self.last_real = {e: None for e in ENGS}
        self.bufs = []

    def buf(self, name=""):
        b = Buf(name)
        self.bufs.append(b)
        return b

    def op(self, eng, fn, reads=(), writes=(), dma=False):
        o = Op(eng, fn, dma)
        deps = {}
        for b in reads:
            for d in b.writers:
                deps[id(d)] = d
        for b in writes:
            for d in b.writers + b.readers:
                if (not dma) and (not d.dma) and d.eng == eng:
                    continue
                deps[id(d)] = d
        if dma:
            slot = self.dma_n % N_DMA_SEMS
            self.dma_n += 1
            prev = self.dma_slot_last[slot]
            if prev is not None:
                deps[id(prev)] = prev
            self.dma_slot_last[slot] = o
            self.dma_slot_cnt[slot] += 1
            o.sem = ("dma", slot)
            o.val = 16 * self.dma_slot_cnt[slot]
        for d in deps.values():
            d.signal = True
            o.deps.append(d)
        for b in reads:
            if not dma:
                b.readers = [r for r in b.readers if r.dma or r.eng != eng]
            b.readers.append(o)
        for b in writes:
            if b.readers:
                b.writers = [o]
                b.readers = []
            else:
                if not dma:
                    b.writers = [w for w in b.writers if w.dma or w.eng != eng]
                b.writers.append(o)
        self.ops[eng].append(o)
        self.last_real[eng] = o
        return o

    def barrier(self):
        lasts = [self.last_real[e] for e in ENGS if self.last_real[e] is not None]
        dl = [d for d in self.dma_slot_last if d is not None]
        for e in ENGS:
            o = Op(e, None, False)
            for d in lasts + dl:
                if d.eng == e and not d.dma:
                    continue
                d.signal = True
                o.deps.append(d)
            self.ops[e].append(o)
        for b in self.bufs:
            b.writers = []
            b.readers = []

    def finalize(self):
        for e in ENGS:
            for o in self.ops[e]:
                if o.dma or o.fn is None or o.val is not None:
                    continue
                if o.signal:
                    c = self.cnt[e]
                    o.sem = (e, c // EPOCH)
                    o.val = c % EPOCH + 1
                    self.cnt[e] = c + 1

    def replay(self, eng_name, e, get_sem, final=False):
        seen = self.seen[eng_name]
        for o in self.ops[eng_name]:
            waits = {}
            for d in o.deps:
                k = d.sem
                if d.val > waits.get(k, 0):
                    waits[k] = d.val
            for k, v in waits.items():
                if seen.get(k, 0) >= v:
                    continue
                e.wait_ge(get_sem(k), v)
                seen[k] = v
            if o.fn is None:
                continue
            ins = o.fn(e)
            if o.dma:
                ins.then_inc(get_sem(o.sem), 16)
            elif o.signal:
                ins.then_inc(get_sem(o.sem), 1)
        if final:
            for slot in range(N_DMA_SEMS):
                c = self.dma_slot_cnt[slot]
                if c and seen.get(("dma", slot), 0) < 16 * c:
                    e.wait_ge(get_sem(("dma", slot)), 16 * c)
        self.ops[eng_name] = []


class Tl:
    __slots__ = ("t", "b")

    def __init__(self, t, b):
        self.t = t
        self.b = b


class Builder:
    def __init__(self, layers, t_len=T):
        self.nc = bass.Bass("TRN2", target_bir_lowering=False)
        self.P = Prog()
        self.ges = ExitStack()
        self.sems = {}
        self.layers = layers
        self.T = t_len
        self.uid = 0

    def get_sem(self, k):
        return self.sems[k]

    def ensure_sems(self):
        need = [("dma", s) for s in range(N_DMA_SEMS)]
        for e in ENGS:
            for ep in range(self.P.cnt[e] // EPOCH + 1):
                need.append((e, ep))
        for k in need:
            if k not in self.sems:
                self.sems[k] = self.ges.enter_context(self.nc.semaphore(f"s_{k[0]}_{k[1]}"))

    def dram(self, name, shape, dt, kind=None):
        if kind:
            return self.nc.dram_tensor(name, list(shape), dt, kind=kind).ap()
        return self.nc.dram_tensor(name, list(shape), dt).ap()

    def sb(self, es, shape, dt, name=None):
        self.uid += 1
        t = es.enter_context(self.nc.sbuf_tensor(f"{name or 't'}_{self.uid}", list(shape), dt))
        return Tl(t, self.P.buf(name))

    def ps(self, es, shape, dt, name=None):
        self.uid += 1
        t = es.enter_context(self.nc.psum_tensor(f"{name or 'p'}_{self.uid}", list(shape), dt))
        return Tl(t, self.P.buf(name))

    @staticmethod
    def _b(xs):
        return [x.b if isinstance(x, Tl) else x for x in xs]

    def mm(self, out, lhsT, rhs, start, stop, R, W):
        self.P.op("pe", lambda e: e.matmul(out, lhsT=lhsT, rhs=rhs, start=start, stop=stop), self._b(R), self._b(W))

    def tr(self, out, in_, ident, R, W):
        self.P.op("pe", lambda e: e.transpose(out, in_, ident), self._b(R), self._b(W))

    def act(self, out, in_, func, R, W, bias=None, scale=None):
        kw = {}
        if bias is not None:
            kw["bias"] = bias
        if scale is not None:
            kw["scale"] = scale
        self.P.op("act", lambda e: e.activation(out=out, in_=in_, func=func, **kw), self._b(R), self._b(W))

    def tt(self, eng, out, in0, in1, op, R, W):
        self.P.op(eng, lambda e: e.tensor_tensor(out=out, in0=in0, in1=in1, op=op), self._b(R), self._b(W))

    def ts(self, eng, out, in0, s1, s2, op0, op1, R, W):
        if s2 is None:
            self.P.op(eng, lambda e: e.tensor_scalar(out=out, in0=in0, scalar1=s1, scalar2=None, op0=op0), self._b(R), self._b(W))
        else:
            self.P.op(eng, lambda e: e.tensor_scalar(out=out, in0=in0, scalar1=s1, scalar2=s2, op0=op0, op1=op1), self._b(R), self._b(W))

    def stt(self, eng, out, in0, scalar, in1, op0, op1, R, W):
        eng = "dve"
        self.P.op(eng, lambda e: e.scalar_tensor_tensor(out=out, in0=in0, scalar=scalar, in1=in1, op0=op0, op1=op1), self._b(R), self._b(W))

    def cp(self, eng, out, in_, R, W):
        if eng == "act":
            self.P.op("act", lambda e: e.activation(out=out, in_=in_, func=AF.Copy), self._b(R), self._b(W))
        else:
            self.P.op(eng, lambda e: e.tensor_copy(out=out, in_=in_), self._b(R), self._b(W))

    def recip(self, out, in_, R, W):
        self.P.op("dve", lambda e: e.reciprocal(out=out, in_=in_), self._b(R), self._b(W))

    def memset(self, eng, ap, val, W):
        self.P.op(eng, lambda e: e.memset(ap, val), [], self._b(W))

    def dma(self, eng, out, in_, R, W):
        self.P.op(eng, lambda e: e.dma_start(out=out, in_=in_), self._b(R), self._b(W), dma=True)

    def emit_phase(self, final=False):
        P = self.P
        P.barrier()
        P.finalize()
        self.ensure_sems()
        nc = self.nc
        with nc.Block() as block:
            @block.tensor
            def _(e):
                P.replay("pe", e, self.get_sem)

            @block.scalar
            def _(e):
                P.replay("act", e, self.get_sem)

            @block.vector
            def _(e):
                P.replay("dve", e, self.get_sem)

            @block.gpsimd
            def _(e):
                P.replay("pool", e, self.get_sem)

            @block.sync
            def _(e):
                P.replay("sp", e, self.get_sem, final=final)


def bcast_free(tl_ap_tensor, offset, pstride, nparts, n):
    return bass.AP(tl_ap_tensor, offset, [[pstride, nparts], [0, n]])


def slabA(w, kchunks):
    K, Fd = w.shape
    return np.ascontiguousarray(w.reshape(kchunks, 128, Fd // 128, 128).transpose(2, 1, 0, 3))


def slabB(w, kchunks, ncol):
    K, Fd = w.shape
    return np.ascontiguousarray(w.reshape(kchunks, 128, Fd // ncol, ncol).transpose(2, 1, 0, 3))


def pvec(v):
    return np.ascontiguousarray(v.reshape(-1, 128).T)


def t5_buckets_np(n):
    dist = np.arange(n, dtype=np.int32)
    max_exact = 16
    ratio = np.maximum(dist, max_exact).astype(np.float32) / np.float32(max_exact)
    large = max_exact + (np.log(ratio).astype(np.float32) / np.float32(math.log(128 / 16)) * np.float32(16)).astype(np.int32)
    large = np.minimum(large, 31)
    return np.where(dist < max_exact, dist, large)


VEC_COLS = {}
_vc = 0


def _vadd(name, n):
    global _vc
    VEC_COLS[name] = (_vc, n)
    _vc += n


for _l in range(DEPTH):
    _vadd(f"nmix{_l}", 8)
    _vadd(f"nffn{_l}", 8)
_vadd("nfinal", 8)
for _i in range(2):
    _vadd(f"subln{_i}", 1)
    _vadd(f"lam{_i}", 4)
    _vadd(f"mu_rkv{_i}", 12)
    _vadd(f"mu_wag{_i}", 24)
    _vadd(f"a0{_i}", 4)
    _vadd(f"k_k{_i}", 4)
    _vadd(f"k_a{_i}", 4)
    _vadd(f"r_k{_i}", 4)
    _vadd(f"ln_w{_i}", 4)
    _vadd(f"ln_b{_i}", 4)
    _vadd(f"gnorm{_i}", 4)
_vadd("bfar", 8)
NVEC = _vc


def pack_vecs(inp):
    v = np.zeros((128, NVEC), np.float32)

    def put(name, arr):
        c, n = VEC_COLS[name]
        v[:, c:c + n] = arr

    for l in range(DEPTH):
        put(f"nmix{l}", pvec(inp["norm_mix"][l]))
        put(f"nffn{l}", pvec(inp["norm_ffn"][l]))
    put("nfinal", pvec(inp["norm_final"]))
    for i in range(2):
        put(f"subln{i}", inp["da_subln"][i].reshape(128, 1))
        lam = np.zeros((128, 4), np.float32)
        for j, nm in enumerate(["da_lam_q1", "da_lam_k1", "da_lam_q2", "da_lam_k2"]):
            lam[:64, j] = inp[nm][i]
        put(f"lam{i}", lam)
        put(f"mu_rkv{i}", pvec(inp["rw_mu_rkv"][i].reshape(-1)))
        put(f"mu_wag{i}", pvec(inp["rw_mu_wag"][i].reshape(-1)))
        put(f"a0{i}", pvec(inp["rw_a0"][i]))
        put(f"k_k{i}", pvec(inp["rw_k_k"][i]))
        put(f"k_a{i}", pvec(inp["rw_k_a"][i]))
        put(f"r_k{i}", pvec(inp["rw_r_k"][i].reshape(-1)))
        put(f"ln_w{i}", pvec(inp["rw_ln_w"][i]))
        put(f"ln_b{i}", pvec(inp["rw_ln_b"][i]))
        put(f"gnorm{i}", pvec(inp["gla_norm"][i]))
    bk = t5_buckets_np(T)
    assert (bk[113:] == bk[113]).all()
    put("bfar", np.broadcast_to(inp["rel_bias"][bk[200]][None, :], (128, 8)))
    return v


def pack_shared(inp):
    sh = {}
    w_in = [inp["ab_w_in"][0], inp["da_w_qkv"][0], inp["ab_w_in"][1], inp["da_w_qkv"][1]]
    w_out = [inp["ab_w_out"][0], inp["da_w_out"][0], inp["ab_w_out"][1], inp["da_w_out"][1]]
    sh["w_inA"] = np.stack([slabA(w, 8) for w in w_in])
    sh["w_inB"] = np.stack([slabB(w, 8, 512) for w in w_in])
    sh["w_out"] = np.stack([slabA(w, 8) for w in w_out])
    gu = []
    for l in range(DEPTH):
        g = slabA(inp["ffn_w_gate"][l], 8)
        u = slabA(inp["ffn_w_up"][l], 8)
        gu.append(np.concatenate([g, u], axis=3))
    sh["w_gu"] = np.stack(gu)
    sh["w_dn"] = np.stack([slabA(inp["ffn_w_down"][l], NFC) for l in range(DEPTH)])
    sh["vecs"] = pack_vecs(inp)
    bk = t5_buckets_np(T)
    bbd = inp["rel_bias"][bk]
    idx = np.arange(1024)[None, :] - np.arange(128)[:, None] - 384
    G = np.full((8, 128, 1024), NEG, np.float32)
    pos = idx >= 0
    for h in range(8):
        G[h][pos] = bbd[idx[pos], h]
    sh["biasG"] = np.ascontiguousarray(G.transpose(1, 0, 2))
    l1 = []
    for i in range(2):
        cat = np.concatenate([inp["rw_w1"][i], inp["rw_a1"][i], inp["rw_g1"][i], inp["gla_wa1"][i]], axis=1)
        l1.append(np.ascontiguousarray(cat.reshape(8, 128, 304).transpose(1, 0, 2)))
    sh["lora1"] = np.stack(l1)
    l2 = np.zeros((2, 128, 5, 512), np.float32)
    for i in range(2):
        l2[i, 0:64, 0, :] = inp["rw_w2"][i]
        l2[i, 64:128, 1, :] = inp["rw_a2"][i]
        l2[i, :, 2, :] = inp["rw_g2"][i][0:128]
        l2[i, 0:32, 3, :] = inp["rw_g2"][i][128:160]
        l2[i, 32:48, 4, 0:256] = inp["gla_wa2"][i]
    sh["lora2"] = l2
    rows = np.zeros((2, 1, 768), np.float32)
    for i in range(2):
        rows[i, 0, 0:512] = inp["rw_w0"][i]
        rows[i, 0, 512:768] = inp["gla_ba"][i]
    sh["rows"] = rows
    return sh


def flat2d(ap, n_elems, maxcols=8192):
    raise NotImplementedError


class Kern(Builder):
    def __init__(self, layers, t_len=T, dbg=False):
        super().__init__(layers, t_len)
        self.dbg = dbg
        nc = self.nc
        Tn = self.T
        self.xT = self.dram("xT", [D, Tn], F32, "ExternalInput")
        shp = {
            "w_inA": [4, 24, 128, 8, 128], "w_inB": [4, 6, 128, 8, 512], "w_out": [4, 8, 128, 8, 128],
            "w_gu": [4, NFC, 128, 8, 256], "w_dn": [4, 8, 128, NFC, 128],
        }
        self.wf = {k: self.dram(k, s, F32, "ExternalInput") for k, s in shp.items()}
        self.wb = {k: self.dram(k + "_bf", s, BF16) for k, s in shp.items()}
        self.wshape = shp
        self.vecs_d = self.dram("vecs", [128, NVEC], F32, "ExternalInput")
        self.biasG_d = self.dram("biasG", [128, 8, 1024], F32, "ExternalInput")
        self.lora1_d = self.dram("lora1", [2, 128, 8, 304], F32, "ExternalInput")
        self.lora2_d = self.dram("lora2", [2, 128, 5, 512], F32, "ExternalInput")
        self.rows_d = self.dram("rows", [2, 1, 768], F32, "ExternalInput")
        self.yT = self.dram("yT", [D, Tn], F32, "ExternalOutput")
        self.x_s = self.dram("x_s", [D, Tn], F32)
        self.mix_s = self.dram("mix_s", [D, Tn], BF16)
        self.q_s = self.dram("q_s", [8, 128, Tn], BF16)
        self.k_s = self.dram("k_s", [8, 128, Tn], BF16)
        self.v_s = self.dram("v_s", [Tn, D], BF16)
        if dbg:
            self.dbg_mix = self.dram("dbg_mix", [D, Tn], BF16, "ExternalOutput")
        self.b_x = self.P.buf("x_dram")
        self.b_mix = self.P.buf("mix_dram")
        self.b_qkv = self.P.buf("qkv_dram")
        self.b_wl = [self.P.buf(f"w_dram{l}") for l in range(DEPTH)]

    def cast_layer(self, l):
        for k, s in self.wshape.items():
            n = int(np.prod(s[1:]))
            cols = n // 128
            src = self.wf[k][l].rearrange("a p k j -> (a p k j)").rearrange("(r c) -> r c", r=128) if False else None
            ft = self.wf[k].tensor
            bt = self.wb[k].tensor
            base = l * n
            piece = 8192
            c0 = 0
            while c0 < cols:
                cc = min(piece, cols - c0)
                src = bass.AP(ft, base + c0, [[cols, 128], [1, cc]])
                dst = bass.AP(bt, base + c0, [[cols, 128], [1, cc]])
                self.dma("pool", dst, src, [], [self.b_wl[l]])
                c0 += cc

    def setup(self):
        nc = self.nc
        es = self.ges
        self.ident = self.sb(es, [128, 128], BF16, "ident")
        self.ones = self.sb(es, [128, 128], BF16, "ones")
        self.vecs = self.sb(es, [128, NVEC], F32, "vecs")
        self.vx = self.sb(es, [128, 64], F32, "vx")
        self.memset("pool", self.ident.t[:], 0.0, [self.ident])
        self.P.op("pool", lambda e: e.affine_select(out=self.ident.t[:], in_=self.ident.t[:], pattern=[[-1, 128]],
                                                    compare_op=ALU.not_equal, fill=1.0, base=0, channel_multiplier=1),
                  [self.ident.b], [self.ident.b])
        self.memset("pool", self.ones.t[:], 1.0, [self.ones])
        self.dma("sp", self.vecs.t[:], self.vecs_d, [], [self.vecs])
        self.cast_layer(self.layers[0])
        self.emit_phase()

    def vcol(self, name, j=0, n=1):
        c, _ = VEC_COLS[name]
        return self.vecs.t[:, c + j:c + j + n]

    def rmsnorm(self, xt, hT, hoff, TT, gsc, sq, pbank, rstd, eps, out_dt_f32=None):
        for kc in range(8):
            self.act(sq.t[:, kc, :], xt.t[:, kc, :], AF.Square, [xt], [sq])
        for kc in range(8):
            self.mm(pbank.t[:, 0:TT], self.ones.t[:], sq.t[:, kc, :], kc == 0, kc == 7, [self.ones, sq], [pbank])
        self.act(rstd.t[:, 0:TT], pbank.t[:, 0:TT], AF.Sqrt, [pbank], [rstd], bias=self.epsD(eps), scale=1.0)
        self.recip(rstd.t[:, 0:TT], rstd.t[:, 0:TT], [rstd], [rstd])
        for kc in range(8):
            eng = "dve" if kc % 2 == 0 else "pool"
            self.stt(eng, hT.t[:, kc, hoff:hoff + TT], xt.t[:, kc, :], gsc[:, kc:kc + 1], rstd.t[:, 0:TT],
                     ALU.mult, ALU.mult, [xt, rstd, self.vx], [hT])

    def epsD(self, eps):
        return float(D * eps)

    def phase_ffn(self, l, x_src, last, next_layer):
        TT = 512
        NT = self.T // TT
        sqD = math.sqrt(D)
        with ExitStack() as es:
            xt = [self.sb(es, [128, 8, TT], F32, "xt") for _ in range(2)]
            mt = [self.sb(es, [128, 8, TT], BF16, "mt") for _ in range(2)]
            hT = self.sb(es, [128, 8, TT], BF16, "hT")
            sq = self.sb(es, [128, 8, TT], BF16, "sq")
            rstd = self.sb(es, [128, TT], F32, "rstd")
            actb = self.sb(es, [128, NFC, TT], BF16, "actb")
            wgu = [self.sb(es, [128, 8, 256], BF16, "wgu") for _ in range(5)]
            wdn = [self.sb(es, [128, NFC, 128], BF16, "wdn") for _ in range(8)]
            sq2 = self.sb(es, [128, 8, TT], BF16, "sq2") if last else None
            rstd2 = self.sb(es, [128, TT], F32, "rstd2") if last else None
            wo = [self.sb(es, [128, 8, 128], BF16, "wo") for _ in range(8)]
            sg = [self.sb(es, [128, TT], BF16, "sg") for _ in range(2)]
            for dc in range(8):
                self.dma("sp", wo[dc].t[:], self.wb["w_out"][l, dc], [self.b_wl[l]], [wo[dc]])
            for dc in range(8):
                self.dma("sp", wdn[dc].t[:], self.wb["w_dn"][l, dc], [self.b_wl[l]], [wdn[dc]])
            pb = [self.ps(es, [128, 512], F32, "pb") for _ in range(8)]
            pbi = [0]

            def bank():
                b = pb[pbi[0] % 8]
                pbi[0] += 1
                return b

            c_ffn, _ = VEC_COLS[f"nffn{l}"]
            self.ts("dve", self.vx.t[:, 0:8], self.vecs.t[:, c_ffn:c_ffn + 8], sqD, None, ALU.mult, None, [self.vecs], [self.vx])
            if last:
                c_fin, _ = VEC_COLS["nfinal"]
                self.ts("dve", self.vx.t[:, 8:16], self.vecs.t[:, c_fin:c_fin + 8], sqD, None, ALU.mult, None, [self.vecs], [self.vx])

            xsrc_v = x_src.rearrange("(kc p) t -> p kc t", p=128)
            xs_v = self.x_s.rearrange("(kc p) t -> p kc t", p=128)
            y_v = self.yT.rearrange("(kc p) t -> p kc t", p=128)
            mix_v = self.mix_s.rearrange("(kc p) t -> p kc t", p=128)
            wi = [0, 0, 0]

            def load_tile(i):
                t0 = i * TT
                self.dma("pool", xt[i % 2].t[:], xsrc_v[:, :, t0:t0 + TT], [self.b_x], [xt[i % 2]])
                self.dma("pool", mt[i % 2].t[:], mix_v[:, :, t0:t0 + TT], [self.b_mix], [mt[i % 2]])

            def outproj(i):
                x = xt[i % 2]
                m = mt[i % 2]
                for dc in range(8):
                    w = wo[dc]
                    p = bank()
                    for kc in range(8):
                        self.mm(p.t[:, 0:TT], w.t[:, kc, :], m.t[:, kc, :], kc == 0, kc == 7, [w, m], [p])
                    self.tt("dve", x.t[:, dc, :], x.t[:, dc, :], p.t[:, 0:TT], ALU.add, [x, p], [x])
                for kc in range(8):
                    self.act(sq.t[:, kc, :], x.t[:, kc, :], AF.Square, [x], [sq])

            def norm_rest(i):
                x = xt[i % 2]
                p = bank()
                for kc in range(8):
                    self.mm(p.t[:, 0:TT], self.ones.t[:], sq.t[:, kc, :], kc == 0, kc == 7, [self.ones, sq], [p])
                self.act(rstd.t[:, 0:TT], p.t[:, 0:TT], AF.Sqrt, [p], [rstd], bias=self.epsD(1e-6), scale=1.0)
                self.recip(rstd.t[:, 0:TT], rstd.t[:, 0:TT], [rstd], [rstd])
                for kc in range(8):
                    self.stt("dve", hT.t[:, kc, 0:TT], x.t[:, kc, :], self.vx.t[:, kc:kc + 1], rstd.t[:, 0:TT],
                             ALU.mult, ALU.mult, [x, rstd, self.vx], [hT])

            load_tile(0)
            if NT > 1:
                load_tile(1)
            outproj(0)
            norm_rest(0)
            for i in range(NT):
                t0 = i * TT
                x = xt[i % 2]
                for fc in range(NFC):
                    w = wgu[wi[1] % len(wgu)]
                    wi[1] += 1
                    self.dma("sp", w.t[:], self.wb["w_gu"][l, fc], [self.b_wl[l]], [w])
                    pg = bank()
                    pu = bank()
                    for kc in range(8):
                        self.mm(pg.t[:, 0:TT], w.t[:, kc, 0:128], hT.t[:, kc, :], kc == 0, kc == 7, [w, hT], [pg])
                    for kc in range(8):
                        self.mm(pu.t[:, 0:TT], w.t[:, kc, 128:256], hT.t[:, kc, :], kc == 0, kc == 7, [w, hT], [pu])
                    s_ = sg[fc % 2]
                    self.act(s_.t[:], pg.t[:, 0:TT], AF.Silu, [pg], [s_])
                    self.tt("dve", actb.t[:, fc, :], s_.t[:], pu.t[:, 0:TT], ALU.mult, [s_, pu], [actb])
                if i + 1 < NT:
                    outproj(i + 1)
                for dc in range(8):
                    if dc == 4 and i + 1 < NT:
                        norm_rest(i + 1)
                    w = wdn[dc]
                    p = bank()
                    for fc in range(NFC):
                        self.mm(p.t[:, 0:TT], w.t[:, fc, :], actb.t[:, fc, :], fc == 0, fc == NFC - 1, [w, actb], [p])
                    self.tt("dve", x.t[:, dc, :], x.t[:, dc, :], p.t[:, 0:TT], ALU.add, [x, p], [x])
                if not last:
                    self.dma("pool", xs_v[:, :, t0:t0 + TT], x.t[:], [x], [self.b_x])
                else:
                    p = bank()
                    self.rmsnorm(x, x, 0, TT, self.vx.t[:, 8:16], sq2, p, rstd2, 1e-6)
                    self.dma("pool", y_v[:, :, t0:t0 + TT], x.t[:], [x], [])
                if i + 2 < NT:
                    load_tile(i + 2)
            self.emit_phase(final=last)

    def phase_qkv(self, l, x_src):
        TT = 512
        NT = self.T // TT
        sqD = math.sqrt(D)
        with ExitStack() as es:
            xt = [self.sb(es, [128, 8, TT], F32, "xt") for _ in range(2)]
            hT = self.sb(es, [128, 8, TT], BF16, "hT")
            sq = self.sb(es, [128, 8, TT], BF16, "sq")
            rstd = self.sb(es, [128, TT], F32, "rstd")
            wa = [self.sb(es, [128, 8, 128], BF16, "wa") for _ in range(3)]
            wbs = [self.sb(es, [128, 8, 512], BF16, "wbs") for _ in range(2)]
            qst = [self.sb(es, [128, 8, TT], BF16, "qst") for _ in range(2)]
            kst = [self.sb(es, [128, 8, TT], BF16, "kst") for _ in range(2)]
            vst = [self.sb(es, [128, 4, D], BF16, "vst") for _ in range(2)]
            pb = [self.ps(es, [128, 512], F32, "pb") for _ in range(8)]
            pbi = [0]

            def bank():
                b = pb[pbi[0] % 8]
                pbi[0] += 1
                return b

            c_n, _ = VEC_COLS[f"nmix{l}"]
            self.ts("dve", self.vx.t[:, 16:24], self.vecs.t[:, c_n:c_n + 8], sqD, None, ALU.mult, None, [self.vecs], [self.vx])
            xsrc_v = x_src.rearrange("(kc p) t -> p kc t", p=128)
            q_v = self.q_s.rearrange("c p t -> p c t")
            k_v = self.k_s.rearrange("c p t -> p c t")
            v_v = self.v_s.rearrange("(tb p) f -> p tb f", p=128)
            wi = [0, 0]
            self.dma("sp", xt[0].t[:], xsrc_v[:, :, 0:TT], [self.b_x], [xt[0]])
            for i in range(NT):
                t0 = i * TT
                x = xt[i % 2]
                if i + 1 < NT:
                    self.dma("sp", xt[(i + 1) % 2].t[:], xsrc_v[:, :, t0 + TT:t0 + 2 * TT], [self.b_x], [xt[(i + 1) % 2]])
                p = bank()
                self.rmsnorm(x, hT, 0, TT, self.vx.t[:, 16:24], sq, p, rstd, 1e-6)
                qs = qst[i % 2]
                ks = kst[i % 2]
                vs = vst[i % 2]
                for c in range(16):
                    w = wa[wi[0] % 3]
                    wi[0] += 1
                    self.dma("sp", w.t[:], self.wb["w_inA"][l, c], [self.b_wl[l]], [w])
                    p = bank()
                    for kc in range(8):
                        self.mm(p.t[:, 0:TT], w.t[:, kc, :], hT.t[:, kc, :], kc == 0, kc == 7, [w, hT], [p])
                    if c < 8:
                        self.act(qs.t[:, c, :], p.t[:, 0:TT], AF.Copy, [p], [qs], scale=0.125)
                    else:
                        self.cp("dve", ks.t[:, c - 8, :], p.t[:, 0:TT], [p], [ks])
                for cg in range(2):
                    w = wbs[wi[1] % 2]
                    wi[1] += 1
                    self.dma("sp", w.t[:], self.wb["w_inB"][l, 4 + cg], [self.b_wl[l]], [w])
                    for tb in range(4):
                        p = bank()
                        for kc in range(8):
                            self.mm(p.t[:, :], hT.t[:, kc, tb * 128:(tb + 1) * 128], w.t[:, kc, :], kc == 0, kc == 7, [w, hT], [p])
                        if tb % 2 == 0:
                            self.cp("act", vs.t[:, tb, cg * 512:(cg + 1) * 512], p.t[:, :], [p], [vs])
                        else:
                            self.cp("dve", vs.t[:, tb, cg * 512:(cg + 1) * 512], p.t[:, :], [p], [vs])
                self.dma("pool", q_v[:, :, t0:t0 + TT], qs.t[:], [qs], [self.b_qkv])
                self.dma("pool", k_v[:, :, t0:t0 + TT], ks.t[:], [ks], [self.b_qkv])
                self.dma("pool", v_v[:, 4 * i:4 * i + 4, :], vs.t[:], [vs], [self.b_qkv])
            self.emit_phase()

    def phase_attn(self, l, next_layer=None):
        i_odd = l // 2
        lam_init = 0.8 - 0.6 * math.exp(-0.3 * l)
        Tn = self.T
        QB = 512
        NQ = Tn // QB
        NKB = Tn // 128
        with ExitStack() as es:
            qT = [self.sb(es, [128, Tn], BF16, "qT") for _ in range(2)]
            kT = [self.sb(es, [128, Tn], BF16, "kT") for _ in range(2)]
            vv = [self.sb(es, [128, NKB, 128], BF16, "vv") for _ in range(2)]
            Gf = self.sb(es, [128, 1024], F32, "Gf")
            Gb = self.sb(es, [128, 8, 1024], BF16, "Gb")
            pt = [[self.sb(es, [128, QB], BF16, "pt") for _ in range(3)] for _ in range(2)]
            pacc = [self.sb(es, [128, QB], F32, "pacc") for _ in range(2)]
            tmp = [self.sb(es, [128, QB], F32, "tmp") for _ in range(5)]
            sqb = self.sb(es, [128, QB], BF16, "sqb")
            ost = [self.sb(es, [128, QB], BF16, "ost") for _ in range(2)]
            lamt = self.sb(es, [128, 8], F32, "lamt")
            stp = [self.ps(es, [128, 512], F32, "st") for _ in range(6)]
            ot = [self.ps(es, [128, 512], F32, "ot") for _ in range(2)]
            sti = [0]
            st = {}

            if next_layer is not None:
                self.cast_layer(next_layer)
            for h in range(8):
                self.dma("sp", Gf.t[:], self.biasG_d[:, h, :], [], [Gf])
                self.cp("dve", Gb.t[:, h, :], Gf.t[:], [Gf], [Gb])
            c_l, _ = VEC_COLS[f"lam{i_odd}"]
            lv = self.vecs.t
            self.tt("dve", lamt.t[:, 0:1], lv[:, c_l:c_l + 1], lv[:, c_l + 1:c_l + 2], ALU.mult, [self.vecs], [lamt])
            self.tt("dve", lamt.t[:, 1:2], lv[:, c_l + 2:c_l + 3], lv[:, c_l + 3:c_l + 4], ALU.mult, [self.vecs], [lamt])
            self.memset("pool", tmp[0].t[:, 0:128], 1.0, [tmp[0]])
            self.mm(stp[0].t[:, 0:2], tmp[0].t[:, 0:128], lamt.t[:, 0:2], True, True, [tmp[0], lamt], [stp[0]])
            self.act(lamt.t[:, 2:4], stp[0].t[:, 0:2], AF.Exp, [stp[0]], [lamt])
            onesf = self.sb(es, [128, 128], F32, "onesf")
            self.memset("pool", onesf.t[:], 1.0, [onesf])
            self.tt("dve", lamt.t[:, 4:5], lamt.t[:, 3:4], lamt.t[:, 2:3], ALU.subtract, [lamt], [lamt])
            self.ts("dve", lamt.t[:, 4:5], lamt.t[:, 4:5], -lam_init, None, ALU.add, None, [lamt], [lamt])
            c_s, _ = VEC_COLS[f"subln{i_odd}"]
            self.ts("dve", lamt.t[:, 5:6], lv[:, c_s:c_s + 1], (1.0 - lam_init) * math.sqrt(128.0), None, ALU.mult, None, [self.vecs], [lamt])
            c_bf, _ = VEC_COLS["bfar"]

            v_v = self.v_s.rearrange("(kb p) f -> p kb f", p=128)

            def load_head(h):
                self.dma("sp", qT[h % 2].t[:], self.q_s[h], [self.b_qkv], [qT[h % 2]])
                self.dma("sp", kT[h % 2].t[:], self.k_s[h], [self.b_qkv], [kT[h % 2]])
                self.dma("sp", vv[h % 2].t[:], v_v[:, :, h * 128:(h + 1) * 128], [self.b_qkv], [vv[h % 2]])

            load_head(0)
            ep = 0
            for h in range(8):
                if h + 1 < 8:
                    load_head(h + 1)
                q = qT[h % 2]
                k = kT[h % 2]
                v = vv[h % 2]
                bfar = lv[:, c_bf + h:c_bf + h + 1]
                for qb in range(NQ):
                    q0 = qb * QB
                    nkb = 4 * qb + 4
                    qsl = slice(q0, q0 + QB)

                    def issue_st(kb):
                        k0 = kb * 128
                        delta = k0 - q0
                        special = delta >= -128
                        for m in range(2):
                            s_ = stp[sti[0] % 6]
                            sti[0] += 1
                            st[(kb, m)] = s_
                            self.mm(s_.t[:, :], k.t[64 * m:64 * m + 64, k0:k0 + 128], q.t[64 * m:64 * m + 64, qsl],
                                    True, not special, [k, q], [s_])
                            if special:
                                j0 = 384 - delta
                                self.mm(s_.t[:, :], self.ident.t[:], Gb.t[:, h, j0:j0 + 512], False, True, [self.ident, Gb], [s_])
                        return special

                    spec = {}
                    for kb in range(min(2, nkb)):
                        spec[kb] = issue_st(kb)
                    for kb in range(nkb):
                        if kb + 2 < nkb:
                            spec[kb + 2] = issue_st(kb + 2)
                        for m in range(2):
                            s_ = st.pop((kb, m))
                            p_ = pt[m][kb % 3]
                            if spec[kb]:
                                self.act(p_.t[:], s_.t[:, :], AF.Exp, [s_], [p_])
                            else:
                                self.act(p_.t[:], s_.t[:, :], AF.Exp, [s_, self.vecs], [p_], bias=bfar)
                            aeng = "pool" if ((2 * kb + m) % 3 == 2) else "dve"
                            if kb == 0:
                                self.cp(aeng, pacc[m].t[:], p_.t[:], [p_], [pacc[m]])
                            else:
                                self.tt(aeng, pacc[m].t[:], pacc[m].t[:], p_.t[:], ALU.add, [p_, pacc[m]], [pacc[m]])
                            self.mm(ot[m].t[:, :], v.t[:, kb, :], p_.t[:], kb == 0, kb == nkb - 1, [v, p_], [ot[m]])
                    lt = []
                    for m in range(2):
                        l_ = stp[sti[0] % 6]
                        sti[0] += 1
                        self.mm(l_.t[:, :], onesf.t[:], pacc[m].t[:], True, True, [onesf, pacc[m]], [l_])
                        lt.append(l_)
                    r0, r1, o0, o1, oo = tmp
                    self.recip(r1.t[:], lt[1].t[:, :], [lt[1]], [r1])
                    self.tt("dve", r0.t[:], lt[0].t[:, :], r1.t[:], ALU.mult, [lt[0], r1], [r0])
                    self.tt("dve", o1.t[:], ot[1].t[:, :], r0.t[:], ALU.mult, [ot[1], r0], [o1])
                    self.stt("dve", oo.t[:], o1.t[:], lamt.t[:, 4:5], ot[0].t[:, :], ALU.mult, ALU.add, [o1, ot[0], lamt], [oo])
                    self.act(sqb.t[:], oo.t[:], AF.Square, [oo], [sqb])
                    self.act(o0.t[:], lt[0].t[:, :], AF.Square, [lt[0]], [o0], scale=math.sqrt(128 * 1e-5))
                    pss = stp[sti[0] % 6]
                    sti[0] += 1
                    self.mm(pss.t[:, :], self.ones.t[:], sqb.t[:], True, True, [self.ones, sqb], [pss])
                    self.tt("dve", r1.t[:], pss.t[:, :], o0.t[:], ALU.add, [pss, o0], [r1])
                    self.act(r1.t[:], r1.t[:], AF.Ln, [r1], [r1])
                    self.act(r1.t[:], r1.t[:], AF.Exp, [r1], [r1], scale=-0.5)
                    os_ = ost[ep % 2]
                    ep += 1
                    self.stt("dve", os_.t[:], oo.t[:], lamt.t[:, 5:6], r1.t[:], ALU.mult, ALU.mult, [oo, lamt, r1], [os_])
                    self.dma("pool", self.mix_s[h * 128:(h + 1) * 128, q0:q0 + QB], os_.t[:], [os_], [self.b_mix])
            self.emit_phase()

    def build(self):
        self.setup()
        x_src = self.xT
        n = len(self.layers)
        for j, l in enumerate(self.layers):
            last = j == n - 1
            nxt = None if last else self.layers[j + 1]
            if l % 2 == 0:
                self.phase_even(l, x_src, nxt)
            else:
                self.phase_qkv(l, x_src)
                self.phase_attn(l, nxt)
            if self.dbg and j == 0:
                self.dma("sp", self.dbg_mix, self.mix_s, [self.b_mix], [])
            self.phase_ffn(l, x_src, last, nxt)
            x_src = self.x_s
        return self.nc


_CACHE = {}


def run(inputs, layers=(0, 1, 2, 3), n_cores=8, dbg=False, trace=False):
    Tn = inputs["x"].shape[1]
    key = (tuple(layers), Tn, dbg)
    if key not in _CACHE:
        kb = Kern(list(layers), Tn, dbg)
        _CACHE[key] = kb.build()
    nc = _CACHE[key]
    sh = pack_shared(inputs)
    in_maps = []
    for b in range(n_cores):
        m = dict(sh)
        m["xT"] = np.ascontiguousarray(inputs["x"][b].T)
        in_maps.append(m)
    res = run_bass_kernel_spmd(nc, in_maps, core_ids=list(range(n_cores)), trace=trace)
    return res


def kernel(**inputs):
    inputs = {k: np.asarray(v) for k, v in inputs.items()}
    res = run(inputs)
    out = np.stack([np.ascontiguousarray(r["yT"].T) for r in res.results], axis=0)
    return out.astype(np.float32)


CDEC = math.exp(-0.5)


def _even_consts(self, es):
    c = {}

    def mask(name, kind, val, dt=F32):
        t = self.sb(es, [128, 128], dt, name)
        self.memset("pool", t.t[:], val, [t])
        if kind == "u_incl":
            pat, cm, op = [[1, 128]], -1, ALU.is_ge
            z = (slice(0, 64), slice(64, 128))
        elif kind == "u_strict":
            pat, cm, op = [[1, 128]], -1, ALU.is_gt
            z = (slice(0, 64), slice(64, 128))
        else:
            pat, cm, op = [[-1, 128]], 1, ALU.is_gt
            z = (slice(64, 128), slice(0, 64))
        self.P.op("pool", lambda e: e.affine_select(out=t.t[:], in_=t.t[:], pattern=pat, compare_op=op, fill=0.0,
                                                    base=0, channel_multiplier=cm), [t.b], [t.b])
        self.memset("pool", t.t[z[0], z[1]], 0.0, [t])
        c[name] = t
        return t

    mask("MleF", "u_incl", -CDEC)
    mask("MltF", "u_strict", -CDEC)
    mask("MgtF", "l_strict", -CDEC)
    mask("MleG", "u_incl", 1.0 / 16.0)
    mask("MgtG", "l_strict", 1.0 / 16.0)
    ui = mask("UI", "u_incl", 1.0, BF16)
    us = mask("US", "u_strict", 1.0, BF16)
    ls = mask("LS", "l_strict", 1.0, BF16)
    for nm, src in (("UI4", ui), ("US4", us), ("LS4", ls)):
        t = self.sb(es, [128, 4, 128], BF16, nm)
        for r in range(4):
            self.cp("pool", t.t[:, r, :], src.t[:], [src], [t])
        c[nm] = t
    t = self.sb(es, [128, 8, 128], BF16, "identrep")
    for r in range(8):
        self.cp("pool", t.t[:, r, :], self.ident.t[:], [self.ident], [t])
    c["identrep"] = t
    ob = self.sb(es, [128, 128], BF16, "onesblk")
    self.memset("pool", ob.t[:], 1.0, [ob])
    self.memset("pool", ob.t[0:64, 64:128], 0.0, [ob])
    self.memset("pool", ob.t[64:128, 0:64], 0.0, [ob])
    c["onesblk"] = ob
    return c


Kern._even_consts = _even_consts


def phase_even(self, l, x_src, next_layer=None):
    i_ev = l // 2
    TT = 256
    NT = self.T // TT
    sqD = math.sqrt(D)
    V = lambda name, j=0, n=1: self.vcol(f"{name}{i_ev}", j, n)
    with ExitStack() as es0:
        C = self._even_consts(es0)
        L1c = self.sb(es0, [128, 8, 304], BF16, "L1c")
        L1p = self.sb(es0, [128, 8, 304], BF16, "L1p")
        with ExitStack() as es:
            L1f = self.sb(es, [128, 8, 304], F32, "L1f")
            L1pf = self.sb(es, [128, 8, 304], F32, "L1pf")
            self.dma("sp", L1f.t[:], self.lora1_d[i_ev], [], [L1f])
            self.memset("pool", L1pf.t[:, :, 288:304], 0.0, [L1pf])
            for kc in range(8):
                for (c0, c1, mj) in ((0, 64, 0), (64, 128, 8), (128, 288, 16)):
                    eng = "dve" if kc % 2 == 0 else "pool"
                    self.ts(eng, L1pf.t[:, kc, c0:c1], L1f.t[:, kc, c0:c1], V("mu_wag", mj + kc), None, ALU.mult, None,
                            [L1f, self.vecs], [L1pf])
            self.cp("act", L1p.t[:], L1pf.t[:], [L1pf], [L1p])
            self.tt("dve", L1c.t[:], L1f.t[:], L1pf.t[:], ALU.subtract, [L1f, L1pf], [L1c])
            c_n, _ = VEC_COLS[f"nmix{l}"]
            self.ts("dve", self.vx.t[:, 16:24], self.vecs.t[:, c_n:c_n + 8], sqD, None, ALU.mult, None, [self.vecs], [self.vx])
            self.ts("dve", self.vx.t[:, 24:36], V("mu_rkv", 0, 12), -1.0, 1.0, ALU.mult, ALU.add, [self.vecs], [self.vx])
            self.ts("dve", self.vx.t[:, 36:40], V("k_a", 0, 4), -1.0, 1.0, ALU.mult, ALU.add, [self.vecs], [self.vx])
            self.emit_phase()
        with ExitStack() as es:
            sb = lambda shape, dt, nm: self.sb(es, shape, dt, nm)
            xt = sb([128, 8, TT], F32, "xt")
            hT = sb([128, 8, TT + 2], BF16, "hT")
            sq = sb([128, 8, TT], BF16, "sq")
            rstd = sb([128, TT], F32, "rstd")
            wA = [sb([128, 8, 128], BF16, "wA") for _ in range(3)]
            wB3 = sb([128, 8, 256], BF16, "wB3")
            wB4 = sb([128, 8, 512], BF16, "wB4")
            L2f = sb([128, 5, 512], F32, "L2f")
            L2 = sb([128, 5, 512], BF16, "L2")
            rows = sb([128, 768], F32, "rows")
            pm = sb([128, 12, TT + 2], BF16, "pm")
            midA = sb([128, TT], BF16, "midA")
            midB = sb([128, TT], BF16, "midB")
            midC = sb([128, TT], BF16, "midC")
            sgw = sb([128, 2, 512], F32, "sgw")
            la = sb([128, 2, 256], F32, "la")
            ztmp = sb([128, 512], F32, "ztmp")
            g_ag = sb([128, 4, TT], F32, "g_ag")
            g_kk = sb([128, 4, TT], F32, "g_kk")
            g_rn = sb([128, 4, TT], F32, "g_rn")
            g_r = sb([128, 4, TT], BF16, "g_r")
            g_k = sb([128, 4, TT], BF16, "g_k")
            g_sq = sb([128, 4, TT], BF16, "g_sq")
            g_en = sb([128, 4, TT], BF16, "g_en")
            g_ex = sb([128, 4, TT], BF16, "g_ex")
            ecum = sb([128, 4, TT], F32, "ecum")
            bb = sb([128, 4, TT], BF16, "bb")
            kp = sb([128, 4, TT], BF16, "kp")
            vv = sb([128, 4, TT], BF16, "vv")
            rt = sb([128, 4, TT], BF16, "rt")
            at_ = sb([128, 4, TT], BF16, "at")
            bt = sb([128, 4, TT], BF16, "bt")
            kt = sb([128, 4, TT], BF16, "kt")
            Bh = sb([128, 2, 512], BF16, "Bh")
            Kh = sb([128, 2, 512], BF16, "Kh")
            Vt = sb([128, 2, 512], BF16, "Vt")
            etoend = sb([128, 2, 512], BF16, "etoend")
            bonus = sb([128, 4, TT], F32, "bonus")
            AabT = sb([128, 8, 128], BF16, "AabT")
            Aab = sb([128, 8, 128], BF16, "Aab")
            AakT = sb([128, 8, 128], BF16, "AakT")
            ArbT = sb([128, 8, 128], BF16, "ArbT")
            ArkT = sb([128, 8, 128], BF16, "ArkT")
            Pn = [sb([128, 8, 128], BF16, "Pn") for _ in range(2)]
            PTn = [sb([128, 8, 128], BF16, "PTn") for _ in range(2)]
            TTn = [sb([128, 8, 128], BF16, "TTn") for _ in range(2)]
            gq = sb([128, 2, TT], BF16, "gq")
            gk = sb([128, 2, TT], BF16, "gk")
            gKh = sb([128, 2, 256], BF16, "gKh")
            gV = sb([128, 2, 512], BF16, "gV")
            gsil = sb([128, 4, TT], BF16, "gsil")
            gecum = sb([128, 2, TT], F32, "gecum")
            gencum = sb([128, 2, TT], F32, "gencum")
            getoend = sb([128, 2, 256], F32, "getoend")
            gST = sb([128, 4, 128], BF16, "gST")
            yT = sb([128, 4, TT], F32, "yT")
            og = sb([128, 4, TT], F32, "og")
            mixo = sb([128, 8, TT], BF16, "mixo")
            Hf = sb([128, 4, 128], F32, "Hf")
            Hb = sb([128, 4, 128], BF16, "Hb")
            Xs = sb([128, 512], BF16, "Xs")
            Us = sb([128, 512], BF16, "Us")
            Sf = sb([128, 2, 256], F32, "Sf")
            Sb = sb([128, 2, 256], BF16, "Sb")
            pb = [self.ps(es, [128, 512], F32, "pb") for _ in range(6)]
            pbf = [self.ps(es, [128, 512], BF16, "pbf") for _ in range(2)]
            pbi = [0, 0]
            tfi = [0, 0]
            if getattr(self, "dbg_mem", False):
                try:
                    print("EVEN sbuf remaining:", self.nc.sbuf_bytes_remaining)
                except Exception as ex:
                    print("sbuf query failed", ex)

            def bank():
                b = pb[pbi[0] % 6]
                pbi[0] += 1
                return b

            def bankbf():
                b = pbf[pbi[1] % 2]
                pbi[1] += 1
                return b

            def TF():
                t = tf[tfi[0] % 8]
                tfi[0] += 1
                return t

            def TB():
                t = tb[tfi[1] % 8]
                tfi[1] += 1
                return t

            if next_layer is not None:
                self.cast_layer(next_layer)
            self.dma("sp", L2f.t[:], self.lora2_d[i_ev], [], [L2f])
            self.cp("act", L2.t[:], L2f.t[:], [L2f], [L2])
            self.dma("sp", rows.t[:], self.rows_d[i_ev].partition_broadcast(128), [], [rows])
            self.dma("sp", wB3.t[:], self.wb["w_inB"][l, 3, :, :, 256:512], [self.b_wl[l]], [wB3])
            self.dma("sp", wB4.t[:], self.wb["w_inB"][l, 4], [self.b_wl[l]], [wB4])
            self.memset("pool", hT.t[:, :, 0:2], 0.0, [hT])
            self.memset("pool", pm.t[:, :, 0:2], 0.0, [pm])
            for z in (Hf, Hb, Xs, Us, Sf, Sb):
                self.memset("pool", z.t[:], 0.0, [z])
            xsrc_v = x_src.rearrange("(kc p) t -> p kc t", p=128)
            mix_v = self.mix_s.rearrange("(kc p) t -> p kc t", p=128)
            wi = [0]
            self.dma("sp", xt.t[:], xsrc_v[:, :, 0:TT], [self.b_x], [xt])

            def slabA(c):
                w = wA[wi[0] % 3]
                wi[0] += 1
                self.dma("sp", w.t[:], self.wb["w_inA"][l, c], [self.b_wl[l]], [w])
                return w

            def proj_fm(c):
                w = slabA(c)
                p = bank()
                for kc in range(8):
                    self.mm(p.t[:, 0:TT], w.t[:, kc, :], hT.t[:, kc, 2:2 + TT], kc == 0, kc == 7, [w, hT], [p])
                return p

            stop = getattr(self, "even_stop", None)

            class _Stop(Exception):
                pass

            def chk(st):
                if stop == st:
                    raise _Stop()

            for i in range(NT):
              try:
                t0 = i * TT
                if i > 0:
                    self.cp("dve", hT.t[:, :, 1:2], hT.t[:, :, TT + 1:TT + 2], [hT], [hT])
                    self.cp("dve", pm.t[:, :, 1:2], pm.t[:, :, TT + 1:TT + 2], [pm], [pm])
                p = bank()
                self.rmsnorm(xt, hT, 2, TT, self.vx.t[:, 16:24], sq, p, rstd, 1e-6)
                if i + 1 < NT:
                    self.dma("sp", xt.t[:], xsrc_v[:, :, t0 + TT:t0 + 2 * TT], [self.b_x], [xt])
                chk("B")
                for (mid, c0, c1) in ((midA, 0, 128), (midB, 128, 256), (midC, 256, 304)):
                    m = c1 - c0
                    p = bank()
                    for kc in range(8):
                        self.mm(p.t[0:m, 0:TT], L1c.t[:, kc, c0:c1], hT.t[:, kc, 2:2 + TT], kc == 0, False, [L1c, hT], [p])
                    for kc in range(8):
                        self.mm(p.t[0:m, 0:TT], L1p.t[:, kc, c0:c1], hT.t[:, kc, 1:1 + TT], False, kc == 7, [L1p, hT], [p])
                    if mid is midA:
                        self.act(mid.t[0:64, :], p.t[0:64, 0:TT], AF.Tanh, [p], [mid])
                        self.cp("dve", mid.t[64:128, :], p.t[64:128, 0:TT], [p], [mid])
                    elif mid is midB:
                        self.act(mid.t[:, :], p.t[:, 0:TT], AF.Sigmoid, [p], [mid])
                    else:
                        self.act(mid.t[0:32, :], p.t[0:32, 0:TT], AF.Sigmoid, [p], [mid])
                        self.cp("dve", mid.t[32:48, :], p.t[32:48, 0:TT], [p], [mid])
                chk("C")
                for b in range(2):
                    bs = slice(b * 128, (b + 1) * 128)
                    p = bank()
                    self.mm(p.t[:, :], midA.t[0:64, bs], L2.t[0:64, 0, :], True, True, [midA, L2], [p])
                    self.tt("dve", ztmp.t[:], p.t[:, :], rows.t[:, 0:512], ALU.add, [p, rows], [ztmp])
                    self.act(sgw.t[:, b, :], ztmp.t[:], AF.Sigmoid, [ztmp], [sgw])
                    p = bank()
                    self.mm(p.t[:, 0:256], midC.t[32:48, bs], L2.t[32:48, 4, 0:256], True, True, [midC, L2], [p])
                    self.tt("dve", ztmp.t[:, 0:256], p.t[:, 0:256], rows.t[:, 512:768], ALU.add, [p, rows], [ztmp])
                    self.act(ztmp.t[:, 256:512], ztmp.t[:, 0:256], AF.Sigmoid, [ztmp], [ztmp])
                    self.act(la.t[:, b, :], ztmp.t[:, 256:512], AF.Ln, [ztmp], [la])
                chk("D")
                for b in range(2):
                    p = bank()
                    self.mm(p.t[:, :], C["MgtF"].t[:], sgw.t[:, b, :], True, True, [C["MgtF"], sgw], [p])
                    self.act(etoend.t[:, b, :], p.t[:, :], AF.Exp, [p], [etoend])
                    p = bank()
                    self.mm(p.t[:, 0:256], C["MgtG"].t[:], la.t[:, b, :], True, True, [C["MgtG"], la], [p])
                    self.act(getoend.t[:, b, :], p.t[:, 0:256], AF.Exp, [p], [getoend])
                for fg in range(2):
                    p = bank()
                    for b in range(2):
                        self.mm(p.t[:, b * 128:(b + 1) * 128], la.t[:, b, fg * 128:(fg + 1) * 128], C["MleG"].t[:], True, True,
                                [la, C["MleG"]], [p])
                    self.act(gecum.t[:, fg, :], p.t[:, 0:TT], AF.Exp, [p], [gecum])
                    self.act(gencum.t[:, fg, :], p.t[:, 0:TT], AF.Exp, [p], [gencum], scale=-1.0)
                chk("E")
                def stage_F1():
                    for fg in range(2):
                        p = proj_fm(12 + fg)
                        self.stt("dve", gq.t[:, fg, :], p.t[:, 0:TT], 0.125, gecum.t[:, fg, :], ALU.mult, ALU.mult, [p, gecum], [gq])
                        p = proj_fm(14 + fg)
                        self.tt("dve", gk.t[:, fg, :], p.t[:, 0:TT], gencum.t[:, fg, :], ALU.mult, [p, gencum], [gk])

                def stage_F2():
                    for fc in range(4):
                        p = proj_fm(20 + fc)
                        self.act(gsil.t[:, fc, :], p.t[:, 0:TT], AF.Silu, [p], [gsil])
                    for b in range(2):
                        p = bank()
                        for kc in range(8):
                            self.mm(p.t[:, 0:256], hT.t[:, kc, 2 + b * 128:2 + (b + 1) * 128], wB3.t[:, kc, :], kc == 0, kc == 7, [hT, wB3], [p])
                        self.tt("dve", gKh.t[:, b, :], p.t[:, 0:256], getoend.t[:, b, :], ALU.mult, [p, getoend], [gKh])
                        p = bank()
                        for kc in range(8):
                            self.mm(p.t[:, :], hT.t[:, kc, 2 + b * 128:2 + (b + 1) * 128], wB4.t[:, kc, :], kc == 0, kc == 7, [hT, wB4], [p])
                        self.cp("act", gV.t[:, b, :], p.t[:, :], [p], [gV])
                chk("F")
                FCS = range(4)
                for q3 in range(3):
                    for fc in FCS:
                        c = 4 * q3 + fc
                        p = proj_fm(c)
                        self.ts("dve", pm.t[:, c, 2:2 + TT], p.t[:, 0:TT], V("mu_rkv", c), None, ALU.mult, None, [p, self.vecs], [pm])
                        dst = (g_r, g_k, vv)[q3]
                        self.stt("dve", dst.t[:, fc, :], p.t[:, 0:TT], self.vx.t[:, 24 + c:25 + c], pm.t[:, c, 1:1 + TT], ALU.mult, ALU.add,
                                 [p, self.vx, pm], [dst])
                chk("G1")
                for fc in FCS:
                    p = bank()
                    self.mm(p.t[:, 0:TT], L2.t[64:128, 1, fc * 128:(fc + 1) * 128], midA.t[64:128, :], True, True, [L2, midA], [p])
                    self.act(g_ag.t[:, fc, :], p.t[:, 0:TT], AF.Sigmoid, [p, self.vecs], [g_ag], bias=V("a0", fc))
                for fc in FCS:
                    pc = bank()
                    for b in range(2):
                        self.mm(pc.t[:, b * 128:(b + 1) * 128], sgw.t[:, b, fc * 128:(fc + 1) * 128], C["MleF"].t[:], True, True,
                                [sgw, C["MleF"]], [pc])
                        self.mm(pc.t[:, 256 + b * 128:256 + (b + 1) * 128], sgw.t[:, b, fc * 128:(fc + 1) * 128], C["MltF"].t[:], True, True,
                                [sgw, C["MltF"]], [pc])
                    self.act(ecum.t[:, fc, :], pc.t[:, 0:TT], AF.Exp, [pc], [ecum])
                    self.act(g_en.t[:, fc, :], pc.t[:, 0:TT], AF.Exp, [pc], [g_en], scale=-1.0)
                    self.act(g_ex.t[:, fc, :], pc.t[:, 256:256 + TT], AF.Exp, [pc], [g_ex])
                for fc in FCS:
                    self.ts("pool", g_kk.t[:, fc, :], g_k.t[:, fc, :], V("k_k", fc), None, ALU.mult, None, [g_k, self.vecs], [g_kk])
                for fc in FCS:
                    self.act(g_sq.t[:, fc, :], g_kk.t[:, fc, :], AF.Square, [g_kk], [g_sq])
                pn = []
                for fc in FCS:
                    p = bank()
                    self.mm(p.t[:, 0:TT], C["onesblk"].t[:], g_sq.t[:, fc, :], True, True, [C["onesblk"], g_sq], [p])
                    pn.append(p)
                for fc in FCS:
                    self.act(g_rn.t[:, fc, :], pn[fc].t[:, 0:TT], AF.Sqrt, [pn[fc]], [g_rn], bias=1e-24, scale=1.0)
                stage_F1()
                self.recip(g_rn.t[:], g_rn.t[:], [g_rn], [g_rn])
                self.tt("dve", g_kk.t[:], g_kk.t[:], g_rn.t[:], ALU.mult, [g_kk, g_rn], [g_kk])
                for fc in FCS:
                    self.ts("pool", g_rn.t[:, fc, :], g_ag.t[:, fc, :], V("k_a", fc), self.vx.t[:, 36 + fc:37 + fc], ALU.mult, ALU.add,
                            [g_ag, self.vecs, self.vx], [g_rn])
                self.tt("pool", kp.t[:], g_k.t[:], g_rn.t[:], ALU.mult, [g_k, g_rn], [kp])
                self.tt("pool", bb.t[:], g_kk.t[:], g_ag.t[:], ALU.mult, [g_kk, g_ag], [bb])
                self.tt("dve", rt.t[:], g_r.t[:], ecum.t[:], ALU.mult, [g_r, ecum], [rt])
                self.stt("dve", at_.t[:], g_kk.t[:], -1.0, g_ex.t[:], ALU.mult, ALU.mult, [g_kk, g_ex], [at_])
                self.tt("pool", bt.t[:], bb.t[:], g_en.t[:], ALU.mult, [bb, g_en], [bt])
                self.tt("pool", kt.t[:], kp.t[:], g_en.t[:], ALU.mult, [kp, g_en], [kt])
                for fc in FCS:
                    self.stt("dve", g_sq.t[:, fc, :], g_r.t[:, fc, :], V("r_k", fc), kp.t[:, fc, :], ALU.mult, ALU.mult, [g_r, kp, self.vecs], [g_sq])
                stage_F2()
                pn = []
                for fc in FCS:
                    p = bank()
                    self.mm(p.t[:, 0:TT], C["onesblk"].t[:], g_sq.t[:, fc, :], True, True, [C["onesblk"], g_sq], [p])
                    pn.append(p)
                for fc in FCS:
                    self.tt("dve", bonus.t[:, fc, :], pn[fc].t[:, 0:TT], vv.t[:, fc, :], ALU.mult, [pn[fc], vv], [bonus])
                chk("G")
                for b in range(2):
                    bs = slice(b * 128, (b + 1) * 128)
                    for (src, dst, useE) in ((bb, Bh, True), (kp, Kh, True), (vv, Vt, False)):
                        p = bankbf()
                        for fc in range(4):
                            self.tr(p.t[:, fc * 128:(fc + 1) * 128], src.t[:, fc, bs], self.ident.t[:], [src, self.ident], [p])
                        if useE:
                            self.tt("dve", dst.t[:, b, :], p.t[:, :], etoend.t[:, b, :], ALU.mult, [p, etoend], [dst])
                        else:
                            self.cp("act", dst.t[:, b, :], p.t[:, :], [p], [dst])
                    chk("H")
                    for (dstS, lh, rh, msk) in ((AabT, bt, at_, "US4"), (Aab, at_, bt, "LS4"), (AakT, kt, at_, "US4"),
                                                (ArbT, bt, rt, "UI4"), (ArkT, kt, rt, "UI4")):
                        pg2 = [bank(), bank()]
                        for fc in range(4):
                            for g in range(2):
                                ro = 64 * g
                                self.mm(pg2[g].t[:, fc * 128:(fc + 1) * 128], lh.t[ro:ro + 64, fc, bs], rh.t[ro:ro + 64, fc, bs], True, True,
                                        [lh, rh], [pg2[g]])
                        for g in range(2):
                            self.tt("dve", dstS.t[:, g:8:2, :], pg2[g].t[:, :].rearrange("p (a b) -> p a b", a=4), C[msk].t[:], ALU.mult,
                                    [pg2[g], C[msk]], [dstS])
                    pg2 = [bank(), bank()]
                    for fg in range(2):
                        for g in range(2):
                            ro = 64 * g
                            self.mm(pg2[g].t[:, fg * 128:(fg + 1) * 128], gk.t[ro:ro + 64, fg, bs], gq.t[ro:ro + 64, fg, bs], True, True,
                                    [gk, gq], [pg2[g]])
                    for g in range(2):
                        self.tt("dve", gST.t[:, g:4:2, :], pg2[g].t[:, 0:256].rearrange("p (a b) -> p a b", a=2), C["UI4"].t[:, 0:2, :], ALU.mult,
                                [pg2[g], C["UI4"]], [gST])
                    chk("S")
                    Pc, PTc = Aab, AabT
                    TTc = TTn[0]
                    self.tt("pool", TTc.t[:], AabT.t[:], C["identrep"].t[:], ALU.add, [AabT, C["identrep"]], [TTc])
                    for it in range(5):
                        Pnew = Pn[it % 2]
                        PTnew = PTn[it % 2]
                        TTnew = TTn[(it + 1) % 2]
                        for g in range(2):
                            p = bank()
                            for hh in range(4):
                                h = 4 * g + hh
                                self.mm(p.t[:, hh * 128:(hh + 1) * 128], PTc.t[:, h, :], Pc.t[:, h, :], True, True, [PTc, Pc], [p])
                            self.cp("act", Pnew.t[:, 4 * g:4 * g + 4, :], p.t[:, :].rearrange("p (a b) -> p a b", a=4), [p], [Pnew])
                        if it < 4:
                            for g in range(2):
                                p = bank()
                                for hh in range(4):
                                    h = 4 * g + hh
                                    self.mm(p.t[:, hh * 128:(hh + 1) * 128], Pc.t[:, h, :], PTc.t[:, h, :], True, True, [PTc, Pc], [p])
                                self.cp("act", PTnew.t[:, 4 * g:4 * g + 4, :], p.t[:, :].rearrange("p (a b) -> p a b", a=4), [p], [PTnew])
                        for g in range(2):
                            p = bank()
                            for hh in range(4):
                                h = 4 * g + hh
                                self.mm(p.t[:, hh * 128:(hh + 1) * 128], Pnew.t[:, h, :], TTc.t[:, h, :], True, True, [Pnew, TTc], [p])
                            self.tt("dve", TTnew.t[:, 4 * g:4 * g + 4, :], p.t[:, :].rearrange("p (a b) -> p a b", a=4),
                                    TTc.t[:, 4 * g:4 * g + 4, :], ALU.add, [p, TTc], [TTnew])
                        Pc, PTc, TTc = Pnew, PTnew, TTnew
                    chk("I")
                    for cc in range(2):
                        tr0 = 64 * cc
                        trs = slice(tr0, tr0 + 64)
                        c0 = b * 128 + tr0
                        ccs = slice(c0, c0 + 64)
                        cend = c0 + 63
                        px = bank()
                        for h in range(8):
                            fc, j = h // 2, h % 2
                            self.mm(px.t[:, h * 64:(h + 1) * 64], at_.t[:, fc, bs], Hb.t[:, fc, 64 * j:64 * j + 64], True, False, [at_, Hb], [px])
                            self.mm(px.t[:, h * 64:(h + 1) * 64], AakT.t[:, h, :], Vt.t[:, b, h * 64:(h + 1) * 64], False, True, [AakT, Vt], [px])
                        self.cp("act", Xs.t[trs, :], px.t[trs, :], [px], [Xs])
                        pu = bank()
                        for h in range(8):
                            self.mm(pu.t[:, h * 64:(h + 1) * 64], TTc.t[:, h, :], Xs.t[:, h * 64:(h + 1) * 64], True, True, [TTc, Xs], [pu])
                        self.cp("act", Us.t[trs, :], pu.t[trs, :], [pu], [Us])
                        py = bank()
                        for fc in range(4):
                            ysl = py.t[:, fc * 64:(fc + 1) * 64]
                            self.mm(ysl, Hb.t[:, fc, :], rt.t[:, fc, ccs], True, False, [Hb, rt], [py])
                            for j in range(2):
                                h = 2 * fc + j
                                ysub = py.t[64 * j:64 * j + 64, fc * 64:(fc + 1) * 64]
                                self.mm(ysub, Us.t[:, h * 64:(h + 1) * 64], ArbT.t[:, h, trs], False, False, [Us, ArbT], [py])
                                self.mm(ysub, Vt.t[:, b, h * 64:(h + 1) * 64], ArkT.t[:, h, trs], False, j == 1, [Vt, ArkT], [py])
                        self.cp("act", yT.t[:, :, ccs], py.t[:, 0:256].rearrange("p (a b) -> p a b", a=4), [py], [yT])
                        pg_ = bank()
                        for h in range(4):
                            fg, j = h // 2, h % 2
                            osl = pg_.t[:, h * 64:(h + 1) * 64]
                            self.mm(osl, Sb.t[:, fg, j * 128:(j + 1) * 128], gq.t[:, fg, ccs], True, False, [Sb, gq], [pg_])
                            self.mm(osl, gV.t[:, b, h * 128:(h + 1) * 128], gST.t[:, h, trs], False, True, [gV, gST], [pg_])
                        self.cp("act", og.t[:, :, ccs], pg_.t[:, 0:256].rearrange("p (a b) -> p a b", a=4), [pg_], [og])
                        ph = bank()
                        for h in range(8):
                            fc, j = h // 2, h % 2
                            hsl = ph.t[:, fc * 128 + 64 * j:fc * 128 + 64 * j + 64]
                            self.mm(hsl, Bh.t[trs, b, fc * 128:(fc + 1) * 128], Us.t[trs, h * 64:(h + 1) * 64], True, False, [Bh, Us], [ph])
                            self.mm(hsl, Kh.t[trs, b, fc * 128:(fc + 1) * 128], Vt.t[trs, b, h * 64:(h + 1) * 64], False, True, [Kh, Vt], [ph])
                        for j in range(2):
                            rs = slice(64 * j, 64 * j + 64)
                            cs_ = slice(64 * j, 64 * j + 64)
                            self.P.op("dve", (lambda e, rs=rs, cs_=cs_, cend=cend:
                                              e.tensor_tensor(out=Hf.t[rs, :, cs_], in0=Hf.t[rs, :, cs_],
                                                              in1=bcast_last(ecum.t[rs, :, cend:cend + 1], 64), op=ALU.mult)),
                                      [ecum.b, Hf.b], [Hf.b])
                            self.tt("dve", Hf.t[rs, :, cs_], Hf.t[rs, :, cs_], ph.t[rs, :].rearrange("p (a b) -> p a b", a=4)[:, :, cs_], ALU.add,
                                    [ph, Hf], [Hf])
                            self.cp("pool", Hb.t[rs, :, cs_], Hf.t[rs, :, cs_], [Hf], [Hb])
                        psg = bank()
                        for h in range(4):
                            fg, j = h // 2, h % 2
                            self.mm(psg.t[:, h * 128:(h + 1) * 128], gKh.t[trs, b, fg * 128:(fg + 1) * 128], gV.t[trs, b, h * 128:(h + 1) * 128],
                                    True, True, [gKh, gV], [psg])
                        for j in range(2):
                            rs = slice(64 * j, 64 * j + 64)
                            cs_ = slice(128 * j, 128 * j + 128)
                            self.P.op("dve", (lambda e, rs=rs, cs_=cs_, cend=cend:
                                              e.tensor_tensor(out=Sf.t[rs, :, cs_], in0=Sf.t[rs, :, cs_],
                                                              in1=bcast_last(gecum.t[rs, :, cend:cend + 1], 128), op=ALU.mult)),
                                      [gecum.b, Sf.b], [Sf.b])
                            self.tt("dve", Sf.t[rs, :, cs_], Sf.t[rs, :, cs_], psg.t[rs, :].rearrange("p (a b) -> p a b", a=2)[:, :, cs_], ALU.add,
                                    [psg, Sf], [Sf])
                            self.cp("pool", Sb.t[rs, :, cs_], Sf.t[rs, :, cs_], [Sf], [Sb])
                chk("R")
                FCS = range(4)
                for fc in FCS:
                    self.cp("act", g_r.t[:, fc, :], yT.t[:, fc, :], [yT], [g_r])
                pn = []
                for fc in FCS:
                    p = bank()
                    self.mm(p.t[:, 0:TT], C["onesblk"].t[:], g_r.t[:, fc, :], True, True, [C["onesblk"], g_r], [p])
                    pn.append(p)
                for fc in FCS:
                    self.stt("dve", g_ag.t[:, fc, :], pn[fc].t[:, 0:TT], -1.0 / 64.0, yT.t[:, fc, :], ALU.mult, ALU.add, [pn[fc], yT], [g_ag])
                for fc in FCS:
                    self.act(g_k.t[:, fc, :], g_ag.t[:, fc, :], AF.Square, [g_ag], [g_k])
                pn = []
                for fc in FCS:
                    p = bank()
                    self.mm(p.t[:, 0:TT], C["onesblk"].t[:], g_k.t[:, fc, :], True, True, [C["onesblk"], g_k], [p])
                    pn.append(p)
                for fc in FCS:
                    self.act(g_kk.t[:, fc, :], pn[fc].t[:, 0:TT], AF.Sqrt, [pn[fc]], [g_kk], bias=64e-5, scale=1.0 / 64.0)
                self.recip(g_kk.t[:], g_kk.t[:], [g_kk], [g_kk])
                self.tt("dve", g_ag.t[:], g_ag.t[:], g_kk.t[:], ALU.mult, [g_ag, g_kk], [g_ag])
                for fc in FCS:
                    self.ts("pool", g_ag.t[:, fc, :], g_ag.t[:, fc, :], V("ln_w", fc), V("ln_b", fc), ALU.mult, ALU.add, [g_ag, self.vecs], [g_ag])
                self.tt("pool", g_ag.t[:], g_ag.t[:], bonus.t[:], ALU.add, [g_ag, bonus], [g_ag])
                pn = []
                for fc in FCS:
                    p = bank()
                    self.mm(p.t[:, 0:TT], L2.t[:, 2, fc * 128:(fc + 1) * 128], midB.t[:, :], True, False, [L2, midB], [p])
                    self.mm(p.t[:, 0:TT], L2.t[0:32, 3, fc * 128:(fc + 1) * 128], midC.t[0:32, :], False, True, [L2, midC], [p])
                    pn.append(p)
                for fc in FCS:
                    self.tt("dve", mixo.t[:, fc, :], g_ag.t[:, fc, :], pn[fc].t[:, 0:TT], ALU.mult, [g_ag, pn[fc]], [mixo])
                for h in FCS:
                    self.act(g_sq.t[:, h, :], og.t[:, h, :], AF.Square, [og], [g_sq])
                pn = []
                for h in FCS:
                    p = bank()
                    self.mm(p.t[:, 0:TT], self.ones.t[:], g_sq.t[:, h, :], True, True, [self.ones, g_sq], [p])
                    pn.append(p)
                for h in FCS:
                    self.act(g_rn.t[:, h, :], pn[h].t[:, 0:TT], AF.Sqrt, [pn[h]], [g_rn], bias=1e-5, scale=1.0 / 128.0)
                self.recip(g_rn.t[:], g_rn.t[:], [g_rn], [g_rn])
                for h in FCS:
                    self.stt("dve", g_kk.t[:, h, :], og.t[:, h, :], V("gnorm", h), g_rn.t[:, h, :], ALU.mult, ALU.mult, [og, g_rn, self.vecs], [g_kk])
                self.tt("pool", mixo.t[:, 4:8, :], g_kk.t[:], gsil.t[:], ALU.mult, [g_kk, gsil], [mixo])
                self.dma("pool", mix_v[:, :, t0:t0 + TT], mixo.t[:], [mixo], [self.b_mix])
              except _Stop:
                pass
            self.emit_phase()


def bcast_last(ap, nb):
    dims = [list(d) for d in ap.ap]
    return bass.AP(ap.tensor, ap.offset, [dims[0], dims[1], [0, nb]])


def bcast3(tensor, p0, nmid, midstride, col, nb):
    return bass.AP(tensor, p0 * nmid * midstride + col, [[nmid * midstride, 64], [midstride, nmid], [0, nb]])


Kern.phase_even = phase_even
```

```python
import math
from contextlib import ExitStack
import numpy as np
import concourse.bass as bass
import concourse.mybir as mybir
from concourse.bass_utils import run_bass_kernel_spmd

F32 = mybir.dt.float32
BF16 = mybir.dt.bfloat16
AF = mybir.ActivationFunctionType
ALU = mybir.AluOpType
AX = mybir.AxisListType

D = 1024
T = 4096
DEPTH = 4
FF = 2816
NFC = FF // 128
NEG = -1e30

ENGS = ("pe", "act", "dve", "pool", "sp")
EPOCH = 30000
N_DMA_SEMS = 24


class Buf:
    __slots__ = ("name", "writers", "readers")

    def __init__(self, name=""):
        self.name = name
        self.writers = []
        self.readers = []


class Op:
    __slots__ = ("eng", "fn", "deps", "signal", "val", "sem", "dma")

    def __init__(self, eng, fn, dma):
        self.eng = eng
        self.fn = fn
        self.dma = dma
        self.deps = []
        self.signal = False
        self.val = None
        self.sem = None


class Prog:
    def __init__(self):
        self.ops = {e: [] for e in ENGS}
        self.dma_slot_last = [None] * N_DMA_SEMS
        self.dma_slot_cnt = [0] * N_DMA_SEMS
        self.dma_n = 0
        self.cnt = {e: 0 for e in ENGS}
        self.seen = {e: {} for e in ENGS}
        self.last_real = {e: None for e in ENGS}
        self.bufs = []

    def buf(self, name=""):
        b = Buf(name)
        self.bufs.append(b)
        return b

    def op(self, eng, fn, reads=(), writes=(), dma=False):
        o = Op(eng, fn, dma)
        deps = {}
        for b in reads:
            for d in b.writers:
                deps[id(d)] = d
        for b in writes:
            for d in b.writers + b.readers:
                if (not dma) and (not d.dma) and d.eng == eng:
                    continue
                deps[id(d)] = d
        if dma:
            slot = self.dma_n % N_DMA_SEMS
            self.dma_n += 1
            prev = self.dma_slot_last[slot]
            if prev is not None:
                deps[id(prev)] = prev
            self.dma_slot_last[slot] = o
            self.dma_slot_cnt[slot] += 1
            o.sem = ("dma", slot)
            o.val = 16 * self.dma_slot_cnt[slot]
        for d in deps.values():
            d.signal = True
            o.deps.append(d)
        for b in reads:
            if not dma:
                b.readers = [r for r in b.readers if r.dma or r.eng != eng]
            b.readers.append(o)
        for b in writes:
            if b.readers:
                b.writers = [o]
                b.readers = []
            else:
                if not dma:
                    b.writers = [w for w in b.writers if w.dma or w.eng != eng]
                b.writers.append(o)
        self.ops[eng].append(o)
        self.last_real[eng] = o
        return o

    def barrier(self):
        lasts = [self.last_real[e] for e in ENGS if self.last_real[e] is not None]
        dl = [d for d in self.dma_slot_last if d is not None]
        for e in ENGS:
            o = Op(e, None, False)
            for d in lasts + dl:
                if d.eng == e and not d.dma:
                    continue
                d.signal = True
                o.deps.append(d)
            self.ops[e].append(o)
        for b in self.bufs:
            b.writers = []
            b.readers = []

    def finalize(self):
        for e in ENGS:
            for o in self.ops[e]:
                if o.dma or o.fn is None or o.val is not None:
                    continue
                if o.signal:
                    c = self.cnt[e]
                    o.sem = (e, c // EPOCH)
                    o.val = c % EPOCH + 1
                    self.cnt[e] = c + 1

    def replay(self, eng_name, e, get_sem, final=False):
        seen = self.seen[eng_name]
        for o in self.ops[eng_name]:
            waits = {}
            for d in o.deps:
                k = d.sem
                if d.val > waits.get(k, 0):
                    waits[k] = d.val
            for k, v in waits.items():
                if seen.get(k, 0) >= v:
                    continue
                e.wait_ge(get_sem(k), v)
                seen[k] = v
            if o.fn is None:
                continue
            ins = o.fn(e)
            if o.dma:
                ins.then_inc(get_sem(o.sem), 16)
            elif o.signal:
                ins.then_inc(get_sem(o.sem), 1)
        if final:
            for slot in range(N_DMA_SEMS):
                c = self.dma_slot_cnt[slot]
                if c and seen.get(("dma", slot), 0) < 16 * c:
                    e.wait_ge(get_sem(("dma", slot)), 16 * c)
        self.ops[eng_name] = []


class Tl:
    __slots__ = ("t", "b")

    def __init__(self, t, b):
        self.t = t
        self.b = b


class Builder:
    def __init__(self, layers, t_len=T):
        self.nc = bass.Bass("TRN2", target_bir_lowering=False)
        self.P = Prog()
        self.ges = ExitStack()
        self.sems = {}
        self.layers = layers
        self.T = t_len
        self.uid = 0

    def get_sem(self, k):
        return self.sems[k]

    def ensure_sems(self):
        need = [("dma", s) for s in range(N_DMA_SEMS)]
        for e in ENGS:
            for ep in range(self.P.cnt[e] // EPOCH + 1):
                need.append((e, ep))
        for k in need:
            if k not in self.sems:
                self.sems[k] = self.ges.enter_context(self.nc.semaphore(f"s_{k[0]}_{k[1]}"))

    def dram(self, name, shape, dt, kind=None):
        if kind:
            return self.nc.dram_tensor(name, list(shape), dt, kind=kind).ap()
        return self.nc.dram_tensor(name, list(shape), dt).ap()

    def sb(self, es, shape, dt, name=None):
        self.uid += 1
        t = es.enter_context(self.nc.sbuf_tensor(f"{name or 't'}_{self.uid}", list(shape), dt))
        return Tl(t, self.P.buf(name))

    def ps(self, es, shape, dt, name=None):
        self.uid += 1
        t = es.enter_context(self.nc.psum_tensor(f"{name or 'p'}_{self.uid}", list(shape), dt))
        return Tl(t, self.P.buf(name))

    @staticmethod
    def _b(xs):
        return [x.b if isinstance(x, Tl) else x for x in xs]

    def mm(self, out, lhsT, rhs, start, stop, R, W):
        self.P.op("pe", lambda e: e.matmul(out, lhsT=lhsT, rhs=rhs, start=start, stop=stop), self._b(R), self._b(W))

    def tr(self, out, in_, ident, R, W):
        self.P.op("pe", lambda e: e.transpose(out, in_, ident), self._b(R), self._b(W))

    def act(self, out, in_, func, R, W, bias=None, scale=None):
        kw = {}
        if bias is not None:
            kw["bias"] = bias
        if scale is not None:
            kw["scale"] = scale
        self.P.op("act", lambda e: e.activation(out=out, in_=in_, func=func, **kw), self._b(R), self._b(W))

    def tt(self, eng, out, in0, in1, op, R, W):
        self.P.op(eng, lambda e: e.tensor_tensor(out=out, in0=in0, in1=in1, op=op), self._b(R), self._b(W))

    def ts(self, eng, out, in0, s1, s2, op0, op1, R, W):
        if s2 is None:
            self.P.op(eng, lambda e: e.tensor_scalar(out=out, in0=in0, scalar1=s1, scalar2=None, op0=op0), self._b(R), self._b(W))
        else:
            self.P.op(eng, lambda e: e.tensor_scalar(out=out, in0=in0, scalar1=s1, scalar2=s2, op0=op0, op1=op1), self._b(R), self._b(W))

    def stt(self, eng, out, in0, scalar, in1, op0, op1, R, W):
        eng = "dve"
        self.P.op(eng, lambda e: e.scalar_tensor_tensor(out=out, in0=in0, scalar=scalar, in1=in1, op0=op0, op1=op1), self._b(R), self._b(W))

    def cp(self, eng, out, in_, R, W):
        if eng == "act":
            self.P.op("act", lambda e: e.activation(out=out, in_=in_, func=AF.Copy), self._b(R), self._b(W))
        else:
            self.P.op(eng, lambda e: e.tensor_copy(out=out, in_=in_), self._b(R), self._b(W))

    def recip(self, out, in_, R, W):
        self.P.op("dve", lambda e: e.reciprocal(out=out, in_=in_), self._b(R), self._b(W))

    def memset(self, eng, ap, val, W):
        self.P.op(eng, lambda e: e.memset(ap, val), [], self._b(W))

    def dma(self, eng, out, in_, R, W):
        self.P.op(eng, lambda e: e.dma_start(out=out, in_=in_), self._b(R), self._b(W), dma=True)

    def emit_phase(self, final=False):
        P = self.P
        P.barrier()
        P.finalize()
        self.ensure_sems()
        nc = self.nc
        with nc.Block() as block:
            @block.tensor
            def _(e):
                P.replay("pe", e, self.get_sem)

            @block.scalar
            def _(e):
                P.replay("act", e, self.get_sem)

            @block.vector
            def _(e):
                P.replay("dve", e, self.get_sem)

            @block.gpsimd
            def _(e):
                P.replay("pool", e, self.get_sem)

            @block.sync
            def _(e):
                P.replay("sp", e, self.get_sem, final=final)


def bcast_free(tl_ap_tensor, offset, pstride, nparts, n):
    return bass.AP(tl_ap_tensor, offset, [[pstride, nparts], [0, n]])


def slabA(w, kchunks):
    K, Fd = w.shape
    return np.ascontiguousarray(w.reshape(kchunks, 128, Fd // 128, 128).transpose(2, 1, 0, 3))


def slabB(w, kchunks, ncol):
    K, Fd = w.shape
    return np.ascontiguousarray(w.reshape(kchunks, 128, Fd // ncol, ncol).transpose(2, 1, 0, 3))


def pvec(v):
    return np.ascontiguousarray(v.reshape(-1, 128).T)


def t5_buckets_np(n):
    dist = np.arange(n, dtype=np.int32)
    max_exact = 16
    ratio = np.maximum(dist, max_exact).astype(np.float32) / np.float32(max_exact)
    large = max_exact + (np.log(ratio).astype(np.float32) / np.float32(math.log(128 / 16)) * np.float32(16)).astype(np.int32)
    large = np.minimum(large, 31)
    return np.where(dist < max_exact, dist, large)


VEC_COLS = {}
_vc = 0


def _vadd(name, n):
    global _vc
    VEC_COLS[name] = (_vc, n)
    _vc += n


for _l in range(DEPTH):
    _vadd(f"nmix{_l}", 8)
    _vadd(f"nffn{_l}", 8)
_vadd("nfinal", 8)
for _i in range(2):
    _vadd(f"subln{_i}", 1)
    _vadd(f"lam{_i}", 4)
    _vadd(f"mu_rkv{_i}", 12)
    _vadd(f"mu_wag{_i}", 24)
    _vadd(f"a0{_i}", 4)
    _vadd(f"k_k{_i}", 4)
    _vadd(f"k_a{_i}", 4)
    _vadd(f"r_k{_i}", 4)
    _vadd(f"ln_w{_i}", 4)
    _vadd(f"ln_b{_i}", 4)
    _vadd(f"gnorm{_i}", 4)
_vadd("bfar", 8)
NVEC = _vc


def pack_vecs(inp):
    v = np.zeros((128, NVEC), np.float32)

    def put(name, arr):
        c, n = VEC_COLS[name]
        v[:, c:c + n] = arr

    for l in range(DEPTH):
        put(f"nmix{l}", pvec(inp["norm_mix"][l]))
        put(f"nffn{l}", pvec(inp["norm_ffn"][l]))
    put("nfinal", pvec(inp["norm_final"]))
    for i in range(2):
        put(f"subln{i}", inp["da_subln"][i].reshape(128, 1))
        lam = np.zeros((128, 4), np.float32)
        for j, nm in enumerate(["da_lam_q1", "da_lam_k1", "da_lam_q2", "da_lam_k2"]):
            lam[:64, j] = inp[nm][i]
        put(f"lam{i}", lam)
        put(f"mu_rkv{i}", pvec(inp["rw_mu_rkv"][i].reshape(-1)))
        put(f"mu_wag{i}", pvec(inp["rw_mu_wag"][i].reshape(-1)))
        put(f"a0{i}", pvec(inp["rw_a0"][i]))
        put(f"k_k{i}", pvec(inp["rw_k_k"][i]))
        put(f"k_a{i}", pvec(inp["rw_k_a"][i]))
        put(f"r_k{i}", pvec(inp["rw_r_k"][i].reshape(-1)))
        put(f"ln_w{i}", pvec(inp["rw_ln_w"][i]))
        put(f"ln_b{i}", pvec(inp["rw_ln_b"][i]))
        put(f"gnorm{i}", pvec(inp["gla_norm"][i]))
    bk = t5_buckets_np(T)
    assert (bk[113:] == bk[113]).all()
    put("bfar", np.broadcast_to(inp["rel_bias"][bk[200]][None, :], (128, 8)))
    return v


def pack_shared(inp):
    sh = {}
    w_in = [inp["ab_w_in"][0], inp["da_w_qkv"][0], inp["ab_w_in"][1], inp["da_w_qkv"][1]]
    w_out = [inp["ab_w_out"][0], inp["da_w_out"][0], inp["ab_w_out"][1], inp["da_w_out"][1]]
    sh["w_inA"] = np.stack([slabA(w, 8) for w in w_in])
    sh["w_inB"] = np.stack([slabB(w, 8, 512) for w in w_in])
    sh["w_out"] = np.stack([slabA(w, 8) for w in w_out])
    gu = []
    for l in range(DEPTH):
        g = slabA(inp["ffn_w_gate"][l], 8)
        u = slabA(inp["ffn_w_up"][l], 8)
        gu.append(np.concatenate([g, u], axis=3))
    sh["w_gu"] = np.stack(gu)
    sh["w_dn"] = np.stack([slabA(inp["ffn_w_down"][l], NFC) for l in range(DEPTH)])
    sh["vecs"] = pack_vecs(inp)
    bk = t5_buckets_np(T)
    bbd = inp["rel_bias"][bk]
    idx = np.arange(1024)[None, :] - np.arange(128)[:, None] - 384
    G = np.full((8, 128, 1024), NEG, np.float32)
    pos = idx >= 0
    for h in range(8):
        G[h][pos] = bbd[idx[pos], h]
    sh["biasG"] = np.ascontiguousarray(G.transpose(1, 0, 2))
    l1 = []
    for i in range(2):
        cat = np.concatenate([inp["rw_w1"][i], inp["rw_a1"][i], inp["rw_g1"][i], inp["gla_wa1"][i]], axis=1)
        l1.append(np.ascontiguousarray(cat.reshape(8, 128, 304).transpose(1, 0, 2)))
    sh["lora1"] = np.stack(l1)
    l2 = np.zeros((2, 128, 5, 512), np.float32)
    for i in range(2):
        l2[i, 0:64, 0, :] = inp["rw_w2"][i]
        l2[i, 64:128, 1, :] = inp["rw_a2"][i]
        l2[i, :, 2, :] = inp["rw_g2"][i][0:128]
        l2[i, 0:32, 3, :] = inp["rw_g2"][i][128:160]
        l2[i, 32:48, 4, 0:256] = inp["gla_wa2"][i]
    sh["lora2"] = l2
    rows = np.zeros((2, 1, 768), np.float32)
    for i in range(2):
        rows[i, 0, 0:512] = inp["rw_w0"][i]
        rows[i, 0, 512:768] = inp["gla_ba"][i]
    sh["rows"] = rows
    return sh


def flat2d(ap, n_elems, maxcols=8192):
    raise NotImplementedError


class Kern(Builder):
    def __init__(self, layers, t_len=T, dbg=False):
        super().__init__(layers, t_len)
        self.dbg = dbg
        nc = self.nc
        Tn = self.T
        self.xT = self.dram("xT", [D, Tn], F32, "ExternalInput")
        shp = {
            "w_inA": [4, 24, 128, 8, 128], "w_inB": [4, 6, 128, 8, 512], "w_out": [4, 8, 128, 8, 128],
            "w_gu": [4, NFC, 128, 8, 256], "w_dn": [4, 8, 128, NFC, 128],
        }
        self.wf = {k: self.dram(k, s, F32, "ExternalInput") for k, s in shp.items()}
        self.wb = {k: self.dram(k + "_bf", s, BF16) for k, s in shp.items()}
        self.wshape = shp
        self.vecs_d = self.dram("vecs", [128, NVEC], F32, "ExternalInput")
        self.biasG_d = self.dram("biasG", [128, 8, 1024], F32, "ExternalInput")
        self.lora1_d = self.dram("lora1", [2, 128, 8, 304], F32, "ExternalInput")
        self.lora2_d = self.dram("lora2", [2, 128, 5, 512], F32, "ExternalInput")
        self.rows_d = self.dram("rows", [2, 1, 768], F32, "ExternalInput")
        self.yT = self.dram("yT", [D, Tn], F32, "ExternalOutput")
        self.x_s = self.dram("x_s", [D, Tn], F32)
        self.mix_s = self.dram("mix_s", [D, Tn], BF16)
        self.q_s = self.dram("q_s", [8, 128, Tn], BF16)
        self.k_s = self.dram("k_s", [8, 128, Tn], BF16)
        self.v_s = self.dram("v_s", [Tn, D], BF16)
        if dbg:
            self.dbg_mix = self.dram("dbg_mix", [D, Tn], BF16, "ExternalOutput")
        self.b_x = self.P.buf("x_dram")
        self.b_mix = self.P.buf("mix_dram")
        self.b_qkv = self.P.buf("qkv_dram")
        self.b_wl = [self.P.buf(f"w_dram{l}") for l in range(DEPTH)]

    def cast_layer(self, l):
        for k, s in self.wshape.items():
            n = int(np.prod(s[1:]))
            cols = n // 128
            src = self.wf[k][l].rearrange("a p k j -> (a p k j)").rearrange("(r c) -> r c", r=128) if False else None
            ft = self.wf[k].tensor
            bt = self.wb[k].tensor
            base = l * n
            piece = 8192
            c0 = 0
            while c0 < cols:
                cc = min(piece, cols - c0)
                src = bass.AP(ft, base + c0, [[cols, 128], [1, cc]])
                dst = bass.AP(bt, base + c0, [[cols, 128], [1, cc]])
                self.dma("pool", dst, src, [], [self.b_wl[l]])
                c0 += cc

    def setup(self):
        nc = self.nc
        es = self.ges
        self.ident = self.sb(es, [128, 128], BF16, "ident")
        self.ones = self.sb(es, [128, 128], BF16, "ones")
        self.vecs = self.sb(es, [128, NVEC], F32, "vecs")
        self.vx = self.sb(es, [128, 64], F32, "vx")
        self.memset("pool", self.ident.t[:], 0.0, [self.ident])
        self.P.op("pool", lambda e: e.affine_select(out=self.ident.t[:], in_=self.ident.t[:], pattern=[[-1, 128]],
                                                    compare_op=ALU.not_equal, fill=1.0, base=0, channel_multiplier=1),
                  [self.ident.b], [self.ident.b])
        self.memset("pool", self.ones.t[:], 1.0, [self.ones])
        self.dma("sp", self.vecs.t[:], self.vecs_d, [], [self.vecs])
        self.cast_layer(self.layers[0])
        self.emit_phase()

    def vcol(self, name, j=0, n=1):
        c, _ = VEC_COLS[name]
        return self.vecs.t[:, c + j:c + j + n]

    def rmsnorm(self, xt, hT, hoff, TT, gsc, sq, pbank, rstd, eps, out_dt_f32=None):
        for kc in range(8):
            self.act(sq.t[:, kc, :], xt.t[:, kc, :], AF.Square, [xt], [sq])
        for kc in range(8):
            self.mm(pbank.t[:, 0:TT], self.ones.t[:], sq.t[:, kc, :], kc == 0, kc == 7, [self.ones, sq], [pbank])
        self.act(rstd.t[:, 0:TT], pbank.t[:, 0:TT], AF.Ln, [pbank], [rstd], bias=self.epsD(eps), scale=1.0)
        self.act(rstd.t[:, 0:TT], rstd.t[:, 0:TT], AF.Exp, [rstd], [rstd], scale=-0.5)
        for kc in range(8):
            eng = "dve" if kc % 2 == 0 else "pool"
            self.stt(eng, hT.t[:, kc, hoff:hoff + TT], xt.t[:, kc, :], gsc[:, kc:kc + 1], rstd.t[:, 0:TT],
                     ALU.mult, ALU.mult, [xt, rstd, self.vx], [hT])

    def epsD(self, eps):
        return float(D * eps)

    def phase_ffn(self, l, x_src, last, next_layer):
        TT = 512
        NT = self.T // TT
        sqD = math.sqrt(D)
        with ExitStack() as es:
            xt = [self.sb(es, [128, 8, TT], F32, "xt") for _ in range(2)]
            mt = [self.sb(es, [128, 8, TT], BF16, "mt") for _ in range(2)]
            hT = self.sb(es, [128, 8, TT], BF16, "hT")
            sq = self.sb(es, [128, 8, TT], BF16, "sq")
            rstd = self.sb(es, [128, TT], F32, "rstd")
            actb = self.sb(es, [128, NFC, TT], BF16, "actb")
            wgu = [self.sb(es, [128, 8, 256], BF16, "wgu") for _ in range(5)]
            wdn = [self.sb(es, [128, NFC, 128], BF16, "wdn") for _ in range(8)]
            sq2 = self.sb(es, [128, 8, TT], BF16, "sq2") if last else None
            rstd2 = self.sb(es, [128, TT], F32, "rstd2") if last else None
            wo = [self.sb(es, [128, 8, 128], BF16, "wo") for _ in range(8)]
            sg = [self.sb(es, [128, TT], BF16, "sg") for _ in range(2)]
            for dc in range(8):
                self.dma("sp", wo[dc].t[:], self.wb["w_out"][l, dc], [self.b_wl[l]], [wo[dc]])
            for dc in range(8):
                self.dma("sp", wdn[dc].t[:], self.wb["w_dn"][l, dc], [self.b_wl[l]], [wdn[dc]])
            pb = [self.ps(es, [128, 512], F32, "pb") for _ in range(8)]
            pbi = [0]

            def bank():
                b = pb[pbi[0] % 8]
                pbi[0] += 1
                return b

            c_ffn, _ = VEC_COLS[f"nffn{l}"]
            self.ts("dve", self.vx.t[:, 0:8], self.vecs.t[:, c_ffn:c_ffn + 8], sqD, None, ALU.mult, None, [self.vecs], [self.vx])
            if last:
                c_fin, _ = VEC_COLS["nfinal"]
                self.ts("dve", self.vx.t[:, 8:16], self.vecs.t[:, c_fin:c_fin + 8], sqD, None, ALU.mult, None, [self.vecs], [self.vx])

            xsrc_v = x_src.rearrange("(kc p) t -> p kc t", p=128)
            xs_v = self.x_s.rearrange("(kc p) t -> p kc t", p=128)
            y_v = self.yT.rearrange("(kc p) t -> p kc t", p=128)
            mix_v = self.mix_s.rearrange("(kc p) t -> p kc t", p=128)
            wi = [0, 0, 0]

            def load_tile(i):
                t0 = i * TT
                self.dma("pool", xt[i % 2].t[:], xsrc_v[:, :, t0:t0 + TT], [self.b_x], [xt[i % 2]])
                self.dma("pool", mt[i % 2].t[:], mix_v[:, :, t0:t0 + TT], [self.b_mix], [mt[i % 2]])

            def outproj(i):
                x = xt[i % 2]
                m = mt[i % 2]
                for dc in range(8):
                    w = wo[dc]
                    p = bank()
                    for kc in range(8):
                        self.mm(p.t[:, 0:TT], w.t[:, kc, :], m.t[:, kc, :], kc == 0, kc == 7, [w, m], [p])
                    self.tt("dve", x.t[:, dc, :], x.t[:, dc, :], p.t[:, 0:TT], ALU.add, [x, p], [x])
                for kc in range(8):
                    self.act(sq.t[:, kc, :], x.t[:, kc, :], AF.Square, [x], [sq])

            def norm_rest(i):
                x = xt[i % 2]
                p = bank()
                for kc in range(8):
                    self.mm(p.t[:, 0:TT], self.ones.t[:], sq.t[:, kc, :], kc == 0, kc == 7, [self.ones, sq], [p])
                self.act(rstd.t[:, 0:TT], p.t[:, 0:TT], AF.Ln, [p], [rstd], bias=self.epsD(1e-6), scale=1.0)
                self.act(rstd.t[:, 0:TT], rstd.t[:, 0:TT], AF.Exp, [rstd], [rstd], scale=-0.5)
                for kc in range(8):
                    self.stt("dve", hT.t[:, kc, 0:TT], x.t[:, kc, :], self.vx.t[:, kc:kc + 1], rstd.t[:, 0:TT],
                             ALU.mult, ALU.mult, [x, rstd, self.vx], [hT])

            load_tile(0)
            if NT > 1:
                load_tile(1)
            outproj(0)
            norm_rest(0)
            for i in range(NT):
                t0 = i * TT
                x = xt[i % 2]
                for fc in range(NFC):
                    w = wgu[wi[1] % len(wgu)]
                    wi[1] += 1
                    self.dma("sp", w.t[:], self.wb["w_gu"][l, fc], [self.b_wl[l]], [w])
                    pg = bank()
                    pu = bank()
                    for kc in range(8):
                        self.mm(pg.t[:, 0:TT], w.t[:, kc, 0:128], hT.t[:, kc, :], kc == 0, kc == 7, [w, hT], [pg])
                    for kc in range(8):
                        self.mm(pu.t[:, 0:TT], w.t[:, kc, 128:256], hT.t[:, kc, :], kc == 0, kc == 7, [w, hT], [pu])
                    s_ = sg[fc % 2]
                    self.act(s_.t[:], pg.t[:, 0:TT], AF.Silu, [pg], [s_])
                    self.tt("dve", actb.t[:, fc, :], s_.t[:], pu.t[:, 0:TT], ALU.mult, [s_, pu], [actb])
                if i + 1 < NT:
                    outproj(i + 1)
                for dc in range(8):
                    if dc == 4 and i + 1 < NT:
                        norm_rest(i + 1)
                    w = wdn[dc]
                    p = bank()
                    for fc in range(NFC):
                        self.mm(p.t[:, 0:TT], w.t[:, fc, :], actb.t[:, fc, :], fc == 0, fc == NFC - 1, [w, actb], [p])
                    self.tt("dve", x.t[:, dc, :], x.t[:, dc, :], p.t[:, 0:TT], ALU.add, [x, p], [x])
                if not last:
                    self.dma("pool", xs_v[:, :, t0:t0 + TT], x.t[:], [x], [self.b_x])
                else:
                    p = bank()
                    self.rmsnorm(x, x, 0, TT, self.vx.t[:, 8:16], sq2, p, rstd2, 1e-6)
                    self.dma("pool", y_v[:, :, t0:t0 + TT], x.t[:], [x], [])
                if i + 2 < NT:
                    load_tile(i + 2)
            self.emit_phase(final=last)

    def phase_qkv(self, l, x_src):
        TT = 512
        NT = self.T // TT
        sqD = math.sqrt(D)
        with ExitStack() as es:
            xt = [self.sb(es, [128, 8, TT], F32, "xt") for _ in range(2)]
            hT = self.sb(es, [128, 8, TT], BF16, "hT")
            sq = self.sb(es, [128, 8, TT], BF16, "sq")
            rstd = self.sb(es, [128, TT], F32, "rstd")
            wa = [self.sb(es, [128, 8, 128], BF16, "wa") for _ in range(3)]
            wbs = [self.sb(es, [128, 8, 512], BF16, "wbs") for _ in range(2)]
            qst = [self.sb(es, [128, 8, TT], BF16, "qst") for _ in range(2)]
            kst = [self.sb(es, [128, 8, TT], BF16, "kst") for _ in range(2)]
            vst = [self.sb(es, [128, 4, D], BF16, "vst") for _ in range(2)]
            pb = [self.ps(es, [128, 512], F32, "pb") for _ in range(8)]
            pbi = [0]

            def bank():
                b = pb[pbi[0] % 8]
                pbi[0] += 1
                return b

            c_n, _ = VEC_COLS[f"nmix{l}"]
            self.ts("dve", self.vx.t[:, 16:24], self.vecs.t[:, c_n:c_n + 8], sqD, None, ALU.mult, None, [self.vecs], [self.vx])
            xsrc_v = x_src.rearrange("(kc p) t -> p kc t", p=128)
            q_v = self.q_s.rearrange("c p t -> p c t")
            k_v = self.k_s.rearrange("c p t -> p c t")
            v_v = self.v_s.rearrange("(tb p) f -> p tb f", p=128)
            wi = [0, 0]
            self.dma("sp", xt[0].t[:], xsrc_v[:, :, 0:TT], [self.b_x], [xt[0]])
            for i in range(NT):
                t0 = i * TT
                x = xt[i % 2]
                if i + 1 < NT:
                    self.dma("sp", xt[(i + 1) % 2].t[:], xsrc_v[:, :, t0 + TT:t0 + 2 * TT], [self.b_x], [xt[(i + 1) % 2]])
                p = bank()
                self.rmsnorm(x, hT, 0, TT, self.vx.t[:, 16:24], sq, p, rstd, 1e-6)
                qs = qst[i % 2]
                ks = kst[i % 2]
                vs = vst[i % 2]
                for c in range(16):
                    w = wa[wi[0] % 3]
                    wi[0] += 1
                    self.dma("sp", w.t[:], self.wb["w_inA"][l, c], [self.b_wl[l]], [w])
                    p = bank()
                    for kc in range(8):
                        self.mm(p.t[:, 0:TT], w.t[:, kc, :], hT.t[:, kc, :], kc == 0, kc == 7, [w, hT], [p])
                    if c < 8:
                        self.act(qs.t[:, c, :], p.t[:, 0:TT], AF.Copy, [p], [qs], scale=0.125)
                    else:
                        self.cp("dve", ks.t[:, c - 8, :], p.t[:, 0:TT], [p], [ks])
                for cg in range(2):
                    w = wbs[wi[1] % 2]
                    wi[1] += 1
                    self.dma("sp", w.t[:], self.wb["w_inB"][l, 4 + cg], [self.b_wl[l]], [w])
                    for tb in range(4):
                        p = bank()
                        for kc in range(8):
                            self.mm(p.t[:, :], hT.t[:, kc, tb * 128:(tb + 1) * 128], w.t[:, kc, :], kc == 0, kc == 7, [w, hT], [p])
                        if tb % 2 == 0:
                            self.cp("act", vs.t[:, tb, cg * 512:(cg + 1) * 512], p.t[:, :], [p], [vs])
                        else:
                            self.cp("dve", vs.t[:, tb, cg * 512:(cg + 1) * 512], p.t[:, :], [p], [vs])
                self.dma("pool", q_v[:, :, t0:t0 + TT], qs.t[:], [qs], [self.b_qkv])
                self.dma("pool", k_v[:, :, t0:t0 + TT], ks.t[:], [ks], [self.b_qkv])
                self.dma("pool", v_v[:, 4 * i:4 * i + 4, :], vs.t[:], [vs], [self.b_qkv])
            self.emit_phase()

    def phase_attn(self, l, next_layer=None):
        i_odd = l // 2
        lam_init = 0.8 - 0.6 * math.exp(-0.3 * l)
        Tn = self.T
        QB = 512
        NQ = Tn // QB
        NKB = Tn // 128
        with ExitStack() as es:
            qT = [self.sb(es, [128, Tn], BF16, "qT") for _ in range(2)]
            kT = [self.sb(es, [128, Tn], BF16, "kT") for _ in range(2)]
            vv = [self.sb(es, [128, NKB, 128], BF16, "vv") for _ in range(2)]
            Gf = self.sb(es, [128, 1024], F32, "Gf")
            Gb = self.sb(es, [128, 8, 1024], BF16, "Gb")
            pt = [[self.sb(es, [128, QB], BF16, "pt") for _ in range(3)] for _ in range(2)]
            pacc = [self.sb(es, [128, QB], F32, "pacc") for _ in range(2)]
            tmp = [self.sb(es, [128, QB], F32, "tmp") for _ in range(5)]
            sqb = self.sb(es, [128, QB], BF16, "sqb")
            ost = [self.sb(es, [128, QB], BF16, "ost") for _ in range(2)]
            lamt = self.sb(es, [128, 8], F32, "lamt")
            stp = [self.ps(es, [128, 512], F32, "st") for _ in range(6)]
            ot = [self.ps(es, [128, 512], F32, "ot") for _ in range(2)]
            sti = [0]
            st = {}

            if next_layer is not None:
                self.cast_layer(next_layer)
            for h in range(8):
                self.dma("sp", Gf.t[:], self.biasG_d[:, h, :], [], [Gf])
                self.cp("dve", Gb.t[:, h, :], Gf.t[:], [Gf], [Gb])
            c_l, _ = VEC_COLS[f"lam{i_odd}"]
            lv = self.vecs.t
            self.tt("dve", lamt.t[:, 0:1], lv[:, c_l:c_l + 1], lv[:, c_l + 1:c_l + 2], ALU.mult, [self.vecs], [lamt])
            self.tt("dve", lamt.t[:, 1:2], lv[:, c_l + 2:c_l + 3], lv[:, c_l + 3:c_l + 4], ALU.mult, [self.vecs], [lamt])
            self.memset("pool", tmp[0].t[:, 0:128], 1.0, [tmp[0]])
            self.mm(stp[0].t[:, 0:2], tmp[0].t[:, 0:128], lamt.t[:, 0:2], True, True, [tmp[0], lamt], [stp[0]])
            self.act(lamt.t[:, 2:4], stp[0].t[:, 0:2], AF.Exp, [stp[0]], [lamt])
            onesf = self.sb(es, [128, 128], F32, "onesf")
            self.memset("pool", onesf.t[:], 1.0, [onesf])
            self.tt("dve", lamt.t[:, 4:5], lamt.t[:, 3:4], lamt.t[:, 2:3], ALU.subtract, [lamt], [lamt])
            self.ts("dve", lamt.t[:, 4:5], lamt.t[:, 4:5], -lam_init, None, ALU.add, None, [lamt], [lamt])
            c_s, _ = VEC_COLS[f"subln{i_odd}"]
            self.ts("dve", lamt.t[:, 5:6], lv[:, c_s:c_s + 1], (1.0 - lam_init) * math.sqrt(128.0), None, ALU.mult, None, [self.vecs], [lamt])
            c_bf, _ = VEC_COLS["bfar"]

            v_v = self.v_s.rearrange("(kb p) f -> p kb f", p=128)

            def load_head(h):
                self.dma("sp", qT[h % 2].t[:], self.q_s[h], [self.b_qkv], [qT[h % 2]])
                self.dma("sp", kT[h % 2].t[:], self.k_s[h], [self.b_qkv], [kT[h % 2]])
                self.dma("sp", vv[h % 2].t[:], v_v[:, :, h * 128:(h + 1) * 128], [self.b_qkv], [vv[h % 2]])

            load_head(0)
            ep = 0
            for h in range(8):
                if h + 1 < 8:
                    load_head(h + 1)
                q = qT[h % 2]
                k = kT[h % 2]
                v = vv[h % 2]
                bfar = lv[:, c_bf + h:c_bf + h + 1]
                for qb in range(NQ):
                    q0 = qb * QB
                    nkb = 4 * qb + 4
                    qsl = slice(q0, q0 + QB)

                    def issue_st(kb):
                        k0 = kb * 128
                        delta = k0 - q0
                        special = delta >= -128
                        for m in range(2):
                            s_ = stp[sti[0] % 6]
                            sti[0] += 1
                            st[(kb, m)] = s_
                            self.mm(s_.t[:, :], k.t[64 * m:64 * m + 64, k0:k0 + 128], q.t[64 * m:64 * m + 64, qsl],
                                    True, not special, [k, q], [s_])
                            if special:
                                j0 = 384 - delta
                                self.mm(s_.t[:, :], self.ident.t[:], Gb.t[:, h, j0:j0 + 512], False, True, [self.ident, Gb], [s_])
                        return special

                    spec = {}
                    for kb in range(min(2, nkb)):
                        spec[kb] = issue_st(kb)
                    for kb in range(nkb):
                        if kb + 2 < nkb:
                            spec[kb + 2] = issue_st(kb + 2)
                        for m in range(2):
                            s_ = st.pop((kb, m))
                            p_ = pt[m][kb % 3]
                            if spec[kb]:
                                self.act(p_.t[:], s_.t[:, :], AF.Exp, [s_], [p_])
                            else:
                                self.act(p_.t[:], s_.t[:, :], AF.Exp, [s_, self.vecs], [p_], bias=bfar)
                            aeng = "pool" if ((2 * kb + m) % 3 == 2) else "dve"
                            if kb == 0:
                                self.cp(aeng, pacc[m].t[:], p_.t[:], [p_], [pacc[m]])
                            else:
                                self.tt(aeng, pacc[m].t[:], pacc[m].t[:], p_.t[:], ALU.add, [p_, pacc[m]], [pacc[m]])
                            self.mm(ot[m].t[:, :], v.t[:, kb, :], p_.t[:], kb == 0, kb == nkb - 1, [v, p_], [ot[m]])
                    lt = []
                    for m in range(2):
                        l_ = stp[sti[0] % 6]
                        sti[0] += 1
                        self.mm(l_.t[:, :], onesf.t[:], pacc[m].t[:], True, True, [onesf, pacc[m]], [l_])
                        lt.append(l_)
                    r0, r1, o0, o1, oo = tmp
                    self.recip(r1.t[:], lt[1].t[:, :], [lt[1]], [r1])
                    self.tt("dve", r0.t[:], lt[0].t[:, :], r1.t[:], ALU.mult, [lt[0], r1], [r0])
                    self.tt("dve", o1.t[:], ot[1].t[:, :], r0.t[:], ALU.mult, [ot[1], r0], [o1])
                    self.stt("dve", oo.t[:], o1.t[:], lamt.t[:, 4:5], ot[0].t[:, :], ALU.mult, ALU.add, [o1, ot[0], lamt], [oo])
                    self.act(sqb.t[:], oo.t[:], AF.Square, [oo], [sqb])
                    self.act(o0.t[:], lt[0].t[:, :], AF.Square, [lt[0]], [o0], scale=math.sqrt(128 * 1e-5))
                    pss = stp[sti[0] % 6]
                    sti[0] += 1
                    self.mm(pss.t[:, :], self.ones.t[:], sqb.t[:], True, True, [self.ones, sqb], [pss])
                    self.tt("dve", r1.t[:], pss.t[:, :], o0.t[:], ALU.add, [pss, o0], [r1])
                    self.act(r1.t[:], r1.t[:], AF.Ln, [r1], [r1])
                    self.act(r1.t[:], r1.t[:], AF.Exp, [r1], [r1], scale=-0.5)
                    os_ = ost[ep % 2]
                    ep += 1
                    self.stt("dve", os_.t[:], oo.t[:], lamt.t[:, 5:6], r1.t[:], ALU.mult, ALU.mult, [oo, lamt, r1], [os_])
                    self.dma("pool", self.mix_s[h * 128:(h + 1) * 128, q0:q0 + QB], os_.t[:], [os_], [self.b_mix])
            self.emit_phase()

    def build(self):
        self.setup()
        x_src = self.xT
        n = len(self.layers)
        for j, l in enumerate(self.layers):
            last = j == n - 1
            nxt = None if last else self.layers[j + 1]
            if l % 2 == 0:
                self.phase_even(l, x_src, nxt)
            else:
                self.phase_qkv(l, x_src)
                self.phase_attn(l, nxt)
            if self.dbg and j == 0:
                self.dma("sp", self.dbg_mix, self.mix_s, [self.b_mix], [])
            self.phase_ffn(l, x_src, last, nxt)
            x_src = self.x_s
        return self.nc


_CACHE = {}


def run(inputs, layers=(0, 1, 2, 3), n_cores=8, dbg=False, trace=False):
    Tn = inputs["x"].shape[1]
    key = (tuple(layers), Tn, dbg)
    if key not in _CACHE:
        kb = Kern(list(layers), Tn, dbg)
        _CACHE[key] = kb.build()
    nc = _CACHE[key]
    sh = pack_shared(inputs)
    in_maps = []
    for b in range(n_cores):
        m = dict(sh)
        m["xT"] = np.ascontiguousarray(inputs["x"][b].T)
        in_maps.append(m)
    res = run_bass_kernel_spmd(nc, in_maps, core_ids=list(range(n_cores)), trace=trace)
    return res


def kernel(**inputs):
    inputs = {k: np.asarray(v) for k, v in inputs.items()}
    res = run(inputs)
    out = np.stack([np.ascontiguousarray(r["yT"].T) for r in res.results], axis=0)
    return out.astype(np.float32)


CDEC = math.exp(-0.5)


def _even_consts(self, es):
    c = {}

    def mask(name, kind, val, dt=F32):
        t = self.sb(es, [128, 128], dt, name)
        self.memset("pool", t.t[:], val, [t])
        if kind == "u_incl":
            pat, cm, op = [[1, 128]], -1, ALU.is_ge
            z = (slice(0, 64), slice(64, 128))
        elif kind == "u_strict":
            pat, cm, op = [[1, 128]], -1, ALU.is_gt
            z = (slice(0, 64), slice(64, 128))
        else:
            pat, cm, op = [[-1, 128]], 1, ALU.is_gt
            z = (slice(64, 128), slice(0, 64))
        self.P.op("pool", lambda e: e.affine_select(out=t.t[:], in_=t.t[:], pattern=pat, compare_op=op, fill=0.0,
                                                    base=0, channel_multiplier=cm), [t.b], [t.b])
        self.memset("pool", t.t[z[0], z[1]], 0.0, [t])
        c[name] = t
        return t

    mask("MleF", "u_incl", -CDEC)
    mask("MltF", "u_strict", -CDEC)
    mask("MgtF", "l_strict", -CDEC)
    mask("MleG", "u_incl", 1.0 / 16.0)
    mask("MgtG", "l_strict", 1.0 / 16.0)
    ui = mask("UI", "u_incl", 1.0, BF16)
    us = mask("US", "u_strict", 1.0, BF16)
    ls = mask("LS", "l_strict", 1.0, BF16)
    for nm, src in (("UI4", ui), ("US4", us), ("LS4", ls)):
        t = self.sb(es, [128, 4, 128], BF16, nm)
        for r in range(4):
            self.cp("pool", t.t[:, r, :], src.t[:], [src], [t])
        c[nm] = t
    t = self.sb(es, [128, 8, 128], BF16, "identrep")
    for r in range(8):
        self.cp("pool", t.t[:, r, :], self.ident.t[:], [self.ident], [t])
    c["identrep"] = t
    ob = self.sb(es, [128, 128], BF16, "onesblk")
    self.memset("pool", ob.t[:], 1.0, [ob])
    self.memset("pool", ob.t[0:64, 64:128], 0.0, [ob])
    self.memset("pool", ob.t[64:128, 0:64], 0.0, [ob])
    c["onesblk"] = ob
    return c


Kern._even_consts = _even_consts


def phase_even(self, l, x_src, next_layer=None):
    i_ev = l // 2
    TT = 256
    NT = self.T // TT
    sqD = math.sqrt(D)
    V = lambda name, j=0, n=1: self.vcol(f"{name}{i_ev}", j, n)
    with ExitStack() as es0:
        C = self._even_consts(es0)
        L1c = self.sb(es0, [128, 8, 304], BF16, "L1c")
        L1p = self.sb(es0, [128, 8, 304], BF16, "L1p")
        with ExitStack() as es:
            L1f = self.sb(es, [128, 8, 304], F32, "L1f")
            L1pf = self.sb(es, [128, 8, 304], F32, "L1pf")
            self.dma("sp", L1f.t[:], self.lora1_d[i_ev], [], [L1f])
            self.memset("pool", L1pf.t[:, :, 288:304], 0.0, [L1pf])
            for kc in range(8):
                for (c0, c1, mj) in ((0, 64, 0), (64, 128, 8), (128, 288, 16)):
                    eng = "dve" if kc % 2 == 0 else "pool"
                    self.ts(eng, L1pf.t[:, kc, c0:c1], L1f.t[:, kc, c0:c1], V("mu_wag", mj + kc), None, ALU.mult, None,
                            [L1f, self.vecs], [L1pf])
            self.cp("act", L1p.t[:], L1pf.t[:], [L1pf], [L1p])
            self.tt("dve", L1c.t[:], L1f.t[:], L1pf.t[:], ALU.subtract, [L1f, L1pf], [L1c])
            c_n, _ = VEC_COLS[f"nmix{l}"]
            self.ts("dve", self.vx.t[:, 16:24], self.vecs.t[:, c_n:c_n + 8], sqD, None, ALU.mult, None, [self.vecs], [self.vx])
            self.ts("dve", self.vx.t[:, 24:36], V("mu_rkv", 0, 12), -1.0, 1.0, ALU.mult, ALU.add, [self.vecs], [self.vx])
            self.ts("dve", self.vx.t[:, 36:40], V("k_a", 0, 4), -1.0, 1.0, ALU.mult, ALU.add, [self.vecs], [self.vx])
            self.emit_phase()
        with ExitStack() as es:
            sb = lambda shape, dt, nm: self.sb(es, shape, dt, nm)
            xt = sb([128, 8, TT], F32, "xt")
            hT = sb([128, 8, TT + 2], BF16, "hT")
            sq = sb([128, 8, TT], BF16, "sq")
            rstd = sb([128, TT], F32, "rstd")
            wA = [sb([128, 8, 128], BF16, "wA") for _ in range(3)]
            wB3 = sb([128, 8, 256], BF16, "wB3")
            wB4 = sb([128, 8, 512], BF16, "wB4")
            L2f = sb([128, 5, 512], F32, "L2f")
            L2 = sb([128, 5, 512], BF16, "L2")
            rows = sb([128, 768], F32, "rows")
            pm = sb([128, 12, TT + 2], BF16, "pm")
            midA = sb([128, TT], BF16, "midA")
            midB = sb([128, TT], BF16, "midB")
            midC = sb([128, TT], BF16, "midC")
            sgw = sb([128, 2, 512], F32, "sgw")
            la = sb([128, 2, 256], F32, "la")
            ztmp = sb([128, 512], F32, "ztmp")
            g_ag = sb([128, 4, TT], F32, "g_ag")
            g_kk = sb([128, 4, TT], F32, "g_kk")
            g_rn = sb([128, 4, TT], F32, "g_rn")
            g_r = sb([128, 4, TT], BF16, "g_r")
            g_k = sb([128, 4, TT], BF16, "g_k")
            g_sq = sb([128, 4, TT], BF16, "g_sq")
            g_en = sb([128, 4, TT], BF16, "g_en")
            g_ex = sb([128, 4, TT], BF16, "g_ex")
            ecum = sb([128, 4, TT], F32, "ecum")
            bb = sb([128, 4, TT], BF16, "bb")
            kp = sb([128, 4, TT], BF16, "kp")
            vv = sb([128, 4, TT], BF16, "vv")
            rt = sb([128, 4, TT], BF16, "rt")
            at_ = sb([128, 4, TT], BF16, "at")
            bt = sb([128, 4, TT], BF16, "bt")
            kt = sb([128, 4, TT], BF16, "kt")
            Bh = sb([128, 2, 512], BF16, "Bh")
            Kh = sb([128, 2, 512], BF16, "Kh")
            Vt = sb([128, 2, 512], BF16, "Vt")
            etoend = sb([128, 2, 512], BF16, "etoend")
            bonus = sb([128, 4, TT], F32, "bonus")
            AabT = sb([128, 8, 128], BF16, "AabT")
            Aab = sb([128, 8, 128], BF16, "Aab")
            AakT = sb([128, 8, 128], BF16, "AakT")
            ArbT = sb([128, 8, 128], BF16, "ArbT")
            ArkT = sb([128, 8, 128], BF16, "ArkT")
            Pn = [sb([128, 8, 128], BF16, "Pn") for _ in range(2)]
            PTn = [sb([128, 8, 128], BF16, "PTn") for _ in range(2)]
            TTn = [sb([128, 8, 128], BF16, "TTn") for _ in range(2)]
            gq = sb([128, 2, TT], BF16, "gq")
            gk = sb([128, 2, TT], BF16, "gk")
            gKh = sb([128, 2, 256], BF16, "gKh")
            gV = sb([128, 2, 512], BF16, "gV")
            gsil = sb([128, 4, TT], BF16, "gsil")
            gecum = sb([128, 2, TT], F32, "gecum")
            gencum = sb([128, 2, TT], F32, "gencum")
            getoend = sb([128, 2, 256], F32, "getoend")
            gST = sb([128, 4, 128], BF16, "gST")
            yT = sb([128, 4, TT], F32, "yT")
            og = sb([128, 4, TT], F32, "og")
            mixo = sb([128, 8, TT], BF16, "mixo")
            Hf = sb([128, 4, 128], F32, "Hf")
            Hb = sb([128, 4, 128], BF16, "Hb")
            Xs = sb([128, 512], BF16, "Xs")
            Us = sb([128, 512], BF16, "Us")
            Sf = sb([128, 2, 256], F32, "Sf")
            Sb = sb([128, 2, 256], BF16, "Sb")
            pb = [self.ps(es, [128, 512], F32, "pb") for _ in range(6)]
            pbf = [self.ps(es, [128, 512], BF16, "pbf") for _ in range(2)]
            pbi = [0, 0]
            tfi = [0, 0]
            if getattr(self, "dbg_mem", False):
                try:
                    print("EVEN sbuf remaining:", self.nc.sbuf_bytes_remaining)
                except Exception as ex:
                    print("sbuf query failed", ex)

            def bank():
                b = pb[pbi[0] % 6]
                pbi[0] += 1
                return b

            def bankbf():
                b = pbf[pbi[1] % 2]
                pbi[1] += 1
                return b

            def TF():
                t = tf[tfi[0] % 8]
                tfi[0] += 1
                return t

            def TB():
                t = tb[tfi[1] % 8]
                tfi[1] += 1
                return t

            if next_layer is not None:
                self.cast_layer(next_layer)
            self.dma("sp", L2f.t[:], self.lora2_d[i_ev], [], [L2f])
            self.cp("act", L2.t[:], L2f.t[:], [L2f], [L2])
            self.dma("sp", rows.t[:], self.rows_d[i_ev].partition_broadcast(128), [], [rows])
            self.dma("sp", wB3.t[:], self.wb["w_inB"][l, 3, :, :, 256:512], [self.b_wl[l]], [wB3])
            self.dma("sp", wB4.t[:], self.wb["w_inB"][l, 4], [self.b_wl[l]], [wB4])
            self.memset("pool", hT.t[:, :, 0:2], 0.0, [hT])
            self.memset("pool", pm.t[:, :, 0:2], 0.0, [pm])
            for z in (Hf, Hb, Xs, Us, Sf, Sb):
                self.memset("pool", z.t[:], 0.0, [z])
            xsrc_v = x_src.rearrange("(kc p) t -> p kc t", p=128)
            mix_v = self.mix_s.rearrange("(kc p) t -> p kc t", p=128)
            wi = [0]
            self.dma("sp", xt.t[:], xsrc_v[:, :, 0:TT], [self.b_x], [xt])

            def slabA(c):
                w = wA[wi[0] % 3]
                wi[0] += 1
                self.dma("sp", w.t[:], self.wb["w_inA"][l, c], [self.b_wl[l]], [w])
                return w

            def proj_fm(c):
                w = slabA(c)
                p = bank()
                for kc in range(8):
                    self.mm(p.t[:, 0:TT], w.t[:, kc, :], hT.t[:, kc, 2:2 + TT], kc == 0, kc == 7, [w, hT], [p])
                return p

            stop = getattr(self, "even_stop", None)

            class _Stop(Exception):
                pass

            def chk(st):
                if stop == st:
                    raise _Stop()

            for i in range(NT):
              try:
                t0 = i * TT
                if i > 0:
                    self.cp("dve", hT.t[:, :, 1:2], hT.t[:, :, TT + 1:TT + 2], [hT], [hT])
                    self.cp("dve", pm.t[:, :, 1:2], pm.t[:, :, TT + 1:TT + 2], [pm], [pm])
                p = bank()
                self.rmsnorm(xt, hT, 2, TT, self.vx.t[:, 16:24], sq, p, rstd, 1e-6)
                if i + 1 < NT:
                    self.dma("sp", xt.t[:], xsrc_v[:, :, t0 + TT:t0 + 2 * TT], [self.b_x], [xt])
                chk("B")
                for (mid, c0, c1) in ((midA, 0, 128), (midB, 128, 256), (midC, 256, 304)):
                    m = c1 - c0
                    p = bank()
                    for kc in range(8):
                        self.mm(p.t[0:m, 0:TT], L1c.t[:, kc, c0:c1], hT.t[:, kc, 2:2 + TT], kc == 0, False, [L1c, hT], [p])
                    for kc in range(8):
                        self.mm(p.t[0:m, 0:TT], L1p.t[:, kc, c0:c1], hT.t[:, kc, 1:1 + TT], False, kc == 7, [L1p, hT], [p])
                    if mid is midA:
                        self.act(mid.t[0:64, :], p.t[0:64, 0:TT], AF.Tanh, [p], [mid])
                        self.cp("dve", mid.t[64:128, :], p.t[64:128, 0:TT], [p], [mid])
                    elif mid is midB:
                        self.act(mid.t[:, :], p.t[:, 0:TT], AF.Sigmoid, [p], [mid])
                    else:
                        self.act(mid.t[0:32, :], p.t[0:32, 0:TT], AF.Sigmoid, [p], [mid])
                        self.cp("dve", mid.t[32:48, :], p.t[32:48, 0:TT], [p], [mid])
                chk("C")
                for b in range(2):
                    bs = slice(b * 128, (b + 1) * 128)
                    p = bank()
                    self.mm(p.t[:, :], midA.t[0:64, bs], L2.t[0:64, 0, :], True, True, [midA, L2], [p])
                    self.tt("dve", ztmp.t[:], p.t[:, :], rows.t[:, 0:512], ALU.add, [p, rows], [ztmp])
                    self.act(sgw.t[:, b, :], ztmp.t[:], AF.Sigmoid, [ztmp], [sgw])
                    p = bank()
                    self.mm(p.t[:, 0:256], midC.t[32:48, bs], L2.t[32:48, 4, 0:256], True, True, [midC, L2], [p])
                    self.tt("dve", ztmp.t[:, 0:256], p.t[:, 0:256], rows.t[:, 512:768], ALU.add, [p, rows], [ztmp])
                    self.act(ztmp.t[:, 256:512], ztmp.t[:, 0:256], AF.Sigmoid, [ztmp], [ztmp])
                    self.act(la.t[:, b, :], ztmp.t[:, 256:512], AF.Ln, [ztmp], [la])
                chk("D")
                for b in range(2):
                    p = bank()
                    self.mm(p.t[:, :], C["MgtF"].t[:], sgw.t[:, b, :], True, True, [C["MgtF"], sgw], [p])
                    self.act(etoend.t[:, b, :], p.t[:, :], AF.Exp, [p], [etoend])
                    p = bank()
                    self.mm(p.t[:, 0:256], C["MgtG"].t[:], la.t[:, b, :], True, True, [C["MgtG"], la], [p])
                    self.act(getoend.t[:, b, :], p.t[:, 0:256], AF.Exp, [p], [getoend])
                for fg in range(2):
                    p = bank()
                    for b in range(2):
                        self.mm(p.t[:, b * 128:(b + 1) * 128], la.t[:, b, fg * 128:(fg + 1) * 128], C["MleG"].t[:], True, True,
                                [la, C["MleG"]], [p])
                    self.act(gecum.t[:, fg, :], p.t[:, 0:TT], AF.Exp, [p], [gecum])
                    self.act(gencum.t[:, fg, :], p.t[:, 0:TT], AF.Exp, [p], [gencum], scale=-1.0)
                chk("E")
                def stage_F1():
                    for fg in range(2):
                        p = proj_fm(12 + fg)
                        self.stt("dve", gq.t[:, fg, :], p.t[:, 0:TT], 0.125, gecum.t[:, fg, :], ALU.mult, ALU.mult, [p, gecum], [gq])
                        p = proj_fm(14 + fg)
                        self.tt("dve", gk.t[:, fg, :], p.t[:, 0:TT], gencum.t[:, fg, :], ALU.mult, [p, gencum], [gk])

                def stage_F2():
                    for fc in range(4):
                        p = proj_fm(20 + fc)
                        self.act(gsil.t[:, fc, :], p.t[:, 0:TT], AF.Silu, [p], [gsil])
                    for b in range(2):
                        p = bank()
                        for kc in range(8):
                            self.mm(p.t[:, 0:256], hT.t[:, kc, 2 + b * 128:2 + (b + 1) * 128], wB3.t[:, kc, :], kc == 0, kc == 7, [hT, wB3], [p])
                        self.tt("dve", gKh.t[:, b, :], p.t[:, 0:256], getoend.t[:, b, :], ALU.mult, [p, getoend], [gKh])
                        p = bank()
                        for kc in range(8):
                            self.mm(p.t[:, :], hT.t[:, kc, 2 + b * 128:2 + (b + 1) * 128], wB4.t[:, kc, :], kc == 0, kc == 7, [hT, wB4], [p])
                        self.cp("act", gV.t[:, b, :], p.t[:, :], [p], [gV])
                chk("F")
                FCS = range(4)
                for q3 in range(3):
                    for fc in FCS:
                        c = 4 * q3 + fc
                        p = proj_fm(c)
                        self.ts("dve", pm.t[:, c, 2:2 + TT], p.t[:, 0:TT], V("mu_rkv", c), None, ALU.mult, None, [p, self.vecs], [pm])
                        dst = (g_r, g_k, vv)[q3]
                        self.stt("dve", dst.t[:, fc, :], p.t[:, 0:TT], self.vx.t[:, 24 + c:25 + c], pm.t[:, c, 1:1 + TT], ALU.mult, ALU.add,
                                 [p, self.vx, pm], [dst])
                chk("G1")
                for fc in FCS:
                    p = bank()
                    self.mm(p.t[:, 0:TT], L2.t[64:128, 1, fc * 128:(fc + 1) * 128], midA.t[64:128, :], True, True, [L2, midA], [p])
                    self.act(g_ag.t[:, fc, :], p.t[:, 0:TT], AF.Sigmoid, [p, self.vecs], [g_ag], bias=V("a0", fc))
                for fc in FCS:
                    pc = bank()
                    for b in range(2):
                        self.mm(pc.t[:, b * 128:(b + 1) * 128], sgw.t[:, b, fc * 128:(fc + 1) * 128], C["MleF"].t[:], True, True,
                                [sgw, C["MleF"]], [pc])
                        self.mm(pc.t[:, 256 + b * 128:256 + (b + 1) * 128], sgw.t[:, b, fc * 128:(fc + 1) * 128], C["MltF"].t[:], True, True,
                                [sgw, C["MltF"]], [pc])
                    self.act(ecum.t[:, fc, :], pc.t[:, 0:TT], AF.Exp, [pc], [ecum])
                    self.act(g_en.t[:, fc, :], pc.t[:, 0:TT], AF.Exp, [pc], [g_en], scale=-1.0)
                    self.act(g_ex.t[:, fc, :], pc.t[:, 256:256 + TT], AF.Exp, [pc], [g_ex])
                for fc in FCS:
                    self.ts("pool", g_kk.t[:, fc, :], g_k.t[:, fc, :], V("k_k", fc), None, ALU.mult, None, [g_k, self.vecs], [g_kk])
                for fc in FCS:
                    self.act(g_sq.t[:, fc, :], g_kk.t[:, fc, :], AF.Square, [g_kk], [g_sq])
                pn = []
                for fc in FCS:
                    p = bank()
                    self.mm(p.t[:, 0:TT], C["onesblk"].t[:], g_sq.t[:, fc, :], True, True, [C["onesblk"], g_sq], [p])
                    pn.append(p)
                for fc in FCS:
                    self.act(g_rn.t[:, fc, :], pn[fc].t[:, 0:TT], AF.Ln, [pn[fc]], [g_rn], bias=1e-30, scale=1.0)
                self.act(g_rn.t[:], g_rn.t[:], AF.Exp, [g_rn], [g_rn], scale=-0.5)
                stage_F1()
                self.tt("dve", g_kk.t[:], g_kk.t[:], g_rn.t[:], ALU.mult, [g_kk, g_rn], [g_kk])
                for fc in FCS:
                    self.ts("pool", g_rn.t[:, fc, :], g_ag.t[:, fc, :], V("k_a", fc), self.vx.t[:, 36 + fc:37 + fc], ALU.mult, ALU.add,
                            [g_ag, self.vecs, self.vx], [g_rn])
                self.tt("pool", kp.t[:], g_k.t[:], g_rn.t[:], ALU.mult, [g_k, g_rn], [kp])
                self.tt("pool", bb.t[:], g_kk.t[:], g_ag.t[:], ALU.mult, [g_kk, g_ag], [bb])
                self.tt("dve", rt.t[:], g_r.t[:], ecum.t[:], ALU.mult, [g_r, ecum], [rt])
                self.stt("dve", at_.t[:], g_kk.t[:], -1.0, g_ex.t[:], ALU.mult, ALU.mult, [g_kk, g_ex], [at_])
                self.tt("pool", bt.t[:], bb.t[:], g_en.t[:], ALU.mult, [bb, g_en], [bt])
                self.tt("pool", kt.t[:], kp.t[:], g_en.t[:], ALU.mult, [kp, g_en], [kt])
                for fc in FCS:
                    self.stt("dve", g_sq.t[:, fc, :], g_r.t[:, fc, :], V("r_k", fc), kp.t[:, fc, :], ALU.mult, ALU.mult, [g_r, kp, self.vecs], [g_sq])
                stage_F2()
                pn = []
                for fc in FCS:
                    p = bank()
                    self.mm(p.t[:, 0:TT], C["onesblk"].t[:], g_sq.t[:, fc, :], True, True, [C["onesblk"], g_sq], [p])
                    pn.append(p)
                for fc in FCS:
                    self.tt("dve", bonus.t[:, fc, :], pn[fc].t[:, 0:TT], vv.t[:, fc, :], ALU.mult, [pn[fc], vv], [bonus])
                chk("G")
                for b in range(2):
                    bs = slice(b * 128, (b + 1) * 128)
                    for (src, dst, useE) in ((bb, Bh, True), (kp, Kh, True), (vv, Vt, False)):
                        p = bankbf()
                        for fc in range(4):
                            self.tr(p.t[:, fc * 128:(fc + 1) * 128], src.t[:, fc, bs], self.ident.t[:], [src, self.ident], [p])
                        if useE:
                            self.tt("dve", dst.t[:, b, :], p.t[:, :], etoend.t[:, b, :], ALU.mult, [p, etoend], [dst])
                        else:
                            self.cp("act", dst.t[:, b, :], p.t[:, :], [p], [dst])
                    chk("H")
                    for (dstS, lh, rh, msk) in ((AabT, bt, at_, "US4"), (Aab, at_, bt, "LS4"), (AakT, kt, at_, "US4"),
                                                (ArbT, bt, rt, "UI4"), (ArkT, kt, rt, "UI4")):
                        pg2 = [bank(), bank()]
                        for fc in range(4):
                            for g in range(2):
                                ro = 64 * g
                                self.mm(pg2[g].t[:, fc * 128:(fc + 1) * 128], lh.t[ro:ro + 64, fc, bs], rh.t[ro:ro + 64, fc, bs], True, True,
                                        [lh, rh], [pg2[g]])
                        for g in range(2):
                            self.tt("dve", dstS.t[:, g:8:2, :], pg2[g].t[:, :].rearrange("p (a b) -> p a b", a=4), C[msk].t[:], ALU.mult,
                                    [pg2[g], C[msk]], [dstS])
                    pg2 = [bank(), bank()]
                    for fg in range(2):
                        for g in range(2):
                            ro = 64 * g
                            self.mm(pg2[g].t[:, fg * 128:(fg + 1) * 128], gk.t[ro:ro + 64, fg, bs], gq.t[ro:ro + 64, fg, bs], True, True,
                                    [gk, gq], [pg2[g]])
                    for g in range(2):
                        self.tt("dve", gST.t[:, g:4:2, :], pg2[g].t[:, 0:256].rearrange("p (a b) -> p a b", a=2), C["UI4"].t[:, 0:2, :], ALU.mult,
                                [pg2[g], C["UI4"]], [gST])
                    chk("S")
                    Pc, PTc = Aab, AabT
                    TTc = TTn[0]
                    self.tt("pool", TTc.t[:], AabT.t[:], C["identrep"].t[:], ALU.add, [AabT, C["identrep"]], [TTc])
                    for it in range(5):
                        Pnew = Pn[it % 2]
                        PTnew = PTn[it % 2]
                        TTnew = TTn[(it + 1) % 2]
                        for g in range(2):
                            p = bank()
                            for hh in range(4):
                                h = 4 * g + hh
                                self.mm(p.t[:, hh * 128:(hh + 1) * 128], PTc.t[:, h, :], Pc.t[:, h, :], True, True, [PTc, Pc], [p])
                            self.cp("act", Pnew.t[:, 4 * g:4 * g + 4, :], p.t[:, :].rearrange("p (a b) -> p a b", a=4), [p], [Pnew])
                        if it < 4:
                            for g in range(2):
                                p = bank()
                                for hh in range(4):
                                    h = 4 * g + hh
                                    self.mm(p.t[:, hh * 128:(hh + 1) * 128], Pc.t[:, h, :], PTc.t[:, h, :], True, True, [PTc, Pc], [p])
                                self.cp("act", PTnew.t[:, 4 * g:4 * g + 4, :], p.t[:, :].rearrange("p (a b) -> p a b", a=4), [p], [PTnew])
                        for g in range(2):
                            p = bank()
                            for hh in range(4):
                                h = 4 * g + hh
                                self.mm(p.t[:, hh * 128:(hh + 1) * 128], Pnew.t[:, h, :], TTc.t[:, h, :], True, True, [Pnew, TTc], [p])
                            self.tt("dve", TTnew.t[:, 4 * g:4 * g + 4, :], p.t[:, :].rearrange("p (a b) -> p a b", a=4),
                                    TTc.t[:, 4 * g:4 * g + 4, :], ALU.add, [p, TTc], [TTnew])
                        Pc, PTc, TTc = Pnew, PTnew, TTnew
                    chk("I")
                    for cc in range(2):
                        tr0 = 64 * cc
                        trs = slice(tr0, tr0 + 64)
                        c0 = b * 128 + tr0
                        ccs = slice(c0, c0 + 64)
                        cend = c0 + 63
                        px = bank()
                        for h in range(8):
                            fc, j = h // 2, h % 2
                            self.mm(px.t[:, h * 64:(h + 1) * 64], at_.t[:, fc, bs], Hb.t[:, fc, 64 * j:64 * j + 64], True, False, [at_, Hb], [px])
                            self.mm(px.t[:, h * 64:(h + 1) * 64], AakT.t[:, h, :], Vt.t[:, b, h * 64:(h + 1) * 64], False, True, [AakT, Vt], [px])
                        self.cp("act", Xs.t[trs, :], px.t[trs, :], [px], [Xs])
                        pu = bank()
                        for h in range(8):
                            self.mm(pu.t[:, h * 64:(h + 1) * 64], TTc.t[:, h, :], Xs.t[:, h * 64:(h + 1) * 64], True, True, [TTc, Xs], [pu])
                        self.cp("act", Us.t[trs, :], pu.t[trs, :], [pu], [Us])
                        py = bank()
                        for fc in range(4):
                            ysl = py.t[:, fc * 64:(fc + 1) * 64]
                            self.mm(ysl, Hb.t[:, fc, :], rt.t[:, fc, ccs], True, False, [Hb, rt], [py])
                            for j in range(2):
                                h = 2 * fc + j
                                ysub = py.t[64 * j:64 * j + 64, fc * 64:(fc + 1) * 64]
                                self.mm(ysub, Us.t[:, h * 64:(h + 1) * 64], ArbT.t[:, h, trs], False, False, [Us, ArbT], [py])
                                self.mm(ysub, Vt.t[:, b, h * 64:(h + 1) * 64], ArkT.t[:, h, trs], False, j == 1, [Vt, ArkT], [py])
                        self.cp("act", yT.t[:, :, ccs], py.t[:, 0:256].rearrange("p (a b) -> p a b", a=4), [py], [yT])
                        pg_ = bank()
                        for h in range(4):
                            fg, j = h // 2, h % 2
                            osl = pg_.t[:, h * 64:(h + 1) * 64]
                            self.mm(osl, Sb.t[:, fg, j * 128:(j + 1) * 128], gq.t[:, fg, ccs], True, False, [Sb, gq], [pg_])
                            self.mm(osl, gV.t[:, b, h * 128:(h + 1) * 128], gST.t[:, h, trs], False, True, [gV, gST], [pg_])
                        self.cp("act", og.t[:, :, ccs], pg_.t[:, 0:256].rearrange("p (a b) -> p a b", a=4), [pg_], [og])
                        ph = bank()
                        for h in range(8):
                            fc, j = h // 2, h % 2
                            hsl = ph.t[:, fc * 128 + 64 * j:fc * 128 + 64 * j + 64]
                            self.mm(hsl, Bh.t[trs, b, fc * 128:(fc + 1) * 128], Us.t[trs, h * 64:(h + 1) * 64], True, False, [Bh, Us], [ph])
                            self.mm(hsl, Kh.t[trs, b, fc * 128:(fc + 1) * 128], Vt.t[trs, b, h * 64:(h + 1) * 64], False, True, [Kh, Vt], [ph])
                        for j in range(2):
                            rs = slice(64 * j, 64 * j + 64)
                            cs_ = slice(64 * j, 64 * j + 64)
                            self.P.op("dve", (lambda e, rs=rs, cs_=cs_, cend=cend:
                                              e.tensor_tensor(out=Hf.t[rs, :, cs_], in0=Hf.t[rs, :, cs_],
                                                              in1=bcast_last(ecum.t[rs, :, cend:cend + 1], 64), op=ALU.mult)),
                                      [ecum.b, Hf.b], [Hf.b])
                            self.tt("dve", Hf.t[rs, :, cs_], Hf.t[rs, :, cs_], ph.t[rs, :].rearrange("p (a b) -> p a b", a=4)[:, :, cs_], ALU.add,
                                    [ph, Hf], [Hf])
                            self.cp("pool", Hb.t[rs, :, cs_], Hf.t[rs, :, cs_], [Hf], [Hb])
                        psg = bank()
                        for h in range(4):
                            fg, j = h // 2, h % 2
                            self.mm(psg.t[:, h * 128:(h + 1) * 128], gKh.t[trs, b, fg * 128:(fg + 1) * 128], gV.t[trs, b, h * 128:(h + 1) * 128],
                                    True, True, [gKh, gV], [psg])
                        for j in range(2):
                            rs = slice(64 * j, 64 * j + 64)
                            cs_ = slice(128 * j, 128 * j + 128)
                            self.P.op("dve", (lambda e, rs=rs, cs_=cs_, cend=cend:
                                              e.tensor_tensor(out=Sf.t[rs, :, cs_], in0=Sf.t[rs, :, cs_],
                                                              in1=bcast_last(gecum.t[rs, :, cend:cend + 1], 128), op=ALU.mult)),
                                      [gecum.b, Sf.b], [Sf.b])
                            self.tt("dve", Sf.t[rs, :, cs_], Sf.t[rs, :, cs_], psg.t[rs, :].rearrange("p (a b) -> p a b", a=2)[:, :, cs_], ALU.add,
                                    [psg, Sf], [Sf])
                            self.cp("pool", Sb.t[rs, :, cs_], Sf.t[rs, :, cs_], [Sf], [Sb])
                chk("R")
                FCS = range(4)
                for fc in FCS:
                    self.cp("act", g_r.t[:, fc, :], yT.t[:, fc, :], [yT], [g_r])
                pn = []
                for fc in FCS:
                    p = bank()
                    self.mm(p.t[:, 0:TT], C["onesblk"].t[:], g_r.t[:, fc, :], True, True, [C["onesblk"], g_r], [p])
                    pn.append(p)
                for fc in FCS:
                    self.stt("dve", g_ag.t[:, fc, :], pn[fc].t[:, 0:TT], -1.0 / 64.0, yT.t[:, fc, :], ALU.mult, ALU.add, [pn[fc], yT], [g_ag])
                for fc in FCS:
                    self.act(g_k.t[:, fc, :], g_ag.t[:, fc, :], AF.Square, [g_ag], [g_k])
                pn = []
                for fc in FCS:
                    p = bank()
                    self.mm(p.t[:, 0:TT], C["onesblk"].t[:], g_k.t[:, fc, :], True, True, [C["onesblk"], g_k], [p])
                    pn.append(p)
                for fc in FCS:
                    self.act(g_kk.t[:, fc, :], pn[fc].t[:, 0:TT], AF.Ln, [pn[fc]], [g_kk], bias=64e-5, scale=1.0 / 64.0)
                self.act(g_kk.t[:], g_kk.t[:], AF.Exp, [g_kk], [g_kk], scale=-0.5)
                self.tt("dve", g_ag.t[:], g_ag.t[:], g_kk.t[:], ALU.mult, [g_ag, g_kk], [g_ag])
                for fc in FCS:
                    self.ts("pool", g_ag.t[:, fc, :], g_ag.t[:, fc, :], V("ln_w", fc), V("ln_b", fc), ALU.mult, ALU.add, [g_ag, self.vecs], [g_ag])
                self.tt("pool", g_ag.t[:], g_ag.t[:], bonus.t[:], ALU.add, [g_ag, bonus], [g_ag])
                pn = []
                for fc in FCS:
                    p = bank()
                    self.mm(p.t[:, 0:TT], L2.t[:, 2, fc * 128:(fc + 1) * 128], midB.t[:, :], True, False, [L2, midB], [p])
                    self.mm(p.t[:, 0:TT], L2.t[0:32, 3, fc * 128:(fc + 1) * 128], midC.t[0:32, :], False, True, [L2, midC], [p])
                    pn.append(p)
                for fc in FCS:
                    self.tt("dve", mixo.t[:, fc, :], g_ag.t[:, fc, :], pn[fc].t[:, 0:TT], ALU.mult, [g_ag, pn[fc]], [mixo])
                for h in FCS:
                    self.act(g_sq.t[:, h, :], og.t[:, h, :], AF.Square, [og], [g_sq])
                pn = []
                for h in FCS:
                    p = bank()
                    self.mm(p.t[:, 0:TT], self.ones.t[:], g_sq.t[:, h, :], True, True, [self.ones, g_sq], [p])
                    pn.append(p)
                for h in FCS:
                    self.act(g_rn.t[:, h, :], pn[h].t[:, 0:TT], AF.Ln, [pn[h]], [g_rn], bias=1e-5, scale=1.0 / 128.0)
                self.act(g_rn.t[:], g_rn.t[:], AF.Exp, [g_rn], [g_rn], scale=-0.5)
                for h in FCS:
                    self.stt("dve", g_kk.t[:, h, :], og.t[:, h, :], V("gnorm", h), g_rn.t[:, h, :], ALU.mult, ALU.mult, [og, g_rn, self.vecs], [g_kk])
                self.tt("pool", mixo.t[:, 4:8, :], g_kk.t[:], gsil.t[:], ALU.mult, [g_kk, gsil], [mixo])
                self.dma("pool", mix_v[:, :, t0:t0 + TT], mixo.t[:], [mixo], [self.b_mix])
              except _Stop:
                pass
            self.emit_phase()


def bcast_last(ap, nb):
    dims = [list(d) for d in ap.ap]
    return bass.AP(ap.tensor, ap.offset, [dims[0], dims[1], [0, nb]])


def bcast3(tensor, p0, nmid, midstride, col, nb):
    return bass.AP(tensor, p0 * nmid * midstride + col, [[nmid * midstride, 64], [midstride, nmid], [0, nb]])


Kern.phase_even = phase_even
```
